# Optimizing a Trainium2 kernel written in Bass

```python
import jax
import jax.numpy as jnp
from jax import lax
import numpy as np

D_MODEL = 1024
BATCH = 8
SEQ = 4096
DEPTH = 4

CTX_LEN = 256
GRID_W = 64
EPS = 1e-6
N_BRANCH = 3
BRANCH_W = 1024
CHUNK = 64

GDN_HEADS = 8
GDN_DK = 128
GDN_DV = 128
GDN_QK = GDN_HEADS * GDN_DK
GDN_W = GDN_HEADS * GDN_DV
CONV_K = 5

S5_W = BRANCH_W
S5_H = 16
S5_G = S5_W // S5_H
S5_P = 64
STEP_MIN = 1e-3
STEP_MAX = 1e-1

ML_HEADS = 4
ML_DH = 256
ML_W = ML_HEADS * ML_DH

IN_SIZES = (GDN_QK, GDN_QK, GDN_W, GDN_W, 2 * GDN_HEADS, 2 * GDN_HEADS,
            S5_W, S5_W,
            ML_W, ML_W, ML_W, ML_W, ML_W, 2 * ML_HEADS, 2 * ML_HEADS,
            N_BRANCH * D_MODEL)
N_IN = sum(IN_SIZES)

kernel_name = "hybrid_gdn_s5_mlstm_prefix_dit"


def rms_norm(x, g):
    xf = x.astype(jnp.float32)
    y = xf * lax.rsqrt(jnp.mean(xf * xf, axis=-1, keepdims=True) + EPS)
    return (y * g).astype(x.dtype)


def l2_normalize(t):
    tf = t.astype(jnp.float32)
    return tf * lax.rsqrt(jnp.sum(tf * tf, axis=-1, keepdims=True) + EPS)


def split_cols(p):
    return jnp.split(p, [int(s) for s in np.cumsum(IN_SIZES)[:-1]], axis=-1)


def to_scan_order(t, column_major):
    if not column_major:
        return t
    b, l, d = t.shape
    rows = l // GRID_W
    return t.reshape(b, rows, GRID_W, d).swapaxes(1, 2).reshape(b, l, d)


def from_scan_order(t, column_major):
    if not column_major:
        return t
    b, l, d = t.shape
    rows = l // GRID_W
    return t.reshape(b, GRID_W, rows, d).swapaxes(1, 2).reshape(b, l, d)


def short_conv(x, w):
    pad = CONV_K // 2
    l = x.shape[1]
    xp = jnp.pad(x, ((0, 0), (pad, pad), (0, 0)))
    return sum(w[j] * xp[:, j:j + l] for j in range(CONV_K))


def to_chunks(t):
    b, l, h = t.shape[:3]
    t = t.reshape((b, l // CHUNK, CHUNK, h) + t.shape[3:])
    return jnp.moveaxis(t, (1, 3), (0, 2))


def from_chunks(t):
    n, b, h, c = t.shape[:4]
    return jnp.moveaxis(t, (0, 2), (1, 3)).reshape((b, n * c, h) + t.shape[4:])


def prefix_bidir(run, ctx_in, lat_in, s0):
    flip = lambda arrs: tuple(jnp.flip(a, 1) for a in arrs)
    y_cf, s_f = run(ctx_in, 0, s0)
    y_cb, s_b = run(flip(ctx_in), 1, s0)
    y_lf, _ = run(lat_in, 0, s_f)
    y_lb, _ = run(flip(lat_in), 1, s_b)
    return y_cf + jnp.flip(y_cb, 1), y_lf + jnp.flip(y_lb, 1)


def gdn_chunked(q, k, v, g, beta, s0):
    q, k, v, g, beta = (to_chunks(t.astype(jnp.float32)) for t in (q, k, v, g, beta))
    tril = jnp.tril(jnp.ones((CHUNK, CHUNK), dtype=bool))
    strict = jnp.tril(jnp.ones((CHUNK, CHUNK), dtype=bool), -1)
    gc = jnp.cumsum(g, axis=-1)
    seg = jnp.where(tril, gc[..., :, None] - gc[..., None, :], 0.0)
    decay_mat = jnp.where(tril, jnp.exp(seg), 0.0)
    kb = k * beta[..., None]
    a = jnp.where(strict, jnp.einsum("nbhid,nbhjd->nbhij", kb, k) * decay_mat, 0.0)
    eye = jnp.eye(CHUNK, dtype=jnp.float32)
    t_inv = lax.linalg.triangular_solve(eye + a, jnp.broadcast_to(eye, a.shape),
                                        left_side=True, lower=True, unit_diagonal=True)
    u = jnp.einsum("nbhij,nbhje->nbhie", t_inv, v * beta[..., None])
    w = jnp.einsum("nbhij,nbhjd->nbhid", t_inv, kb * jnp.exp(gc)[..., None])
    qk = jnp.where(tril, jnp.einsum("nbhid,nbhjd->nbhij", q, k) * decay_mat, 0.0)
    k_end = k * jnp.exp(gc[..., -1:] - gc)[..., None]
    q_dec = q * jnp.exp(gc)[..., None]
    g_end = jnp.exp(gc[..., -1])

    def step(s, xs):
        u_c, w_c, qk_c, kend_c, qdec_c, gend_c = xs
        v_new = u_c - jnp.einsum("bhid,bhde->bhie", w_c, s)
        o = jnp.einsum("bhid,bhde->bhie", qdec_c, s) + jnp.einsum("bhij,bhje->bhie", qk_c, v_new)
        s = s * gend_c[..., None, None] + jnp.einsum("bhid,bhie->bhde", kend_c, v_new)
        return s, o

    s_fin, o = lax.scan(step, s0.astype(jnp.float32), (u, w, qk, k_end, q_dec, g_end))
    return from_chunks(o), s_fin


def gdn_branch(cols_ctx, cols_lat, conv_w, a_log, dt_bias, norm_g):
    def prep(cols):
        q, k, v, z, a, b = cols
        qkv = jax.nn.silu(short_conv(jnp.concatenate([q, k, v], axis=-1), conv_w))
        q, k, v = jnp.split(qkv, [GDN_QK, 2 * GDN_QK], axis=-1)
        heads = lambda t: t.reshape(t.shape[:2] + (GDN_HEADS, -1))
        q = l2_normalize(heads(q)) * GDN_DK ** -0.5
        k = l2_normalize(heads(k))
        a = a.reshape(a.shape[:2] + (2, GDN_HEADS)).astype(jnp.float32)
        g = -jnp.exp(a_log.astype(jnp.float32)) * jax.nn.softplus(a + dt_bias.astype(jnp.float32))
        beta = jax.nn.sigmoid(b.reshape(b.shape[:2] + (2, GDN_HEADS)).astype(jnp.float32))
        return (q, k, heads(v), g, beta), z

    def run(inp, d, s0):
        q, k, v, g, beta = inp
        return gdn_chunked(q, k, v, g[:, :, d], beta[:, :, d], s0)

    inp_c, z_c = prep(cols_ctx)
    inp_l, z_l = prep(cols_lat)
    s0 = jnp.zeros((z_l.shape[0], GDN_HEADS, GDN_DK, GDN_DV), jnp.float32)
    o_c, o_l = prefix_bidir(run, inp_c, inp_l, s0)
    post = lambda o, z: rms_norm(o, norm_g).reshape(z.shape).astype(z.dtype) * jax.nn.silu(z)
    return post(o_c, z_c), post(o_l, z_l)


def s5_discretise(lam_re, lam_im, log_step, b_re, b_im):
    lam_re, lam_im, log_step, b_re, b_im = (t.astype(jnp.float32) for t in (lam_re, lam_im, log_step, b_re, b_im))
    step = jnp.exp(log_step)[:, None]
    mag = jnp.exp(lam_re * step)
    lb_re, lb_im = mag * jnp.cos(lam_im * step), mag * jnp.sin(lam_im * step)
    den = lam_re * lam_re + lam_im * lam_im
    f_re = ((lb_re - 1.0) * lam_re + lb_im * lam_im) / den
    f_im = (lb_im * lam_re - (lb_re - 1.0) * lam_im) / den
    bb_re = f_re[..., None] * b_re - f_im[..., None] * b_im
    bb_im = f_re[..., None] * b_im + f_im[..., None] * b_re
    return lb_re, lb_im, bb_re, bb_im


def s5_scan(u, lb_re, lb_im, bb_re, bb_im, x0_re, x0_im):
    bu_re = jnp.einsum("gph,blgh->blgp", bb_re, u)
    bu_im = jnp.einsum("gph,blgh->blgp", bb_im, u)
    bu_re = bu_re.at[:, 0].add(lb_re * x0_re - lb_im * x0_im)
    bu_im = bu_im.at[:, 0].add(lb_re * x0_im + lb_im * x0_re)
    l = u.shape[1]
    a_re = jnp.broadcast_to(lb_re[None, None], (1, l) + lb_re.shape)
    a_im = jnp.broadcast_to(lb_im[None, None], (1, l) + lb_im.shape)

    def combine(e1, e2):
        a1r, a1i, b1r, b1i = e1
        a2r, a2i, b2r, b2i = e2
        return (a2r * a1r - a2i * a1i, a2r * a1i + a2i * a1r,
                a2r * b1r - a2i * b1i + b2r, a2r * b1i + a2i * b1r + b2i)

    _, _, xr, xi = lax.associative_scan(combine, (a_re, a_im, bu_re, bu_im), axis=1)
    return xr, xi


def s5_branch(cols_ctx, cols_lat, lam_re, lam_im, log_step, b_re, b_im, c_re, c_im, d_skip, w_glu, b_glu):
    disc = [s5_discretise(lam_re[d], lam_im[d], log_step[d], b_re, b_im) for d in range(2)]
    c_re32, c_im32 = c_re.astype(jnp.float32), c_im.astype(jnp.float32)

    def run(inp, d, s0):
        (u,) = inp
        xr, xi = s5_scan(u, *disc[d], *s0)
        y = jnp.einsum("ghp,blgp->blgh", c_re32, xr) - jnp.einsum("ghp,blgp->blgh", c_im32, xi)
        return y, (xr[:, -1], xi[:, -1])

    grp = lambda u: u.reshape(u.shape[:2] + (S5_G, S5_H)).astype(jnp.float32)
    u_c, z_c = cols_ctx
    u_l, z_l = cols_lat
    zero = jnp.zeros((u_l.shape[0], S5_G, S5_P), jnp.float32)
    y_c, y_l = prefix_bidir(run, (grp(u_c),), (grp(u_l),), (zero, zero))

    def post(y, u, z):
        y = jax.nn.gelu(y.reshape(z.shape).astype(z.dtype) + d_skip * u)
        ab = y @ w_glu + b_glu
        return ab[..., :S5_W] * jax.nn.sigmoid(ab[..., S5_W:]) * jax.nn.silu(z)

    return post(y_c, u_c, z_c), post(y_l, u_l, z_l)


def mlstm_chunked(q, k, v, i_pre, log_f, state):
    q, k, v, i_pre, log_f = (to_chunks(t.astype(jnp.float32)) for t in (q, k, v, i_pre, log_f))
    tril = jnp.tril(jnp.ones((CHUNK, CHUNK), dtype=bool))
    b_cum = jnp.cumsum(log_f, axis=-1)
    log_d = jnp.where(tril, b_cum[..., :, None] - b_cum[..., None, :] + i_pre[..., None, :], -jnp.inf)
    m_intra = jnp.max(log_d, axis=-1)
    qk = jnp.einsum("nbhid,nbhjd->nbhij", q, k)
    log_end = b_cum[..., -1:] - b_cum + i_pre
    b_end = b_cum[..., -1]

    def step(carry, xs):
        c_s, n_s, m_s = carry
        q_c, k_c, v_c, qk_c, ld_c, mi_c, bc_c, le_c, be_c = xs
        m_t = jnp.maximum(bc_c + m_s[..., None], mi_c)
        w_intra = jnp.exp(ld_c - m_t[..., None]) * qk_c
        w_inter = jnp.exp(bc_c + m_s[..., None] - m_t)
        num = (jnp.einsum("bhij,bhje->bhie", w_intra, v_c)
               + w_inter[..., None] * jnp.einsum("bhid,bhed->bhie", q_c, c_s))
        den = jnp.sum(w_intra, axis=-1) + w_inter * jnp.einsum("bhid,bhd->bhi", q_c, n_s)
        h = num / jnp.maximum(jnp.abs(den), jnp.exp(-m_t))[..., None]
        m_new = jnp.maximum(be_c + m_s, jnp.max(le_c, axis=-1))
        w_state = jnp.exp(le_c - m_new[..., None])
        decay = jnp.exp(be_c + m_s - m_new)
        c_new = decay[..., None, None] * c_s + jnp.einsum("bhj,bhje,bhjd->bhed", w_state, v_c, k_c)
        n_new = decay[..., None] * n_s + jnp.einsum("bhj,bhjd->bhd", w_state, k_c)
        return (c_new, n_new, m_new), h

    init = tuple(s.astype(jnp.float32) for s in state)
    state, h = lax.scan(step, init, (q, k, v, qk, log_d, m_intra, b_cum, log_end, b_end))
    return from_chunks(h), state


def mlstm_branch(cols_ctx, cols_lat, i_bias, f_bias, norm_g):
    def prep(cols):
        q, k, v, o, z, ig, fg = cols
        heads = lambda t: t.reshape(t.shape[:2] + (ML_HEADS, ML_DH))
        dirs = lambda t: t.reshape(t.shape[:2] + (2, ML_HEADS)).astype(jnp.float32)
        i_pre = dirs(ig) + i_bias.astype(jnp.float32)
        log_f = jax.nn.log_sigmoid(dirs(fg) + f_bias.astype(jnp.float32))
        return (heads(q), heads(k) * ML_DH ** -0.5, heads(v), i_pre, log_f), o, z

    def run(inp, d, s0):
        q, k, v, i_pre, log_f = inp
        return mlstm_chunked(q, k, v, i_pre[:, :, d], log_f[:, :, d], s0)

    inp_c, o_c, z_c = prep(cols_ctx)
    inp_l, o_l, z_l = prep(cols_lat)
    b = z_l.shape[0]
    s0 = (jnp.zeros((b, ML_HEADS, ML_DH, ML_DH), jnp.float32),
          jnp.zeros((b, ML_HEADS, ML_DH), jnp.float32),
          jnp.zeros((b, ML_HEADS), jnp.float32))
    h_c, h_l = prefix_bidir(run, inp_c, inp_l, s0)

    def post(h, o, z):
        h = jax.nn.sigmoid(o) * h.reshape(z.shape).astype(z.dtype)
        h = rms_norm(h.reshape(h.shape[:2] + (ML_HEADS, ML_DH)), norm_g).reshape(z.shape)
        return h * jax.nn.silu(z)

    return post(h_c, o_c, z_c), post(h_l, o_l, z_l)


def merge_branches(ys, gate_cols, w_branch, w_out):
    gates = jax.nn.sigmoid(gate_cols)
    merged = sum(gates[..., i * D_MODEL:(i + 1) * D_MODEL] * (ys[i] @ w_branch[i]) for i in range(N_BRANCH))
    return merged @ w_out


def hybrid_layer(x_ctx, x_lat, mod_ctx, mod_lat, column_major, last,
                 norm_g, w_in, gdn_conv, gdn_a_log, gdn_dt_bias, gdn_norm_g,
                 s5_lam_re, s5_lam_im, s5_log_step, s5_b_re, s5_b_im, s5_c_re, s5_c_im,
                 s5_d, s5_w_glu, s5_b_glu, ml_i_bias, ml_f_bias, ml_norm_g, w_branch, w_out):
    shift_c, scale_c, gate_c = jnp.split(mod_ctx, 3, axis=-1)
    shift_l, scale_l, gate_l = jnp.split(mod_lat[:, None, :], 3, axis=-1)
    h_ctx = rms_norm(x_ctx, norm_g) * (1.0 + scale_c) + shift_c
    h_lat = to_scan_order(rms_norm(x_lat, norm_g) * (1.0 + scale_l) + shift_l, column_major)
    cc = split_cols(h_ctx @ w_in)
    cl = split_cols(h_lat @ w_in)
    ya_c, ya_l = gdn_branch(cc[0:6], cl[0:6], gdn_conv, gdn_a_log, gdn_dt_bias, gdn_norm_g)
    yb_c, yb_l = s5_branch(cc[6:8], cl[6:8], s5_lam_re, s5_lam_im, s5_log_step, s5_b_re, s5_b_im,
                           s5_c_re, s5_c_im, s5_d, s5_w_glu, s5_b_glu)
    yc_c, yc_l = mlstm_branch(cc[8:15], cl[8:15], ml_i_bias, ml_f_bias, ml_norm_g)
    out_l = merge_branches((ya_l, yb_l, yc_l), cl[15], w_branch, w_out)
    x_lat_new = x_lat + gate_l * from_scan_order(out_l, column_major)
    if last:
        return x_ctx, x_lat_new
    out_c = merge_branches((ya_c, yb_c, yc_c), cc[15], w_branch, w_out)
    return x_ctx + gate_c * out_c, x_lat_new


def setup_inputs(seed: int = 0) -> dict:
    key = jax.random.key(seed)
    ks = list(jax.random.split(key, 32))

    def nrm(shape, scale):
        return jax.random.normal(ks.pop(), shape, jnp.float32) * scale

    def unif(shape, lo, hi):
        return jax.random.uniform(ks.pop(), shape, jnp.float32, lo, hi)

    x = nrm((BATCH, SEQ, D_MODEL), 1.0)
    c = nrm((BATCH, D_MODEL), 1.0)
    ctx = nrm((BATCH, CTX_LEN, D_MODEL), 1.0)
    c_ctx = nrm((D_MODEL,), 1.0)
    w_ada = nrm((DEPTH, D_MODEL, 3 * D_MODEL), 0.5 * D_MODEL ** -0.5)
    b_ada = nrm((DEPTH, 3 * D_MODEL), 0.02)
    norm_g = 1.0 + nrm((DEPTH, D_MODEL), 0.05)
    w_in = nrm((DEPTH, D_MODEL, N_IN), D_MODEL ** -0.5)
    gdn_conv = nrm((DEPTH, CONV_K, 2 * GDN_QK + GDN_W), CONV_K ** -0.5)
    gdn_a_log = jnp.log(unif((DEPTH, 2, GDN_HEADS), 1.0, 16.0))
    dt = jnp.exp(unif((DEPTH, 2, GDN_HEADS), float(np.log(STEP_MIN)), float(np.log(STEP_MAX))))
    gdn_dt_bias = dt + jnp.log(-jnp.expm1(-dt))
    gdn_norm_g = 1.0 + nrm((DEPTH, GDN_DV), 0.05)
    n_idx = jnp.arange(S5_P, dtype=jnp.float32)
    s5_lam_re = -0.5 + nrm((DEPTH, 2, S5_G, S5_P), 0.01)
    s5_lam_im = jnp.pi * n_idx + nrm((DEPTH, 2, S5_G, S5_P), 0.01)
    s5_log_step = unif((DEPTH, 2, S5_G), float(np.log(STEP_MIN)), float(np.log(STEP_MAX)))
    s5_b_re = nrm((DEPTH, S5_G, S5_P, S5_H), (2 * S5_H) ** -0.5)
    s5_b_im = nrm((DEPTH, S5_G, S5_P, S5_H), (2 * S5_H) ** -0.5)
    s5_c_re = nrm((DEPTH, S5_G, S5_H, S5_P), S5_P ** -0.5)
    s5_c_im = nrm((DEPTH, S5_G, S5_H, S5_P), S5_P ** -0.5)
    s5_d = nrm((DEPTH, S5_W), 0.5)
    s5_w_glu = nrm((DEPTH, S5_W, 2 * S5_W), S5_W ** -0.5)
    s5_b_glu = nrm((DEPTH, 2 * S5_W), 0.02)
    ml_i_bias = nrm((DEPTH, 2, ML_HEADS), 0.1)
    ml_f_bias = jnp.linspace(3.0, 6.0, ML_HEADS, dtype=jnp.float32) + nrm((DEPTH, 2, ML_HEADS), 0.1)
    ml_norm_g = 1.0 + nrm((DEPTH, ML_DH), 0.05)
    w_branch = nrm((DEPTH, N_BRANCH, BRANCH_W, D_MODEL), BRANCH_W ** -0.5)
    w_out = nrm((DEPTH, D_MODEL, D_MODEL), D_MODEL ** -0.5)
    final_norm_g = 1.0 + nrm((D_MODEL,), 0.05)
    return {"x": x, "c": c, "ctx": ctx, "c_ctx": c_ctx, "w_ada": w_ada, "b_ada": b_ada,
            "norm_g": norm_g, "w_in": w_in, "gdn_conv": gdn_conv, "gdn_a_log": gdn_a_log,
            "gdn_dt_bias": gdn_dt_bias, "gdn_norm_g": gdn_norm_g,
            "s5_lam_re": s5_lam_re, "s5_lam_im": s5_lam_im, "s5_log_step": s5_log_step,
            "s5_b_re": s5_b_re, "s5_b_im": s5_b_im, "s5_c_re": s5_c_re, "s5_c_im": s5_c_im,
            "s5_d": s5_d, "s5_w_glu": s5_w_glu, "s5_b_glu": s5_b_glu,
            "ml_i_bias": ml_i_bias, "ml_f_bias": ml_f_bias, "ml_norm_g": ml_norm_g,
            "w_branch": w_branch, "w_out": w_out, "final_norm_g": final_norm_g}


def reference(x, c, ctx, c_ctx, w_ada, b_ada, norm_g, w_in, gdn_conv, gdn_a_log, gdn_dt_bias, gdn_norm_g,
              s5_lam_re, s5_lam_im, s5_log_step, s5_b_re, s5_b_im, s5_c_re, s5_c_im, s5_d, s5_w_glu, s5_b_glu,
              ml_i_bias, ml_f_bias, ml_norm_g, w_branch, w_out, final_norm_g):
    x_ctx, x_lat = ctx, x
    silu_c, silu_cc = jax.nn.silu(c), jax.nn.silu(c_ctx)
    for l in range(DEPTH):
        mod_lat = silu_c @ w_ada[l] + b_ada[l]
        mod_ctx = silu_cc @ w_ada[l] + b_ada[l]
        x_ctx, x_lat = hybrid_layer(
            x_ctx, x_lat, mod_ctx, mod_lat, l % 2 == 1, l == DEPTH - 1,
            norm_g[l], w_in[l], gdn_conv[l], gdn_a_log[l], gdn_dt_bias[l], gdn_norm_g[l],
            s5_lam_re[l], s5_lam_im[l], s5_log_step[l], s5_b_re[l], s5_b_im[l], s5_c_re[l], s5_c_im[l],
            s5_d[l], s5_w_glu[l], s5_b_glu[l], ml_i_bias[l], ml_f_bias[l], ml_norm_g[l],
            w_branch[l], w_out[l])
    return rms_norm(x_lat, final_norm_g)
```

```python
from contextlib import ExitStack
import os
import numpy as np
import concourse.bass as bass
import concourse.mybir as mybir
from concourse.bass_utils import run_bass_kernel_spmd

F32 = mybir.dt.float32
AF = mybir.ActivationFunctionType
ALU = mybir.AluOpType
AX = mybir.AxisListType

D = 1024
KC = 8
CTX = 256
EPS = 1e-6
N_CORES = 8
NEG = -30000.0

SEGS = [("QKV", 0, 3072, "copy"), ("ZG", 3072, 1024, "silu"), ("AB", 4096, 32, "copy"),
        ("U5", 4128, 1024, "copy"), ("Z5", 5152, 1024, "silu"), ("MQ", 6176, 1024, "copy"),
        ("MK", 7200, 1024, "copy"), ("MV", 8224, 1024, "copy"), ("MO", 9248, 1024, "sigmoid"),
        ("MZ", 10272, 1024, "silu"), ("IF", 11296, 16, "copy"), ("GATE", 11312, 3072, "sigmoid")]

C_IDENT, C_ONES, C_NEGONES, C_UP, C_LO, C_NUI, C_NUS, C_NLI, C_NLS = range(9)


def make_consts():
    x = np.arange(128)[:, None]
    y = np.arange(128)[None, :]
    c = np.zeros((128, 9, 128), np.float32)
    c[:, C_IDENT] = (x == y)
    c[:, C_ONES] = 1.0
    c[:, C_NEGONES] = -1.0
    c[:, C_UP] = (x <= y)
    c[:, C_LO] = (x >= y)
    c[:, C_NUI] = np.where(x <= y, 0.0, NEG)
    c[:, C_NUS] = np.where(x < y, 0.0, NEG)
    c[:, C_NLI] = np.where(x >= y, 0.0, NEG)
    c[:, C_NLS] = np.where(x > y, 0.0, NEG)
    return c


class Cfg:
    def __init__(self, L=4096, GW=64, DEPTH=4):
        self.L, self.GW, self.DEPTH = L, GW, DEPTH
        self.ROWS = L // GW
        self.T = CTX + L
        self.NT = self.T // 128
        self.groups = [(0, 256)] + [(CTX + 512 * i, 512) for i in range(L // 512)]


class Res:
    __slots__ = ("name", "lw", "rd")

    def __init__(self, name=""):
        self.name = name
        self.lw = None
        self.rd = []


class Buf:
    def __init__(self, t, name):
        self.t = t
        self.r = Res(name)

    def __getitem__(self, k):
        return self.t[k]


class FW:
    ENG = ("pe", "act", "dve", "pool", "sp")

    def __init__(self, nc, n_dma_slots=16):
        self.nc = nc
        self.sem, self.cnt = {}, {}
        self.prog = {e: [] for e in self.ENG}
        self.waited = {e: {} for e in self.ENG}
        for e in self.ENG:
            self.sem[e] = nc.alloc_semaphore(name=f"s_{e}")
            self.cnt[e] = 0
        self.slots = []
        for i in range(n_dma_slots):
            k = f"dma{i}"
            self.sem[k] = nc.alloc_semaphore(name=f"s_{k}")
            self.cnt[k] = 0
            self.slots.append(k)
        self.slot_rr = 0
        self.n_ops = 0

    def _wait(self, eng, k, v):
        wl = self.waited[eng]
        if (k != eng or eng != "pe") and wl.get(k, 0) < v:
            wl[k] = v
            self.prog[eng].append(lambda h, sem=self.sem[k], v=v: h.wait_ge(sem, v))

    def _deps(self, eng, reads, writes):
        deps = {}

        def add(ts):
            if ts is not None and deps.get(ts[0], 0) < ts[1]:
                deps[ts[0]] = ts[1]
        for r in reads:
            add(r.lw)
        for w in writes:
            add(w.lw)
            for t in w.rd:
                add(t)
        for k, v in deps.items():
            self._wait(eng, k, v)

    def _mark(self, ts, reads, writes):
        for r in reads:
            r.rd.append(ts)
            if len(r.rd) > 64:
                best = {}
                for k, v in r.rd:
                    if best.get(k, 0) < v:
                        best[k] = v
                r.rd = list(best.items())
        for w in writes:
            w.lw = ts
            w.rd = []

    def op(self, eng, fn, reads=(), writes=()):
        self._deps(eng, reads, writes)
        self.cnt[eng] += 1
        self.prog[eng].append(lambda h, fn=fn, sem=self.sem[eng]: fn(h).then_inc(sem, 1))
        self._mark((eng, self.cnt[eng]), reads, writes)
        self.n_ops += 1

    def dma(self, out, in_, reads=(), writes=(), q="sp"):
        self._deps(q, reads, writes)
        k = self.slots[self.slot_rr]
        self.slot_rr = (self.slot_rr + 1) % len(self.slots)
        self._wait(q, k, self.cnt[k])
        self.cnt[k] += 16
        self.prog[q].append(
            lambda h, sem=self.sem[k], out=out, in_=in_: h.dma_start(out=out, in_=in_).then_inc(sem, 16))
        self._mark((k, self.cnt[k]), reads, writes)
        self.n_ops += 1

    def barrier(self, engines=None):
        for e in (engines or self.ENG):
            for k in self.sem:
                self._wait(e, k, self.cnt[k])

    def emit(self):
        self.barrier(["sp"])
        with self.nc.Block() as block:
            @block.tensor
            def _(h):
                for f in self.prog["pe"]:
                    f(h)

            @block.scalar
            def _(h):
                for f in self.prog["act"]:
                    f(h)

            @block.vector
            def _(h):
                for f in self.prog["dve"]:
                    f(h)

            @block.gpsimd
            def _(h):
                for f in self.prog["pool"]:
                    f(h)

            @block.sync
            def _(h):
                for f in self.prog["sp"]:
                    f(h)


def bc(ap, shape):
    return ap.to_broadcast(list(shape))


class KB:
    def __init__(self, cfg, dbg=(), stages=None):
        self.cfg = cfg
        self.dbg = set(dbg)
        self.stages = stages
        self.nc = nc = bass.Bass("TRN2", target_bir_lowering=False)
        self.fw = FW(nc)
        self.inp = {}
        self.scr = {}
        self.P = nc.alloc_psum_tensor("P", [128, 4096], F32)
        self.pr = [Res(f"bank{i}") for i in range(8)]
        self.pptr = 0
        self.uid = 0
        self.stop_at = float(os.environ.get('KSTOP', '999'))

    def din(self, name, shape):
        self.inp[name] = self.nc.dram_tensor(name, list(shape), F32, kind="ExternalInput").ap()
        return self.inp[name]

    def dscr(self, name, shape):
        kind = "ExternalOutput" if name in self.dbg else "Internal"
        self.scr[name] = self.nc.dram_tensor(name, list(shape), F32, kind=kind).ap()
        return self.scr[name]

    def sb(self, es, name, shape):
        self.uid += 1
        t = es.enter_context(self.nc.sbuf_tensor(f"{name}_{self.uid}", list(shape), F32))
        return Buf(t, name)

    def pb(self, n=1):
        if self.pptr % n:
            self.pptr += n - self.pptr % n
        if self.pptr + n > 8:
            self.pptr = 0
        b0 = self.pptr
        self.pptr += n
        return self.P[:, b0 * 512:(b0 + n) * 512], self.pr[b0:b0 + n]

    def cst(self, i):
        return self.consts[:, i, :]

    def mm(self, out, lhsT, rhs, reads, writes, start=True, stop=True):
        self.fw.op("pe", lambda h: h.matmul(out, lhsT=lhsT, rhs=rhs, start=start, stop=stop),
                   reads=reads, writes=writes)

    def tr(self, out, in_, reads, writes):
        n = in_.shape[0]
        ident = self.consts[0:n, C_IDENT, 0:n]
        self.fw.op("pe", lambda h: h.transpose(out=out, in_=in_, identity=ident),
                   reads=list(reads) + [self.consts.r], writes=writes)

    def act(self, out, in_, func, reads, writes, bias=None, scale=None):
        kw = {}
        if bias is not None:
            kw["bias"] = bias
        if scale is not None:
            kw["scale"] = scale
        self.fw.op("act", lambda h: h.activation(out=out, in_=in_, func=func, **kw), reads=reads, writes=writes)

    def tt(self, eng, out, in0, in1, op, reads, writes):
        self.fw.op(eng, lambda h: h.tensor_tensor(out=out, in0=in0, in1=in1, op=op), reads=reads, writes=writes)

    def ts(self, eng, out, in0, s1, op0, reads, writes, s2=None, op1=None):
        if op1 is None:
            self.fw.op(eng, lambda h: h.tensor_scalar(out=out, in0=in0, scalar1=s1, scalar2=None, op0=op0),
                       reads=reads, writes=writes)
        else:
            self.fw.op(eng, lambda h: h.tensor_scalar(out=out, in0=in0, scalar1=s1, scalar2=s2, op0=op0, op1=op1),
                       reads=reads, writes=writes)

    def stt(self, out, in0, scalar, in1, op0, op1, reads, writes):
        self.fw.op("dve", lambda h: h.scalar_tensor_tensor(out=out, in0=in0, scalar=scalar, in1=in1,
                                                           op0=op0, op1=op1), reads=reads, writes=writes)

    def dump(self, name, buf, ap=None):
        if name not in self.dbg or name in self.scr:
            return
        ap = buf[:] if ap is None else ap
        t = self.nc.dram_tensor(name, list(ap.shape), F32, kind="ExternalOutput").ap()
        self.scr[name] = t
        self.fw.dma(t, ap, reads=[buf.r])

    def ck(self, n):
        return n >= self.stop_at

    def on(self, name):
        return self.stages is None or name in self.stages

    def build(self):
        cfg, nc, fw = self.cfg, self.nc, self.fw
        T, L, DEPTH = cfg.T, cfg.L, cfg.DEPTH
        self.din("x", [L, D])
        self.din("ctx", [CTX, D])
        self.din("cvec", [128, KC, 2])
        self.din("w_ada", [DEPTH, D, 3 * D])
        self.din("b_ada_c", [128, DEPTH, 24])
        self.din("norm_g_c", [128, DEPTH, KC])
        self.din("w_in", [DEPTH, D, 14384])
        self.din("consts", [128, 9, 128])
        self.din("final_norm_g", [1, D])
        self.din("ml_small", [DEPTH, 272])
        self.din("gdn_small", [DEPTH, 160])
        self.din("gdn_conv_c", [128, DEPTH, 5, 24])
        self.din("s5_lam", [128, DEPTH, 32, 2, 2])
        self.din("s5_lstep", [128, DEPTH, 32, 2])
        self.din("s5_Bblk", [32, DEPTH, 32, 2, 128])
        self.din("s5_Cblk", [128, DEPTH, 32, 2, 32])
        self.din("s5_d_q", [32, DEPTH, 32])
        self.din("b_glu_c", [128, DEPTH, 16])
        self.din("s5_w_glu", [DEPTH, D, 2 * D])
        self.din("w_branch", [DEPTH, 3, D, D])
        self.din("w_out", [DEPTH, D, D])
        self.out = nc.dram_tensor("y", [L, D], F32, kind="ExternalOutput").ap()
        self.dscr("XT", [KC, 128, T])
        for name, c0, n, f in SEGS:
            self.dscr(name, [n, T])
        for name in ("YA", "YB", "YC", "OUTT"):
            self.dscr(name, [D, T])
        self.dscr("HF", [T, D])
        self.dscr("Y5", [D, T])

        with ExitStack() as es:
            self.consts = self.sb(es, "consts", [128, 9, 128])
            fw.dma(self.consts[:], self.inp["consts"], writes=[self.consts.r])
            self.mod = self.sb(es, "mod", [128, DEPTH, 24, 2])
            self.gs = self.sb(es, "gs", [128, DEPTH, KC, 2])
            if self.on("xin"):
                self.stage_xin()
                fw.barrier()
            if self.on("mods"):
                self.stage_mods()
                fw.barrier()
            for l in range(DEPTH):
                with ExitStack() as esl:
                    if self.on("norm"):
                        hT = self.sb(esl, "hT", [128, KC, T])
                        self.stage_norm(l, hT)
                        fw.barrier()
                        if self.on("inproj"):
                            self.stage_inproj(l, hT)
                            fw.barrier()
                if self.on("mlstm"):
                    self.stage_mlstm(l)
                    fw.barrier()
                if self.on("gdn"):
                    self.stage_gdn(l)
                    fw.barrier()
                if self.on("s5"):
                    self.stage_s5(l)
                    fw.barrier()
                    self.stage_s5post(l)
                    fw.barrier()
                if self.on("merge"):
                    self.stage_merge(l)
                    fw.barrier()
            if self.on("final"):
                self.stage_final()
        fw.emit()
        return nc

    def stage_xin(self):
        cfg, fw = self.cfg, self.fw
        XTv = self.scr["XT"].rearrange("k p t -> p k t")
        with ExitStack() as es:
            xt = [self.sb(es, f"xin{i}", [128, D]) for i in range(2)]
            xo = [self.sb(es, f"xo{i}", [128, KC, 128]) for i in range(2)]
            for tl in range(cfg.NT):
                b = tl % 2
                src = (self.inp["ctx"][tl * 128:(tl + 1) * 128, :] if tl < 2
                       else self.inp["x"][(tl - 2) * 128:(tl - 1) * 128, :])
                fw.dma(xt[b][:], src, writes=[xt[b].r])
                pv, prs = self.pb(2)
                pv3 = pv.rearrange("p (k t) -> p k t", t=128)
                for k in range(KC):
                    self.tr(pv3[:, k, :], xt[b][:, k * 128:(k + 1) * 128], [xt[b].r], prs)
                self.act(xo[b][:], pv3, AF.Copy, prs, [xo[b].r])
                fw.dma(XTv[:, :, tl * 128:(tl + 1) * 128], xo[b][:], reads=[xo[b].r])

    def stage_mods(self):
        cfg, fw = self.cfg, self.fw
        with ExitStack() as es:
            sc = self.sb(es, "sc", [128, KC, 2])
            bada = self.sb(es, "bada", [128, cfg.DEPTH, 24])
            ng = self.sb(es, "ng", [128, cfg.DEPTH, KC])
            wt = [self.sb(es, f"wada{i}", [128, KC, 512]) for i in range(2)]
            fw.dma(sc[:], self.inp["cvec"], writes=[sc.r])
            fw.dma(bada[:], self.inp["b_ada_c"], writes=[bada.r])
            fw.dma(ng[:], self.inp["norm_g_c"], writes=[ng.r])
            self.act(sc[:], sc[:], AF.Silu, [sc.r], [sc.r])
            it = 0
            for l in range(cfg.DEPTH):
                pv, prs = self.pb(1)
                pm = pv[:, 0:48].rearrange("p (c v) -> p c v", v=2)
                wv = self.inp["w_ada"][l].rearrange("(k p) n -> p k n", p=128)
                for cg in range(6):
                    b = it % 2
                    it += 1
                    fw.dma(wt[b][:], wv[:, :, cg * 512:(cg + 1) * 512], writes=[wt[b].r])
                    for j in range(4):
                        for k in range(KC):
                            self.mm(pm[:, cg * 4 + j, :], wt[b][:, k, j * 128:(j + 1) * 128], sc[:, k, :],
                                    [wt[b].r, sc.r], prs, start=(k == 0), stop=(k == KC - 1))
                self.tt("dve", self.mod[:, l], pm, bc(bada[:, l].unsqueeze(2), [128, 24, 2]), ALU.add,
                        prs + [bada.r], [self.mod.r])
                self.ts("dve", self.gs[:, l], self.mod[:, l, 8:16, :], 1.0, ALU.add, [self.mod.r], [self.gs.r])
                self.tt("dve", self.gs[:, l], self.gs[:, l], bc(ng[:, l].unsqueeze(2), [128, KC, 2]), ALU.mult,
                        [self.gs.r, ng.r], [self.gs.r])

    def stage_norm(self, l, hT):
        cfg, fw = self.cfg, self.fw
        XTv = self.scr["XT"].rearrange("k p t -> p k t")
        NG = 256
        with ExitStack() as es:
            xg = [self.sb(es, f"xg{i}", [128, KC, NG]) for i in range(2)]
            sq = self.sb(es, "sq", [128, KC, NG])
            rs = self.sb(es, "rs", [128, NG])
            for gi, s0 in enumerate(range(0, cfg.T, NG)):
                v = 1 if s0 < CTX else 0
                b = gi % 2
                fw.dma(xg[b][:], XTv[:, :, s0:s0 + NG], writes=[xg[b].r])
                self.act(sq[:], xg[b][:], AF.Square, [xg[b].r], [sq.r])
                pv, prs = self.pb(1)
                for k in range(KC):
                    self.mm(pv[:, 0:NG], self.cst(C_ONES), sq[:, k, :], [sq.r, self.consts.r], prs,
                            start=(k == 0), stop=(k == KC - 1))
                self.act(rs[:], pv[:, 0:NG], AF.Sqrt, prs, [rs.r], bias=EPS, scale=1.0 / D)
                self.fw.op("dve", lambda h: h.reciprocal(out=rs[:], in_=rs[:]), reads=[rs.r], writes=[rs.r])
                self.tt("dve", xg[b][:], xg[b][:], bc(rs[:].unsqueeze(1), [128, KC, NG]), ALU.mult,
                        [xg[b].r, rs.r], [xg[b].r])
                for k in range(KC):
                    self.act(hT[:, k, s0:s0 + NG], xg[b][:, k, :], AF.Identity, [xg[b].r, self.gs.r, self.mod.r],
                             [hT.r], bias=self.mod[:, l, k, v:v + 1], scale=self.gs[:, l, k, v:v + 1])

    def hview(self, hT, l, k, gi):
        cfg = self.cfg
        s0, n = cfg.groups[gi]
        if gi == 0 or l % 2 == 0:
            return hT[:, k, s0:s0 + n]
        c0 = (s0 - CTX) // cfg.ROWS
        c1 = c0 + n // cfg.ROWS
        return hT[:, k, CTX:].rearrange("p (r c) -> p c r", c=cfg.GW)[:, c0:c1, :]

    def pview(self, ap2d, l, gi):
        cfg = self.cfg
        if gi == 0 or l % 2 == 0:
            return ap2d
        return ap2d.rearrange("p (c r) -> p c r", r=cfg.ROWS)

    def stage_inproj(self, l, hT):
        cfg, fw = self.cfg, self.fw
        wv = self.inp["w_in"][l].rearrange("(k p) n -> p k n", p=128)
        funcs = {"copy": AF.Copy, "silu": AF.Silu, "sigmoid": AF.Sigmoid}
        with ExitStack() as es:
            wt = [self.sb(es, f"win{i}", [128, KC, 128]) for i in range(2)]
            stg = [self.sb(es, f"stg{i}", [128, cfg.T]) for i in range(2)]
            it = 0
            for name, col0, ncols, fname in SEGS:
                for c0 in range(0, ncols, 128):
                    nn = min(128, ncols - c0)
                    b = it % 2
                    it += 1
                    fw.dma(wt[b][:, :, 0:nn], wv[:, :, col0 + c0:col0 + c0 + nn], writes=[wt[b].r])
                    for gi, (s0, n) in enumerate(cfg.groups):
                        pv, prs = self.pb(1)
                        for k in range(KC):
                            self.mm(self.pview(pv[0:nn, 0:n], l, gi), wt[b][:, k, 0:nn], self.hview(hT, l, k, gi),
                                    [wt[b].r, hT.r], prs, start=(k == 0), stop=(k == KC - 1))
                        self.act(stg[b][0:nn, s0:s0 + n], pv[0:nn, 0:n], funcs[fname], prs, [stg[b].r])
                    fw.dma(self.scr[name][c0:c0 + nn, :], stg[b][0:nn, :], reads=[stg[b].r])


    def tile_order(self, d):
        NT = self.cfg.NT
        return [0, 1] + list(range(2, NT)) if d == 0 else [1, 0] + list(range(NT - 1, 1, -1))

    def stage_mlstm(self, l):
        cfg, fw = self.cfg, self.fw
        H = 4
        fm = lambda name: self.scr[name].rearrange("(c p) t -> p c t", p=128)
        MQ, MK, MV, MO, MZ, YC = fm("MQ"), fm("MK"), fm("MV"), fm("MO"), fm("MZ"), fm("YC")
        IFs, HF = self.scr["IF"], self.scr["HF"]
        cr = self.consts.r
        with ExitStack() as es:
            sm = self.sb(es, "mlsm", [128, 272])
            fw.dma(sm[:], bc(self.inp["ml_small"][l:l + 1, :], [128, 272]), writes=[sm.r])
            qT = self.sb(es, "qT", [128, 8, 128])
            kT = self.sb(es, "kT", [128, 8, 128])
            vT = self.sb(es, "vT", [128, 8, 128])
            ktok = self.sb(es, "ktok", [128, 8, 128])
            vtok = self.sb(es, "vtok", [128, H, 257])
            ifT = self.sb(es, "ifT", [16, 128])
            ift = self.sb(es, "ift", [128, 16])
            gt = self.sb(es, "gt", [128, 24])
            CT = self.sb(es, "CT", [128, 8, 257])
            GL = self.sb(es, "GL", [128, H, 128])
            M2 = self.sb(es, "M2", [128, H, 128])
            DT = self.sb(es, "DT", [128, H, 128])
            EB = self.sb(es, "EB", [128, H, 128])
            WT = self.sb(es, "WT", [128, H, 128])
            qd = self.sb(es, "qd", [128, 8, 128])
            kw = self.sb(es, "kw", [128, 8, 128])
            cols = self.sb(es, "cols", [128, 4, H])
            hd = self.sb(es, "hd", [128, H, 256])
            hr = self.sb(es, "hr", [128, H, 257])
            hf = self.sb(es, "hf", [128, H, 256])
            sq = self.sb(es, "msq", [128, H, 256])
            zs = self.sb(es, "zs", [128, 8, 128])
            oT = self.sb(es, "oT", [128, 8, 128])
            yo = self.sb(es, "yo", [128, 8, 128])
            fw.op("pool", lambda h: h.memset(vtok[:], 1.0), writes=[vtok.r])
            for d in range(2):
                Cm, NM, last = (C_UP, C_NUI, 127) if d == 0 else (C_LO, C_NLI, 0)
                fw.op("pool", lambda h: h.memset(CT[:], 0.0), writes=[CT.r])
                for tl in self.tile_order(d):
                    t0 = tl * 128
                    tsl = slice(t0, t0 + 128)
                    fw.dma(qT[:], MQ[:, :, tsl], writes=[qT.r])
                    fw.dma(kT[:], MK[:, :, tsl], writes=[kT.r])
                    fw.dma(vT[:], MV[:, :, tsl], writes=[vT.r])
                    fw.dma(ifT[:], IFs[:, tsl], writes=[ifT.r])
                    self.ts("pool", kT[:], kT[:], 0.0625, ALU.mult, [kT.r], [kT.r])
                    if self.ck(1):
                        return
                    pv, prs = self.pb(2)
                    pv3 = pv.rearrange("p (c t) -> p c t", t=128)
                    for c in range(8):
                        self.tr(pv3[:, c, :], kT[:, c, :], [kT.r], prs)
                    self.act(ktok[:], pv3, AF.Copy, prs, [ktok.r])
                    pv, prs = self.pb(2)
                    pv3 = pv.rearrange("p (c t) -> p c t", t=128)
                    for c in range(8):
                        self.tr(pv3[:, c, :], vT[:, c, :], [vT.r], prs)
                    self.act(vtok[:, :, 0:256].rearrange("p h (c e) -> p h c e", e=128),
                             pv.rearrange("p (h c e) -> p h c e", c=2, e=128), AF.Copy, prs, [vtok.r])
                    if self.ck(2):
                        return
                    pv, prs = self.pb(1)
                    self.tr(pv[:, 0:16], ifT[:], [ifT.r], prs)
                    self.act(ift[:], pv[:, 0:16], AF.Copy, prs, [ift.r])
                    if self.ck(2.2):
                        return
                    self.tt("dve", gt[:, 0:8], ift[:, 0:8], sm[:, 0:8], ALU.add, [ift.r, sm.r], [gt.r])
                    self.tt("dve", gt[:, 16:24], ift[:, 8:16], sm[:, 8:16], ALU.add, [ift.r, sm.r], [gt.r])
                    if self.ck(2.4):
                        return
                    self.act(gt[:, 16:24], gt[:, 16:24], AF.Exp, [gt.r], [gt.r], scale=-1.0)
                    if self.ck(2.6):
                        return
                    self.act(gt[:, 16:24], gt[:, 16:24], AF.Ln, [gt.r], [gt.r], bias=1.0)
                    self.ts("dve", gt[:, 8:16], gt[:, 16:24], -1.0, ALU.mult, [gt.r], [gt.r])
                    if self.ck(3):
                        return
                    ig = gt[:, d * 4:d * 4 + 4]
                    lf = gt[:, 8 + d * 4:8 + d * 4 + 4]
                    colA, colE, den, rc = cols[:, 0, :], cols[:, 1, :], cols[:, 2, :], cols[:, 3, :]
                    self.tt("dve", GL[:], bc(self.cst(Cm).unsqueeze(1), [128, H, 128]),
                            bc(lf.unsqueeze(2), [128, H, 128]), ALU.mult, [gt.r, cr], [GL.r])
                    if self.ck(3.1):
                        return
                    pv, prs = self.pb(1)
                    self.mm(pv[:, 0:H], self.cst(Cm), lf, [gt.r, cr], prs)
                    self.tt("dve", colA, ig, pv[:, 0:H], ALU.subtract, prs + [gt.r], [cols.r])
                    if self.ck(3.2):
                        return
                    self.tt("dve", M2[:], bc(self.cst(NM).unsqueeze(1), [128, H, 128]),
                            bc(colA.unsqueeze(2), [128, H, 128]), ALU.add, [cols.r, cr], [M2.r])
                    if self.ck(3.3):
                        return
                    pv, prs = self.pb(1)
                    self.mm(pv, self.cst(C_ONES), GL[:].rearrange("p h y -> p (h y)"), [GL.r, cr], prs, True, False)
                    self.mm(pv, self.cst(C_IDENT), M2[:].rearrange("p h y -> p (h y)"), [M2.r, cr], prs, False, True)
                    self.act(DT[:].rearrange("p h y -> p (h y)"), pv, AF.Exp, prs, [DT.r])
                    if self.ck(3.4):
                        return
                    pv, prs = self.pb(1)
                    pe3 = pv.rearrange("p (h y) -> p h y", y=128)
                    self.mm(pv, self.cst(C_ONES), GL[:].rearrange("p h y -> p (h y)"), [GL.r, cr], prs)
                    self.act(EB[:].rearrange("p h y -> p (h y)"), pv, AF.Exp, prs, [EB.r])
                    if self.ck(3.5):
                        return
                    self.act(colE, colA, AF.Exp, [cols.r], [cols.r])
                    self.tt("dve", colE, colE, EB[:, :, last], ALU.mult, [cols.r, EB.r], [cols.r])
                    if self.ck(4):
                        return
                    pv, prs = self.pb(1)
                    ps3 = pv.rearrange("p (h y) -> p h y", y=128)
                    for h_ in range(H):
                        for kc in range(2):
                            c = 2 * h_ + kc
                            self.mm(ps3[:, h_, :], kT[:, c, :], qT[:, c, :], [kT.r, qT.r], prs, kc == 0, kc == 1)
                    self.tt("dve", WT[:], ps3, DT[:], ALU.mult, prs + [DT.r], [WT.r])
                    self.tt("dve", qd[:].rearrange("p (h c) t -> p h c t", c=2),
                            qT[:].rearrange("p (h c) t -> p h c t", c=2),
                            bc(EB[:].unsqueeze(2), [128, H, 2, 128]), ALU.mult, [qT.r, EB.r], [qd.r])
                    self.tt("pool", kw[:].rearrange("p (h c) t -> p h c t", c=2),
                            ktok[:].rearrange("p (h c) t -> p h c t", c=2),
                            bc(colE.unsqueeze(2).unsqueeze(3), [128, H, 2, 128]), ALU.mult, [ktok.r, cols.r], [kw.r])
                    if self.ck(5):
                        return
                    pv, prs = self.pb(4)
                    pn = pv.rearrange("p (h y) -> p h y", y=512)
                    for h_ in range(H):
                        self.mm(pn[:, h_, 0:257], WT[:, h_, :], vtok[:, h_, :], [WT.r, vtok.r], prs, True, False)
                        for kc in range(2):
                            c = 2 * h_ + kc
                            self.mm(pn[:, h_, 0:257], qd[:, c, :], CT[:, c, :], [qd.r, CT.r], prs, False, kc == 1)
                    self.act(hr[:], pn[:, :, 0:257], AF.Copy, prs, [hr.r])
                    self.act(den, hr[:, :, 256], AF.Abs, [hr.r], [cols.r])
                    self.ts("dve", den, den, 1.0, ALU.max, [cols.r], [cols.r])
                    fw.op("dve", lambda h: h.reciprocal(out=rc, in_=den), reads=[cols.r], writes=[cols.r])
                    self.tt("dve", hd[:], hr[:, :, 0:256], bc(rc.unsqueeze(2), [128, H, 256]), ALU.mult,
                            [hr.r, cols.r], [hd.r])
                    if self.ck(6):
                        return
                    for nm, bf in (("d_gt", gt), ("d_cols", cols), ("d_DT", DT), ("d_EB", EB), ("d_WT", WT),
                                   ("d_hr", hr), ("d_GL", GL), ("d_M2", M2), ("d_vtok", vtok), ("d_qd", qd)):
                        self.dump(nm, bf)
                    bend = EB[:, :, last]
                    for kc in range(2):
                        pv, prs = self.pb(4)
                        pc = pv.rearrange("p (h y) -> p h y", y=512)
                        for h_ in range(H):
                            self.mm(pc[:, h_, 0:257], kw[:, 2 * h_ + kc, :], vtok[:, h_, :], [kw.r, vtok.r], prs)
                        cv = CT[:].rearrange("p (h c) e -> p h c e", c=2)[:, :, kc, :]
                        self.tt("dve", cv, cv, bc(bend.unsqueeze(2), [128, H, 257]), ALU.mult, [CT.r, EB.r], [CT.r])
                        self.tt("dve", cv, cv, pc[:, :, 0:257], ALU.add, prs + [CT.r], [CT.r])
                    if self.ck(7):
                        return
                    if d == 0:
                        fw.dma(HF[tsl, :], hd[:].rearrange("p h e -> p (h e)"), reads=[hd.r])
                        continue
                    fw.dma(hf[:].rearrange("p h e -> p (h e)"), HF[tsl, :], writes=[hf.r])
                    fw.dma(oT[:], MO[:, :, tsl], writes=[oT.r])
                    fw.dma(zs[:], MZ[:, :, tsl], writes=[zs.r])
                    self.tt("dve", hd[:], hd[:], hf[:], ALU.add, [hd.r, hf.r], [hd.r])
                    pv, prs = self.pb(2)
                    for c in range(8):
                        self.tr(pv[:, c * 128:(c + 1) * 128], oT[:, c, :], [oT.r], prs)
                    self.tt("dve", hd[:].rearrange("p h e -> p (h e)"), hd[:].rearrange("p h e -> p (h e)"), pv,
                            ALU.mult, prs + [hd.r], [hd.r])
                    self.act(sq[:], hd[:], AF.Square, [hd.r], [sq.r])
                    fw.op("dve", lambda h: h.tensor_reduce(out=den, in_=sq[:], axis=AX.X, op=ALU.add),
                          reads=[sq.r], writes=[cols.r])
                    self.act(den, den, AF.Sqrt, [cols.r], [cols.r], bias=EPS, scale=1.0 / 256)
                    fw.op("dve", lambda h: h.reciprocal(out=rc, in_=den), reads=[cols.r], writes=[cols.r])
                    self.tt("dve", hd[:], hd[:], bc(rc.unsqueeze(2), [128, H, 256]), ALU.mult, [hd.r, cols.r], [hd.r])
                    self.tt("pool", hd[:], hd[:], bc(sm[:, 16:272].unsqueeze(1), [128, H, 256]), ALU.mult,
                            [hd.r, sm.r], [hd.r])
                    pv, prs = self.pb(2)
                    pv3 = pv.rearrange("p (c t) -> p c t", t=128)
                    hd2 = hd[:].rearrange("p h e -> p (h e)")
                    for c in range(8):
                        self.tr(pv3[:, c, :], hd2[:, c * 128:(c + 1) * 128], [hd.r], prs)
                    self.tt("dve", yo[:], pv3, zs[:], ALU.mult, prs + [zs.r], [yo.r])
                    fw.dma(YC[:, :, tsl], yo[:], reads=[yo.r])


    def stage_gdn(self, l):
        cfg, fw = self.cfg, self.fw
        H = 8
        fm = lambda name: self.scr[name].rearrange("(c p) t -> p c t", p=128)
        QKV, ZG, YA = fm("QKV"), fm("ZG"), fm("YA")
        ABs, HF = self.scr["AB"], self.scr["HF"]
        cr = self.consts.r
        B3 = [128, H, 128]
        flat = lambda ap: ap.rearrange("p h y -> p (h y)")
        with ExitStack() as es:
            sm = self.sb(es, "gsm", [128, 160])
            cw = self.sb(es, "cw", [128, 5, 24])
            fw.dma(sm[:], bc(self.inp["gdn_small"][l:l + 1, :], [128, 160]), writes=[sm.r])
            fw.dma(cw[:], self.inp["gdn_conv_c"][:, l], writes=[cw.r])
            self.act(sm[:, 0:16], sm[:, 0:16], AF.Exp, [sm.r], [sm.r])
            self.ts("dve", sm[:, 0:16], sm[:, 0:16], -1.0, ALU.mult, [sm.r], [sm.r])
            buf = self.sb(es, "buf", [128, 24, 132])
            acc = self.sb(es, "acc", [128, 24, 128])
            tmp = self.sb(es, "tmp", [128, 24, 128])
            sq = self.sb(es, "gsq", [128, 16, 128])
            rn = self.sb(es, "grn", [128, 16, 128])
            ktok = self.sb(es, "gktok", B3)
            vtok = self.sb(es, "gvtok", B3)
            abT = self.sb(es, "abT", [32, 128])
            gt = self.sb(es, "ggt", [128, 4, 16])
            cl = self.sb(es, "gcl", [128, 6, H])
            GL = self.sb(es, "gGL", B3)
            GLb = self.sb(es, "gGLb", B3)
            PQ = [self.sb(es, f"gPQ{i}", [128, 2, H, 128]) for i in range(2)]
            TT = self.sb(es, "gTT", B3)
            QKD = self.sb(es, "gQKD", B3)
            kend = self.sb(es, "gkend", B3)
            kbg = self.sb(es, "gkbg", B3)
            vb = self.sb(es, "gvb", B3)
            qdec = self.sb(es, "gqdec", B3)
            nwT = self.sb(es, "gnwT", B3)
            vnew = self.sb(es, "gvnew", B3)
            ob = self.sb(es, "gob", B3)
            hf = self.sb(es, "ghf", B3)
            S = self.sb(es, "gS", B3)
            zs = self.sb(es, "gzs", B3)
            yo = self.sb(es, "gyo", B3)
            M2 = [tmp[:, 0:8, :], tmp[:, 8:16, :], tmp[:, 16:24, :]]
            DTi, Q0D, P0D, EG = sq[:, 0:8, :], sq[:, 8:16, :], rn[:, 0:8, :], rn[:, 8:16, :]
            qT, kT, vT = acc[:, 0:8, :], acc[:, 8:16, :], acc[:, 16:24, :]
            gall, lnball, betall, gtmp = gt[:, 0, :], gt[:, 1, :], gt[:, 2, :], gt[:, 3, :]
            gcc, col2, glast, colE, col3, gend = (cl[:, i, :] for i in range(6))

            def decay(dst, dst_r, lhs_c, GLx, M2x):
                pv, prs = self.pb(2)
                for half in range(2):
                    o = pv[:, half * 512:(half + 1) * 512]
                    self.mm(o, self.cst(lhs_c), flat(GLx[:, 4 * half:4 * half + 4, :]), [GLx_r(GLx), cr], prs,
                            True, M2x is None)
                    if M2x is not None:
                        self.mm(o, self.cst(C_IDENT), flat(M2x[:, 4 * half:4 * half + 4, :]), [tmp.r, cr], prs,
                                False, True)
                self.act(flat(dst), pv, AF.Exp, prs, [dst_r])

            def GLx_r(g):
                return GL.r if g is GLt else GLb.r
            GLt, GLbt = GL[:], GLb[:]

            for d in range(2):
                if d == 0:
                    Cm, NDTi, NDTs, NDs = C_UP, C_NUI, C_NUS, C_NLS
                else:
                    Cm, NDTi, NDTs, NDs = C_LO, C_NLI, C_NLS, C_NUS
                fw.op("pool", lambda h: h.memset(S[:], 0.0), writes=[S.r])
                for tl in self.tile_order(d):
                    t0 = tl * 128
                    tsl = slice(t0, t0 + 128)
                    seg_lo, seg_hi = (0, CTX) if tl < 2 else (CTX, cfg.T)
                    lo, hi = max(t0 - 2, seg_lo), min(t0 + 130, seg_hi)
                    if lo > t0 - 2:
                        fw.op("pool", lambda h: h.memset(buf[:, :, 0:2], 0.0), writes=[buf.r])
                    if hi < t0 + 130:
                        fw.op("pool", lambda h: h.memset(buf[:, :, 130:132], 0.0), writes=[buf.r])
                    for i in range(3):
                        fw.dma(buf[:, 8 * i:8 * i + 8, lo - (t0 - 2):hi - (t0 - 2)], QKV[:, 8 * i:8 * i + 8, lo:hi],
                               writes=[buf.r])
                    fw.dma(abT[:], ABs[:, tsl], writes=[abT.r])
                    for j in range(5):
                        wj = bc(cw[:, j, :].unsqueeze(2), [128, 24, 128])
                        if j == 0:
                            self.tt("dve", acc[:], buf[:, :, 0:128], wj, ALU.mult, [buf.r, cw.r], [acc.r])
                        else:
                            self.tt("pool", tmp[:], buf[:, :, j:j + 128], wj, ALU.mult, [buf.r, cw.r], [tmp.r])
                            self.tt("dve", acc[:], acc[:], tmp[:], ALU.add, [acc.r, tmp.r], [acc.r])
                    self.act(acc[:], acc[:], AF.Silu, [acc.r], [acc.r])
                    self.act(sq[:], acc[:, 0:16, :], AF.Square, [acc.r], [sq.r])
                    pv, prs = self.pb(4)
                    for i in range(4):
                        self.mm(pv[:, i * 512:(i + 1) * 512], self.cst(C_ONES), flat(sq[:, 4 * i:4 * i + 4, :]),
                                [sq.r, cr], prs)
                    self.act(flat(rn[:, 0:8, :]), pv[:, 0:1024], AF.Sqrt, prs, [rn.r], bias=128.0 * EPS, scale=128.0)
                    self.act(flat(rn[:, 8:16, :]), pv[:, 1024:2048], AF.Sqrt, prs, [rn.r], bias=EPS, scale=1.0)
                    fw.op("dve", lambda h: h.reciprocal(out=rn[:], in_=rn[:]), reads=[rn.r], writes=[rn.r])
                    self.tt("dve", acc[:, 0:16, :], acc[:, 0:16, :], rn[:], ALU.mult, [acc.r, rn.r], [acc.r])
                    for src, dstb in ((kT, ktok), (vT, vtok)):
                        pv, prs = self.pb(2)
                        pv3 = pv.rearrange("p (h y) -> p h y", y=128)
                        for h_ in range(H):
                            self.tr(pv3[:, h_, :], src[:, h_, :], [acc.r], prs)
                        self.act(dstb[:], pv3, AF.Copy, prs, [dstb.r])
                    pv, prs = self.pb(1)
                    self.tr(pv[:, 0:32], abT[:], [abT.r], prs)
                    self.act(gall, pv[:, 0:16], AF.Identity, prs, [gt.r])
                    self.act(gtmp, pv[:, 16:32], AF.Exp, prs, [gt.r], scale=-1.0)
                    self.tt("dve", gall, gall, sm[:, 16:32], ALU.add, [gt.r, sm.r], [gt.r])
                    self.act(gall, gall, AF.Exp, [gt.r], [gt.r])
                    self.act(gall, gall, AF.Ln, [gt.r], [gt.r], bias=1.0)
                    self.tt("dve", gall, gall, sm[:, 0:16], ALU.mult, [gt.r, sm.r], [gt.r])
                    self.act(gtmp, gtmp, AF.Ln, [gt.r], [gt.r], bias=1.0)
                    self.ts("dve", lnball, gtmp, -1.0, ALU.mult, [gt.r], [gt.r])
                    self.act(betall, lnball, AF.Exp, [gt.r], [gt.r])
                    g_d, lnb_d, beta_d = gall[:, 8 * d:8 * d + 8], lnball[:, 8 * d:8 * d + 8], betall[:, 8 * d:8 * d + 8]
                    self.tt("dve", GL[:], bc(self.cst(Cm).unsqueeze(1), B3), bc(g_d.unsqueeze(2), B3), ALU.mult,
                            [gt.r, cr], [GL.r])
                    self.tt("pool", GLb[:], bc(self.cst(C_IDENT).unsqueeze(1), B3), bc(lnb_d.unsqueeze(2), B3),
                            ALU.mult, [gt.r, cr], [GLb.r])
                    self.tt("dve", GLb[:], GLb[:], GL[:], ALU.add, [GLb.r, GL.r], [GLb.r])
                    pv, prs = self.pb(1)
                    self.mm(pv[:, 0:H], self.cst(Cm), g_d, [gt.r, cr], prs)
                    self.mm(pv[:, 8:8 + H], self.cst(C_ONES), g_d, [gt.r, cr], prs)
                    self.act(cl[:, 0, :], pv[:, 0:H], AF.Copy, prs, [cl.r])
                    self.act(cl[:, 2, :], pv[:, 8:8 + H], AF.Copy, prs, [cl.r])
                    self.tt("dve", col2, gcc, lnb_d, ALU.add, [cl.r, gt.r], [cl.r])
                    self.tt("dve", colE, glast, gcc, ALU.subtract, [cl.r], [cl.r])
                    self.act(colE, colE, AF.Exp, [cl.r], [cl.r])
                    self.act(col3, col2, AF.Exp, [cl.r], [cl.r])
                    self.act(gend, glast, AF.Exp, [cl.r], [cl.r])
                    self.tt("dve", M2[0], bc(self.cst(NDTi).unsqueeze(1), B3), bc(gcc.unsqueeze(2), B3), ALU.subtract,
                            [cl.r, cr], [tmp.r])
                    self.tt("dve", M2[1], bc(self.cst(NDTs).unsqueeze(1), B3), bc(gcc.unsqueeze(2), B3), ALU.subtract,
                            [cl.r, cr], [tmp.r])
                    self.tt("dve", M2[2], bc(self.cst(NDs).unsqueeze(1), B3), bc(col2.unsqueeze(2), B3), ALU.add,
                            [cl.r, cr], [tmp.r])
                    decay(DTi, sq.r, C_ONES, GLt, M2[0])
                    decay(Q0D, sq.r, C_ONES, GLbt, M2[1])
                    decay(P0D, rn.r, C_NEGONES, GLt, M2[2])
                    decay(EG, rn.r, C_ONES, GLt, None)
                    pv, prs = self.pb(2)
                    pk3 = pv.rearrange("p (h y) -> p h y", y=128)
                    for h_ in range(H):
                        self.mm(pk3[:, h_, :], kT[:, h_, :], kT[:, h_, :], [acc.r], prs)
                    Pc, Pn = PQ[0], PQ[1]
                    self.stt(Pc[:, 0], pk3, -1.0, P0D, ALU.mult, ALU.mult, prs + [rn.r], [Pc.r])
                    self.stt(Pc[:, 1], pk3, -1.0, Q0D, ALU.mult, ALU.mult, prs + [sq.r], [Pc.r])
                    pv, prs = self.pb(2)
                    pq3 = pv.rearrange("p (h y) -> p h y", y=128)
                    for h_ in range(H):
                        self.mm(pq3[:, h_, :], kT[:, h_, :], qT[:, h_, :], [acc.r], prs)
                    self.tt("dve", QKD[:], pq3, DTi, ALU.mult, prs + [sq.r], [QKD.r])
                    self.tt("pool", kend[:], ktok[:], bc(colE.unsqueeze(2), B3), ALU.mult, [ktok.r, cl.r], [kend.r])
                    self.tt("pool", kbg[:], ktok[:], bc(col3.unsqueeze(2), B3), ALU.mult, [ktok.r, cl.r], [kbg.r])
                    self.tt("pool", vb[:], vtok[:], bc(beta_d.unsqueeze(2), B3), ALU.mult, [vtok.r, gt.r], [vb.r])
                    self.tt("dve", qdec[:], qT, EG, ALU.mult, [acc.r, rn.r], [qdec.r])
                    self.tt("dve", TT[:], Pc[:, 1], bc(self.cst(C_IDENT).unsqueeze(1), B3), ALU.add, [Pc.r, cr], [TT.r])
                    for k in range(1, 8):
                        need_q = k <= 5
                        last_it = k == 7
                        if need_q:
                            pv1, prs1 = self.pb(2)
                            p13 = pv1.rearrange("p (h y) -> p h y", y=128)
                            for h_ in range(H):
                                self.mm(p13[:, h_, :], Pc[:, 0, h_, :], Pc[:, 1, h_, :], [Pc.r], prs1)
                        if k >= 2:
                            pv2, prs2 = self.pb(2)
                            p23 = pv2.rearrange("p (h y) -> p h y", y=128)
                            for h_ in range(H):
                                self.mm(p23[:, h_, :], Pc[:, 0, h_, :], TT[:, h_, :], [Pc.r, TT.r], prs2)
                        if not last_it:
                            pv3_, prs3 = self.pb(2)
                            p33 = pv3_.rearrange("p (h y) -> p h y", y=128)
                            for h_ in range(H):
                                self.mm(p33[:, h_, :], Pc[:, 1, h_, :], Pc[:, 0, h_, :], [Pc.r], prs3)
                        if need_q:
                            self.act(Pn[:, 1], p13, AF.Copy, prs1, [Pn.r])
                        if k >= 2:
                            self.tt("dve", TT[:], TT[:], p23, ALU.add, prs2 + [TT.r], [TT.r])
                        if not last_it:
                            self.act(Pn[:, 0], p33, AF.Copy, prs3, [Pn.r])
                        Pc, Pn = Pn, Pc
                    pv, prs = self.pb(2)
                    pw3 = pv.rearrange("p (h y) -> p h y", y=128)
                    for h_ in range(H):
                        self.mm(pw3[:, h_, :], kbg[:, h_, :], TT[:, h_, :], [kbg.r, TT.r], prs)
                    self.act(nwT[:], pw3, AF.Identity, prs, [nwT.r], scale=-1.0)
                    pv, prs = self.pb(2)
                    pn3 = pv.rearrange("p (h y) -> p h y", y=128)
                    for h_ in range(H):
                        self.mm(pn3[:, h_, :], TT[:, h_, :], vb[:, h_, :], [TT.r, vb.r], prs, True, False)
                        self.mm(pn3[:, h_, :], nwT[:, h_, :], S[:, h_, :], [nwT.r, S.r], prs, False, True)
                    self.act(vnew[:], pn3, AF.Copy, prs, [vnew.r])
                    pv, prs = self.pb(2)
                    po3 = pv.rearrange("p (h y) -> p h y", y=128)
                    for h_ in range(H):
                        self.mm(po3[:, h_, :], qdec[:, h_, :], S[:, h_, :], [qdec.r, S.r], prs, True, False)
                        self.mm(po3[:, h_, :], QKD[:, h_, :], vnew[:, h_, :], [QKD.r, vnew.r], prs, False, True)
                    self.act(ob[:], po3, AF.Copy, prs, [ob.r])
                    pv, prs = self.pb(2)
                    ps3 = pv.rearrange("p (h y) -> p h y", y=128)
                    for h_ in range(H):
                        self.mm(ps3[:, h_, :], kend[:, h_, :], vnew[:, h_, :], [kend.r, vnew.r], prs)
                    self.tt("dve", S[:], S[:], bc(gend.unsqueeze(2), B3), ALU.mult, [S.r, cl.r], [S.r])
                    self.tt("dve", S[:], S[:], ps3, ALU.add, prs + [S.r], [S.r])
                    for nm, bf in (("g_acc", acc), ("g_gt", gt), ("g_cl", cl), ("g_sq", sq), ("g_rn", rn), ("g_TT", TT),
                                   ("g_ob", ob), ("g_vnew", vnew), ("g_QKD", QKD)):
                        self.dump(nm, bf)
                    if d == 0:
                        fw.dma(HF[tsl, :], flat(ob[:]), reads=[ob.r])
                        continue
                    fw.dma(flat(hf[:]), HF[tsl, :], writes=[hf.r])
                    fw.dma(zs[:], ZG[:, :, tsl], writes=[zs.r])
                    self.tt("dve", ob[:], ob[:], hf[:], ALU.add, [ob.r, hf.r], [ob.r])
                    self.act(vnew[:], ob[:], AF.Square, [ob.r], [vnew.r])
                    fw.op("dve", lambda h: h.tensor_reduce(out=gcc, in_=vnew[:], axis=AX.X, op=ALU.add),
                          reads=[vnew.r], writes=[cl.r])
                    self.act(gcc, gcc, AF.Sqrt, [cl.r], [cl.r], bias=EPS, scale=1.0 / 128)
                    fw.op("dve", lambda h: h.reciprocal(out=gcc, in_=gcc), reads=[cl.r], writes=[cl.r])
                    self.tt("dve", ob[:], ob[:], bc(gcc.unsqueeze(2), B3), ALU.mult, [ob.r, cl.r], [ob.r])
                    self.tt("pool", ob[:], ob[:], bc(sm[:, 32:160].unsqueeze(1), B3), ALU.mult, [ob.r, sm.r], [ob.r])
                    pv, prs = self.pb(2)
                    pv3 = pv.rearrange("p (h y) -> p h y", y=128)
                    for h_ in range(H):
                        self.tr(pv3[:, h_, :], ob[:, h_, :], [ob.r], prs)
                    self.tt("dve", yo[:], pv3, zs[:], ALU.mult, prs + [zs.r], [yo.r])
                    fw.dma(YA[:, :, tsl], yo[:], reads=[yo.r])


    def stage_s5(self, l):
        cfg, fw = self.cfg, self.fw
        T = cfg.T
        NL = (T - 1).bit_length()
        cr = self.consts.r
        with ExitStack() as es:
            lam = self.sb(es, "lam", [128, 32, 2, 2])
            stp = self.sb(es, "stp", [128, 32, 2])
            Bb = self.sb(es, "Bb", [32, 2, 128])
            Cb = self.sb(es, "Cb", [128, 32, 2, 32])
            dq = self.sb(es, "dq", [32, 32])
            fw.dma(lam[:], self.inp["s5_lam"][:, l], writes=[lam.r])
            fw.dma(stp[:], self.inp["s5_lstep"][:, l], writes=[stp.r])
            fw.dma(Cb[:], self.inp["s5_Cblk"][:, l], writes=[Cb.r])
            fw.dma(dq[:], self.inp["s5_d_q"][:, l], writes=[dq.r])
            self.ts("dve", Cb[:, :, 1, :], Cb[:, :, 1, :], -1.0, ALU.mult, [Cb.r], [Cb.r])
            LAM = self.sb(es, "LAM", [128, NL, 32, 2, 2])
            B2 = self.sb(es, "B2", [128, NL, 32, 2, 2])
            FF = self.sb(es, "FF", [128, 32, 2, 2])
            FS = self.sb(es, "FS", [128, 32, 2, 2])
            w = [self.sb(es, f"s5w{i}", [128, 32, 2]) for i in range(6)]
            S3 = [128, 32, 2]
            lre, lim = lam[:, :, :, 0], lam[:, :, :, 1]
            ar, th, cc, ss, t1, t2 = (x[:] for x in w)
            wr = [x.r for x in w]
            self.act(stp[:], stp[:], AF.Exp, [stp.r], [stp.r])
            self.tt("dve", ar, lre, stp[:], ALU.mult, [lam.r, stp.r], [wr[0]])
            self.tt("dve", th, lim, stp[:], ALU.mult, [lam.r, stp.r], [wr[1]])
            self.act(ar, ar, AF.Exp, [wr[0]], [wr[0]])
            hp = self.sb(es, "halfpi", [128, 1])
            fw.op("dve", lambda h: h.memset(hp[:], float(np.pi / 2)), writes=[hp.r])
            self.act(cc, th, AF.Sin, [wr[1], hp.r], [wr[2]], bias=hp[:, 0:1], scale=1.0 / 16)
            self.act(ss, th, AF.Sin, [wr[1]], [wr[3]], scale=1.0 / 16)

            def csq(a, b, ra, rb):
                self.tt("dve", t1, a, a, ALU.mult, [ra], [wr[4]])
                self.tt("dve", t2, b, b, ALU.mult, [rb], [wr[5]])
                self.stt(b, a, 2.0, b, ALU.mult, ALU.mult, [ra, rb], [rb])
                self.tt("dve", a, t1, t2, ALU.subtract, [wr[4], wr[5]], [ra])
            for _ in range(4):
                csq(cc, ss, wr[2], wr[3])
            self.tt("dve", cc, cc, ar, ALU.mult, [wr[2], wr[0]], [wr[2]])
            self.tt("dve", ss, ss, ar, ALU.mult, [wr[3], wr[0]], [wr[3]])
            self.tt("dve", t1, lre, lre, ALU.mult, [lam.r], [wr[4]])
            self.tt("dve", t2, lim, lim, ALU.mult, [lam.r], [wr[5]])
            self.tt("dve", t1, t1, t2, ALU.add, [wr[4], wr[5]], [wr[4]])
            fw.op("dve", lambda h: h.reciprocal(out=t1, in_=t1), reads=[wr[4]], writes=[wr[4]])
            self.ts("dve", ar, cc, -1.0, ALU.add, [wr[2]], [wr[0]])
            self.tt("dve", t2, ar, lre, ALU.mult, [wr[0], lam.r], [wr[5]])
            self.tt("dve", th, ss, lim, ALU.mult, [wr[3], lam.r], [wr[1]])
            self.tt("dve", t2, t2, th, ALU.add, [wr[5], wr[1]], [wr[5]])
            self.tt("dve", FF[:, :, :, 0], t2, t1, ALU.mult, [wr[5], wr[4]], [FF.r])
            self.tt("dve", t2, ss, lre, ALU.mult, [wr[3], lam.r], [wr[5]])
            self.tt("dve", th, ar, lim, ALU.mult, [wr[0], lam.r], [wr[1]])
            self.tt("dve", t2, t2, th, ALU.subtract, [wr[5], wr[1]], [wr[5]])
            self.tt("dve", FF[:, :, :, 1], t2, t1, ALU.mult, [wr[5], wr[4]], [FF.r])
            self.ts("dve", FS[:, :, :, 0], FF[:, :, :, 1], -1.0, ALU.mult, [FF.r], [FS.r])
            self.act(FS[:, :, :, 1], FF[:, :, :, 1], AF.Copy, [FF.r], [FS.r])
            for k in range(NL):
                self.act(LAM[:, k, :, :, 0], cc, AF.Copy, [wr[2]], [LAM.r])
                self.act(LAM[:, k, :, :, 1], ss, AF.Copy, [wr[3]], [LAM.r])
                self.ts("dve", B2[:, k, :, :, 0], ss, -1.0, ALU.mult, [wr[3]], [B2.r])
                self.act(B2[:, k, :, :, 1], ss, AF.Copy, [wr[3]], [B2.r])
                if k < NL - 1:
                    csq(cc, ss, wr[2], wr[3])
            u32 = self.sb(es, "u32", [32, T])
            XA = self.sb(es, "XA", [128, 2, T])
            XB = self.sb(es, "XB", [128, 2, T])
            TM = self.sb(es, "TM", [128, 2, T])
            Y = self.sb(es, "Y5y", [32, T])
            yt = self.sb(es, "Y5t", [32, 512])
            segs = [(0, CTX), (CTX, T)]

            def rev(ap3, lo, hi, d):
                if d == 0:
                    return ap3[:, :, lo:hi]
                return ap3[:, :, hi - 1:lo - 1:-1] if lo > 0 else ap3[:, :, hi - 1::-1]

            for q in range(32):
                fw.dma(u32[:], self.scr["U5"][32 * q:32 * q + 32, :], writes=[u32.r])
                fw.dma(Bb[:], self.inp["s5_Bblk"][:, l, q], writes=[Bb.r])
                for d in range(2):
                    RAW = XB
                    for c0 in range(0, T, 2048):
                        n = min(2048, T - c0)
                        for ri in range(2):
                            pv, prs = self.pb(4)
                            for s0 in range(0, n, 512):
                                m = min(512, n - s0)
                                self.mm(pv[:, s0:s0 + m], Bb[:, ri, :], u32[:, c0 + s0:c0 + s0 + m], [Bb.r, u32.r], prs)
                            self.act(RAW[:, ri, c0:c0 + n], pv[:, 0:n], AF.Copy, prs, [RAW.r])
                    fre = FF[:, q, d, 0:1]
                    fs2 = FS[:, q, d, :]
                    for lo, hi in segs:
                        n = hi - lo
                        self.ts("dve", XA[:, :, lo:hi], rev(RAW[:], lo, hi, d), fre, ALU.mult, [RAW.r, FF.r], [XA.r])
                        self.tt("pool", TM[:, :, lo:hi], rev(RAW[:, ::-1, :], lo, hi, d),
                                bc(fs2.unsqueeze(2), [128, 2, n]), ALU.mult, [RAW.r, FS.r], [TM.r])
                    self.tt("dve", XA[:], XA[:], TM[:], ALU.add, [XA.r, TM.r], [XA.r])
                    cur, nxt = XA, XB
                    for k in range(NL):
                        sh = 1 << k
                        n = T - sh
                        self.act(nxt[:, :, 0:sh], cur[:, :, 0:sh], AF.Copy, [cur.r], [nxt.r])
                        self.stt(nxt[:, :, sh:T], cur[:, :, 0:n], LAM[:, k, q, d, 0:1], cur[:, :, sh:T], ALU.mult, ALU.add,
                                 [cur.r, LAM.r], [nxt.r])
                        self.tt("pool", TM[:, :, 0:n], cur[:, ::-1, 0:n], bc(B2[:, k, q, d, :].unsqueeze(2), [128, 2, n]),
                                ALU.mult, [cur.r, B2.r], [TM.r])
                        self.tt("dve", nxt[:, :, sh:T], nxt[:, :, sh:T], TM[:, :, 0:n], ALU.add, [nxt.r, TM.r], [nxt.r])
                        cur, nxt = nxt, cur
                    for lo, hi in segs:
                        for g0 in range(lo, hi, 512):
                            m = min(512, hi - g0)
                            pv, prs = self.pb(1)
                            self.mm(pv[0:32, 0:m], Cb[:, q, 0, :], cur[:, 0, g0:g0 + m], [Cb.r, cur.r], prs, True, False)
                            self.mm(pv[0:32, 0:m], Cb[:, q, 1, :], cur[:, 1, g0:g0 + m], [Cb.r, cur.r], prs, False, True)
                            if d == 0:
                                self.act(Y[:, g0:g0 + m], pv[0:32, 0:m], AF.Copy, prs, [Y.r])
                            else:
                                p_hi = hi - (g0 - lo)
                                p_lo = p_hi - m
                                self.act(yt[:, 0:m], pv[0:32, 0:m], AF.Copy, prs, [yt.r])
                                self.tt("dve", Y[:, p_lo:p_hi], Y[:, p_lo:p_hi], yt[:, m - 1::-1] if m > 0 else yt[:, 0:m],
                                        ALU.add, [Y.r, yt.r], [Y.r])
                self.stt(Y[:], u32[:], dq[:, q:q + 1], Y[:], ALU.mult, ALU.add, [u32.r, dq.r, Y.r], [Y.r])
                self.act(Y[:], Y[:], AF.Gelu, [Y.r], [Y.r])
                fw.dma(self.scr["Y5"][32 * q:32 * q + 32, :], Y[:], reads=[Y.r])

    def stage_s5post(self, l):
        cfg, fw = self.cfg, self.fw
        fm = lambda name: self.scr[name].rearrange("(c p) t -> p c t", p=128)
        Y5, Z5, YB = fm("Y5"), fm("Z5"), fm("YB")
        wv = self.inp["s5_w_glu"][l].rearrange("(k p) n -> p k n", p=128)
        with ExitStack() as es:
            bg = self.sb(es, "bglu", [128, 16])
            fw.dma(bg[:], self.inp["b_glu_c"][:, l], writes=[bg.r])
            yg = self.sb(es, "y5g", [128, KC, 512])
            wa = [self.sb(es, f"wa{i}", [128, KC, 128]) for i in range(2)]
            wb = [self.sb(es, f"wb{i}", [128, KC, 128]) for i in range(2)]
            zt = [self.sb(es, f"z5t{i}", [128, 512]) for i in range(2)]
            sg = self.sb(es, "sg5", [128, 512])
            ot = [self.sb(es, f"o5t{i}", [128, 512]) for i in range(2)]
            it = 0
            for (s0, n) in cfg.groups:
                fw.dma(yg[:, :, 0:n], Y5[:, :, s0:s0 + n], writes=[yg.r])
                for ct in range(8):
                    b = it % 2
                    it += 1
                    fw.dma(wa[b][:], wv[:, :, ct * 128:(ct + 1) * 128], writes=[wa[b].r])
                    fw.dma(wb[b][:], wv[:, :, D + ct * 128:D + (ct + 1) * 128], writes=[wb[b].r])
                    fw.dma(zt[b][:, 0:n], Z5[:, ct, s0:s0 + n], writes=[zt[b].r])
                    pa, pra = self.pb(1)
                    for k in range(KC):
                        self.mm(pa[:, 0:n], wa[b][:, k, :], yg[:, k, 0:n], [wa[b].r, yg.r], pra, k == 0, k == KC - 1)
                    pb_, prb = self.pb(1)
                    for k in range(KC):
                        self.mm(pb_[:, 0:n], wb[b][:, k, :], yg[:, k, 0:n], [wb[b].r, yg.r], prb, k == 0, k == KC - 1)
                    self.act(sg[:, 0:n], pb_[:, 0:n], AF.Sigmoid, prb + [bg.r], [sg.r], bias=bg[:, 8 + ct:9 + ct])
                    self.stt(ot[b][:, 0:n], pa[:, 0:n], bg[:, ct:ct + 1], sg[:, 0:n], ALU.add, ALU.mult,
                             pra + [bg.r, sg.r], [ot[b].r])
                    self.tt("dve", ot[b][:, 0:n], ot[b][:, 0:n], zt[b][:, 0:n], ALU.mult, [ot[b].r, zt[b].r], [ot[b].r])
                    fw.dma(YB[:, ct, s0:s0 + n], ot[b][:, 0:n], reads=[ot[b].r])


    def stage_merge(self, l):
        cfg, fw = self.cfg, self.fw
        T = cfg.T
        last = (l == cfg.DEPTH - 1)
        fm = lambda name: self.scr[name].rearrange("(c p) t -> p c t", p=128)
        YS = [fm("YA"), fm("YB"), fm("YC")]
        GT = self.scr["GATE"].rearrange("(i c p) t -> p i c t", i=3, p=128)
        OUTT = fm("OUTT")
        with ExitStack() as es:
            wo = self.sb(es, "wo", [128, KC, D])
            fw.dma(wo[:], self.inp["w_out"][l].rearrange("(k p) n -> p k n", p=128), writes=[wo.r])
            ys = self.sb(es, "ys", [128, 3, KC, 512])
            mg_ = self.sb(es, "mg", [128, KC, 512])
            og = self.sb(es, "og", [128, KC, 512])
            wt = [[self.sb(es, f"wbr{b}{i}", [128, KC, 128]) for i in range(3)] for b in range(2)]
            sgt = [self.sb(es, f"sgt{b}", [128, 3, 512]) for b in range(2)]
            tm = self.sb(es, "mtm", [128, 512])
            it = 0
            for gi, (s0, n) in enumerate(cfg.groups):
                if last and gi == 0:
                    continue
                for i in range(3):
                    fw.dma(ys[:, i, :, 0:n], YS[i][:, :, s0:s0 + n], writes=[ys.r])
                for dc in range(KC):
                    b = it % 2
                    it += 1
                    for i in range(3):
                        fw.dma(wt[b][i][:], self.inp["w_branch"][l, i].rearrange("(k p) n -> p k n", p=128)
                               [:, :, dc * 128:(dc + 1) * 128], writes=[wt[b][i].r])
                    fw.dma(sgt[b][:, :, 0:n], GT[:, :, dc, s0:s0 + n], writes=[sgt[b].r])
                    pp = []
                    for i in range(3):
                        pv, prs = self.pb(1)
                        for k in range(KC):
                            self.mm(pv[:, 0:n], wt[b][i][:, k, :], ys[:, i, k, 0:n], [wt[b][i].r, ys.r], prs,
                                    k == 0, k == KC - 1)
                        pp.append((pv, prs))
                    self.tt("dve", mg_[:, dc, 0:n], pp[0][0][:, 0:n], sgt[b][:, 0, 0:n], ALU.mult,
                            pp[0][1] + [sgt[b].r], [mg_.r])
                    for i in (1, 2):
                        self.tt("dve", tm[:, 0:n], pp[i][0][:, 0:n], sgt[b][:, i, 0:n], ALU.mult,
                                pp[i][1] + [sgt[b].r], [tm.r])
                        self.tt("dve", mg_[:, dc, 0:n], mg_[:, dc, 0:n], tm[:, 0:n], ALU.add, [mg_.r, tm.r], [mg_.r])
                for dc in range(KC):
                    pv, prs = self.pb(1)
                    for k in range(KC):
                        self.mm(pv[:, 0:n], wo[:, k, dc * 128:(dc + 1) * 128], mg_[:, k, 0:n], [wo.r, mg_.r], prs,
                                k == 0, k == KC - 1)
                    self.act(og[:, dc, 0:n], pv[:, 0:n], AF.Copy, prs, [og.r])
                fw.dma(OUTT[:, :, s0:s0 + n], og[:, :, 0:n], reads=[og.r])
        fw.barrier()
        with ExitStack() as es:
            xt = [self.sb(es, f"rxt{i}", [128, T]) for i in range(2)]
            ot = [self.sb(es, f"rot{i}", [128, T]) for i in range(2)]
            for dc in range(KC):
                b = dc % 2
                fw.dma(xt[b][:], self.scr["XT"][dc], writes=[xt[b].r])
                fw.dma(ot[b][:], OUTT[:, dc, :], writes=[ot[b].r])
                rd = [xt[b].r, ot[b].r, self.mod.r]
                if not last:
                    self.stt(xt[b][:, 0:CTX], ot[b][:, 0:CTX], self.mod[:, l, 16 + dc, 1:2], xt[b][:, 0:CTX],
                             ALU.mult, ALU.add, rd, [xt[b].r])
                if l % 2 == 0:
                    xv, ov = xt[b][:, CTX:], ot[b][:, CTX:]
                else:
                    xv = xt[b][:, CTX:].rearrange("p (r c) -> p c r", c=cfg.GW)
                    ov = ot[b][:, CTX:].rearrange("p (c r) -> p c r", r=cfg.ROWS)
                self.stt(xv, ov, self.mod[:, l, 16 + dc, 0:1], xv, ALU.mult, ALU.add, rd, [xt[b].r])
                fw.dma(self.scr["XT"][dc], xt[b][:], reads=[xt[b].r])

    def stage_final(self):
        cfg, fw = self.cfg, self.fw
        XTv = self.scr["XT"].rearrange("k p t -> p k t")
        with ExitStack() as es:
            gb = self.sb(es, "gb", [128, D])
            fw.dma(gb[:], bc(self.inp["final_norm_g"][0:1, :], [128, D]), writes=[gb.r])
            xf = [self.sb(es, f"xf{i}", [128, KC, 128]) for i in range(2)]
            sq = [self.sb(es, f"fsq{i}", [128, D]) for i in range(2)]
            st = [self.sb(es, f"fst{i}", [128, 2]) for i in range(2)]
            for tl in range(2, cfg.NT):
                b = tl % 2
                fw.dma(xf[b][:], XTv[:, :, tl * 128:(tl + 1) * 128], writes=[xf[b].r])
                pv, prs = self.pb(2)
                for k in range(KC):
                    self.tr(pv[:, k * 128:(k + 1) * 128], xf[b][:, k, :], [xf[b].r], prs)
                self.act(sq[b][:], pv, AF.Square, prs, [sq[b].r])
                self.fw.op("dve", lambda h, b=b: h.tensor_reduce(out=st[b][:, 0:1], in_=sq[b][:], axis=AX.X, op=ALU.add),
                           reads=[sq[b].r], writes=[st[b].r])
                self.act(st[b][:, 1:2], st[b][:, 0:1], AF.Sqrt, [st[b].r], [st[b].r], bias=EPS, scale=1.0 / D)
                self.fw.op("dve", lambda h, b=b: h.reciprocal(out=st[b][:, 0:1], in_=st[b][:, 1:2]),
                           reads=[st[b].r], writes=[st[b].r])
                self.stt(sq[b][:], pv, st[b][:, 0:1], gb[:], ALU.mult, ALU.mult, prs + [st[b].r, gb.r], [sq[b].r])
                fw.dma(self.out[(tl - 2) * 128:(tl - 1) * 128, :], sq[b][:], reads=[sq[b].r])


def colfmt(v, nk):
    v = np.asarray(v, np.float32)
    lead = v.shape[:-1]
    return np.ascontiguousarray(np.moveaxis(v.reshape(lead + (nk, 128)), -1, 0))


def host_inputs(inputs, cfg, b):
    Dp = cfg.DEPTH
    m = {}
    f32 = lambda a: np.asarray(a, np.float32)
    m["x"] = np.ascontiguousarray(inputs["x"][b], dtype=np.float32)
    m["ctx"] = np.ascontiguousarray(inputs["ctx"][b], dtype=np.float32)
    cv = np.stack([inputs["c"][b], inputs["c_ctx"]], axis=0)
    m["cvec"] = np.ascontiguousarray(np.transpose(colfmt(cv, KC), (0, 2, 1)))
    m["w_ada"] = np.ascontiguousarray(inputs["w_ada"][:Dp], dtype=np.float32)
    m["b_ada_c"] = colfmt(inputs["b_ada"][:Dp], 24)
    m["norm_g_c"] = colfmt(inputs["norm_g"][:Dp], KC)
    m["w_in"] = np.ascontiguousarray(inputs["w_in"][:Dp], dtype=np.float32)
    m["consts"] = make_consts()
    m["final_norm_g"] = np.asarray(inputs["final_norm_g"], np.float32).reshape(1, D)
    m["w_branch"] = np.ascontiguousarray(f32(inputs["w_branch"][:Dp]))
    m["w_out"] = np.ascontiguousarray(f32(inputs["w_out"][:Dp]))
    G, Pn, Hh = 64, 64, 16
    lam = np.stack([f32(inputs["s5_lam_re"][:Dp]), f32(inputs["s5_lam_im"][:Dp])], axis=-1)
    lam = lam.reshape(Dp, 2, 32, 2, Pn, 2)
    m["s5_lam"] = np.ascontiguousarray(np.transpose(lam, (3, 4, 0, 2, 1, 5)).reshape(128, Dp, 32, 2, 2))
    ls = f32(inputs["s5_log_step"][:Dp]).reshape(Dp, 2, 32, 2)
    ls = np.broadcast_to(ls[..., None], (Dp, 2, 32, 2, Pn))
    m["s5_lstep"] = np.ascontiguousarray(np.transpose(ls, (3, 4, 0, 2, 1)).reshape(128, Dp, 32, 2))
    Bblk = np.zeros((2, Hh, Dp, 32, 2, 2, Pn), np.float32)
    Cblk = np.zeros((2, Pn, Dp, 32, 2, 2, Hh), np.float32)
    for ri, (bn, cn) in enumerate((("s5_b_re", "s5_c_re"), ("s5_b_im", "s5_c_im"))):
        Bm = f32(inputs[bn][:Dp]).reshape(Dp, 32, 2, Pn, Hh)
        Cm = f32(inputs[cn][:Dp]).reshape(Dp, 32, 2, Hh, Pn)
        for gl in range(2):
            Bblk[gl, :, :, :, ri, gl, :] = np.transpose(Bm[:, :, gl], (3, 0, 1, 2))
            Cblk[gl, :, :, :, ri, gl, :] = np.transpose(Cm[:, :, gl], (3, 0, 1, 2))
    m["s5_Bblk"] = np.ascontiguousarray(Bblk.reshape(32, Dp, 32, 2, 128))
    m["s5_Cblk"] = np.ascontiguousarray(Cblk.reshape(128, Dp, 32, 2, 32))
    m["s5_d_q"] = np.ascontiguousarray(np.transpose(f32(inputs["s5_d"][:Dp]).reshape(Dp, 32, 32), (2, 0, 1)))
    m["b_glu_c"] = colfmt(inputs["s5_b_glu"][:Dp], 16)
    m["s5_w_glu"] = np.ascontiguousarray(f32(inputs["s5_w_glu"][:Dp]))
    m["gdn_small"] = np.ascontiguousarray(np.concatenate(
        [f32(inputs["gdn_a_log"][:Dp]).reshape(Dp, 16), f32(inputs["gdn_dt_bias"][:Dp]).reshape(Dp, 16),
         f32(inputs["gdn_norm_g"][:Dp]).reshape(Dp, 128)], axis=1))
    m["gdn_conv_c"] = colfmt(inputs["gdn_conv"][:Dp], 24)
    m["ml_small"] = np.ascontiguousarray(np.concatenate(
        [f32(inputs["ml_i_bias"][:Dp]).reshape(Dp, 8), f32(inputs["ml_f_bias"][:Dp]).reshape(Dp, 8),
         f32(inputs["ml_norm_g"][:Dp]).reshape(Dp, 256)], axis=1))
    return m


def run(inputs, cfg, cores, dbg=(), stages=None):
    kb = KB(cfg, dbg=dbg, stages=stages)
    nc = kb.build()
    in_maps = [host_inputs(inputs, cfg, b) for b in cores]
    res = run_bass_kernel_spmd(nc, in_maps, core_ids=list(range(len(cores))))
    return kb, res


def kernel(**inputs):
    cfg = Cfg()
    kb, res = run(inputs, cfg, list(range(N_CORES)))
    return np.stack([np.asarray(r["y"], dtype=np.float32) for r in res.results], axis=0)
```

```python
from contextlib import ExitStack
import os
import numpy as np
import concourse.bass as bass
import concourse.mybir as mybir
from concourse.bass_utils import run_bass_kernel_spmd

F32 = mybir.dt.float32
F32R = mybir.dt.float32r
FAST_DENSE = False
AF = mybir.ActivationFunctionType
ALU = mybir.AluOpType
AX = mybir.AxisListType

D = 1024
KC = 8
CTX = 256
EPS = 1e-6
N_CORES = 8
NEG = -30000.0

SEGS = [("QKV", 0, 3072, "copy"), ("ZG", 3072, 1024, "silu"), ("AB", 4096, 32, "copy"),
        ("U5", 4128, 1024, "copy"), ("Z5", 5152, 1024, "silu"), ("MQ", 6176, 1024, "copy"),
        ("MK", 7200, 1024, "copy"), ("MV", 8224, 1024, "copy"), ("MO", 9248, 1024, "sigmoid"),
        ("MZ", 10272, 1024, "silu"), ("IF", 11296, 16, "copy"), ("GATE", 11312, 3072, "sigmoid")]

C_IDENT, C_ONES, C_NEGONES, C_UP, C_LO, C_NUI, C_NUS, C_NLI, C_NLS = range(9)


def make_consts():
    x = np.arange(128)[:, None]
    y = np.arange(128)[None, :]
    c = np.zeros((128, 9, 128), np.float32)
    c[:, C_IDENT] = (x == y)
    c[:, C_ONES] = 1.0
    c[:, C_NEGONES] = -1.0
    c[:, C_UP] = (x <= y)
    c[:, C_LO] = (x >= y)
    c[:, C_NUI] = np.where(x <= y, 0.0, NEG)
    c[:, C_NUS] = np.where(x < y, 0.0, NEG)
    c[:, C_NLI] = np.where(x >= y, 0.0, NEG)
    c[:, C_NLS] = np.where(x > y, 0.0, NEG)
    return c


class Cfg:
    def __init__(self, L=4096, GW=64, DEPTH=4):
        self.L, self.GW, self.DEPTH = L, GW, DEPTH
        self.ROWS = L // GW
        self.T = CTX + L
        self.NT = self.T // 128
        self.groups = [(0, 256)] + [(CTX + 512 * i, 512) for i in range(L // 512)]


class Res:
    __slots__ = ("name", "lw", "rd")

    def __init__(self, name=""):
        self.name = name
        self.lw = None
        self.rd = []


class Buf:
    def __init__(self, t, name):
        self.t = t
        self.r = Res(name)

    def __getitem__(self, k):
        return self.t[k]


class FW:
    ENG = ("pe", "act", "dve", "pool", "sp")

    def __init__(self, nc, n_dma_slots=16):
        self.nc = nc
        self.sem, self.cnt = {}, {}
        self.prog = {e: [] for e in self.ENG}
        self.waited = {e: {} for e in self.ENG}
        for e in self.ENG:
            self.sem[e] = nc.alloc_semaphore(name=f"s_{e}")
            self.cnt[e] = 0
        self.slots = []
        for i in range(n_dma_slots):
            k = f"dma{i}"
            self.sem[k] = nc.alloc_semaphore(name=f"s_{k}")
            self.cnt[k] = 0
            self.slots.append(k)
        self.slot_rr = 0
        self.n_ops = 0

    def _wait(self, eng, k, v):
        wl = self.waited[eng]
        if (k != eng or eng != "pe") and wl.get(k, 0) < v:
            wl[k] = v
            self.prog[eng].append(lambda h, sem=self.sem[k], v=v: h.wait_ge(sem, v))

    def _deps(self, eng, reads, writes):
        deps = {}

        def add(ts):
            if ts is not None and deps.get(ts[0], 0) < ts[1]:
                deps[ts[0]] = ts[1]
        for r in reads:
            add(r.lw)
        for w in writes:
            add(w.lw)
            for t in w.rd:
                add(t)
        for k, v in deps.items():
            self._wait(eng, k, v)

    def _mark(self, ts, reads, writes):
        for r in reads:
            r.rd.append(ts)
            if len(r.rd) > 64:
                best = {}
                for k, v in r.rd:
                    if best.get(k, 0) < v:
                        best[k] = v
                r.rd = list(best.items())
        for w in writes:
            w.lw = ts
            w.rd = []

    def op(self, eng, fn, reads=(), writes=()):
        self._deps(eng, reads, writes)
        self.cnt[eng] += 1
        self.prog[eng].append(lambda h, fn=fn, sem=self.sem[eng]: fn(h).then_inc(sem, 1))
        self._mark((eng, self.cnt[eng]), reads, writes)
        self.n_ops += 1

    def dma(self, out, in_, reads=(), writes=(), q="sp"):
        self._deps(q, reads, writes)
        k = self.slots[self.slot_rr]
        self.slot_rr = (self.slot_rr + 1) % len(self.slots)
        self._wait(q, k, self.cnt[k])
        self.cnt[k] += 16
        self.prog[q].append(
            lambda h, sem=self.sem[k], out=out, in_=in_: h.dma_start(out=out, in_=in_).then_inc(sem, 16))
        self._mark((k, self.cnt[k]), reads, writes)
        self.n_ops += 1

    def barrier(self, engines=None):
        for e in (engines or self.ENG):
            for k in self.sem:
                self._wait(e, k, self.cnt[k])

    def emit(self):
        self.barrier(["sp"])
        with self.nc.Block() as block:
            @block.tensor
            def _(h):
                for f in self.prog["pe"]:
                    f(h)

            @block.scalar
            def _(h):
                for f in self.prog["act"]:
                    f(h)

            @block.vector
            def _(h):
                for f in self.prog["dve"]:
                    f(h)

            @block.gpsimd
            def _(h):
                for f in self.prog["pool"]:
                    f(h)

            @block.sync
            def _(h):
                for f in self.prog["sp"]:
                    f(h)


def bc(ap, shape):
    return ap.to_broadcast(list(shape))


class KB:
    def __init__(self, cfg, dbg=(), stages=None):
        self.cfg = cfg
        self.dbg = set(dbg)
        self.stages = stages
        self.nc = nc = bass.Bass("TRN2", target_bir_lowering=False)
        self.fw = FW(nc)
        self.inp = {}
        self.scr = {}
        self.P = nc.alloc_psum_tensor("P", [128, 4096], F32)
        self.pr = [Res(f"bank{i}") for i in range(8)]
        self.pptr = 0
        self.uid = 0
        self.stop_at = float(os.environ.get('KSTOP', '999'))

    def din(self, name, shape):
        self.inp[name] = self.nc.dram_tensor(name, list(shape), F32, kind="ExternalInput").ap()
        return self.inp[name]

    def dscr(self, name, shape):
        kind = "ExternalOutput" if name in self.dbg else "Internal"
        self.scr[name] = self.nc.dram_tensor(name, list(shape), F32, kind=kind).ap()
        return self.scr[name]

    def sb(self, es, name, shape):
        self.uid += 1
        t = es.enter_context(self.nc.sbuf_tensor(f"{name}_{self.uid}", list(shape), F32))
        return Buf(t, name)

    def pb(self, n=1):
        if self.pptr % n:
            self.pptr += n - self.pptr % n
        if self.pptr + n > 8:
            self.pptr = 0
        b0 = self.pptr
        self.pptr += n
        return self.P[:, b0 * 512:(b0 + n) * 512], self.pr[b0:b0 + n]

    def cst(self, i):
        return self.consts[:, i, :]

    def mm(self, out, lhsT, rhs, reads, writes, start=True, stop=True, fast=False):
        if fast and FAST_DENSE:
            lhsT, rhs = lhsT.bitcast(F32R), rhs.bitcast(F32R)
        self.fw.op("pe", lambda h: h.matmul(out, lhsT=lhsT, rhs=rhs, start=start, stop=stop),
                   reads=reads, writes=writes)

    def tr(self, out, in_, reads, writes):
        n = in_.shape[0]
        ident = self.consts[0:n, C_IDENT, 0:n]
        self.fw.op("pe", lambda h: h.transpose(out=out, in_=in_, identity=ident),
                   reads=list(reads) + [self.consts.r], writes=writes)

    def act(self, out, in_, func, reads, writes, bias=None, scale=None):
        kw = {}
        if bias is not None:
            kw["bias"] = bias
        if scale is not None:
            kw["scale"] = scale
        self.fw.op("act", lambda h: h.activation(out=out, in_=in_, func=func, **kw), reads=reads, writes=writes)

    def tt(self, eng, out, in0, in1, op, reads, writes):
        self.fw.op(eng, lambda h: h.tensor_tensor(out=out, in0=in0, in1=in1, op=op), reads=reads, writes=writes)

    def ts(self, eng, out, in0, s1, op0, reads, writes, s2=None, op1=None):
        if op1 is None:
            self.fw.op(eng, lambda h: h.tensor_scalar(out=out, in0=in0, scalar1=s1, scalar2=None, op0=op0),
                       reads=reads, writes=writes)
        else:
            self.fw.op(eng, lambda h: h.tensor_scalar(out=out, in0=in0, scalar1=s1, scalar2=s2, op0=op0, op1=op1),
                       reads=reads, writes=writes)

    def stt(self, out, in0, scalar, in1, op0, op1, reads, writes):
        self.fw.op("dve", lambda h: h.scalar_tensor_tensor(out=out, in0=in0, scalar=scalar, in1=in1,
                                                           op0=op0, op1=op1), reads=reads, writes=writes)

    def dump(self, name, buf, ap=None):
        if name not in self.dbg or name in self.scr:
            return
        ap = buf[:] if ap is None else ap
        t = self.nc.dram_tensor(name, list(ap.shape), F32, kind="ExternalOutput").ap()
        self.scr[name] = t
        self.fw.dma(t, ap, reads=[buf.r])

    def ck(self, n):
        return n >= self.stop_at

    def on(self, name):
        return self.stages is None or name in self.stages

    def build(self):
        cfg, nc, fw = self.cfg, self.nc, self.fw
        T, L, DEPTH = cfg.T, cfg.L, cfg.DEPTH
        self.din("x", [L, D])
        self.din("ctx", [CTX, D])
        self.din("cvec", [128, KC, 2])
        self.din("w_ada", [DEPTH, D, 3 * D])
        self.din("b_ada_c", [128, DEPTH, 24])
        self.din("norm_g_c", [128, DEPTH, KC])
        self.din("w_in", [DEPTH, D, 14384])
        self.din("consts", [128, 9, 128])
        self.din("final_norm_g", [1, D])
        self.din("ml_small", [DEPTH, 272])
        self.din("gdn_small", [DEPTH, 160])
        self.din("gdn_conv_c", [128, DEPTH, 5, 24])
        self.din("s5_lam", [128, DEPTH, 32, 2, 2])
        self.din("s5_lstep", [128, DEPTH, 32, 2])
        self.din("s5_Bblk", [32, DEPTH, 32, 2, 128])
        self.din("s5_Cblk", [128, DEPTH, 32, 2, 32])
        self.din("s5_d_q", [32, DEPTH, 32])
        self.din("b_glu_c", [128, DEPTH, 16])
        self.din("s5_w_glu", [DEPTH, D, 2 * D])
        self.din("w_branch", [DEPTH, 3, D, D])
        self.din("w_out", [DEPTH, D, D])
        self.out = nc.dram_tensor("y", [L, D], F32, kind="ExternalOutput").ap()
        self.dscr("XT", [KC, 128, T])
        for name, c0, n, f in SEGS:
            self.dscr(name, [n, T])
        for name in ("YA", "YB", "YC", "OUTT"):
            self.dscr(name, [D, T])
        self.dscr("HF", [T, D])
        self.dscr("Y5", [D, T])

        with ExitStack() as es:
            self.consts = self.sb(es, "consts", [128, 9, 128])
            fw.dma(self.consts[:], self.inp["consts"], writes=[self.consts.r])
            self.mod = self.sb(es, "mod", [128, DEPTH, 24, 2])
            self.gs = self.sb(es, "gs", [128, DEPTH, KC, 2])
            if self.on("xin"):
                self.stage_xin()
                fw.barrier()
            if self.on("mods"):
                self.stage_mods()
                fw.barrier()
            for l in range(DEPTH):
                with ExitStack() as esl:
                    if self.on("norm"):
                        hT = self.sb(esl, "hT", [128, KC, T])
                        self.stage_norm(l, hT)
                        fw.barrier()
                        if self.on("inproj"):
                            self.stage_inproj(l, hT)
                            fw.barrier()
                if self.on("mlstm"):
                    self.stage_mlstm(l)
                    fw.barrier()
                if self.on("gdn"):
                    self.stage_gdn(l)
                    fw.barrier()
                if self.on("s5"):
                    self.stage_s5(l)
                    fw.barrier()
                    self.stage_s5post(l)
                    fw.barrier()
                if self.on("merge"):
                    self.stage_merge(l)
                    fw.barrier()
            if self.on("final"):
                self.stage_final()
        fw.emit()
        return nc

    def stage_xin(self):
        cfg, fw = self.cfg, self.fw
        XTv = self.scr["XT"].rearrange("k p t -> p k t")
        with ExitStack() as es:
            xt = [self.sb(es, f"xin{i}", [128, D]) for i in range(2)]
            xo = [self.sb(es, f"xo{i}", [128, KC, 128]) for i in range(2)]
            for tl in range(cfg.NT):
                b = tl % 2
                src = (self.inp["ctx"][tl * 128:(tl + 1) * 128, :] if tl < 2
                       else self.inp["x"][(tl - 2) * 128:(tl - 1) * 128, :])
                fw.dma(xt[b][:], src, writes=[xt[b].r])
                pv, prs = self.pb(2)
                pv3 = pv.rearrange("p (k t) -> p k t", t=128)
                for k in range(KC):
                    self.tr(pv3[:, k, :], xt[b][:, k * 128:(k + 1) * 128], [xt[b].r], prs)
                self.act(xo[b][:], pv3, AF.Copy, prs, [xo[b].r])
                fw.dma(XTv[:, :, tl * 128:(tl + 1) * 128], xo[b][:], reads=[xo[b].r])

    def stage_mods(self):
        cfg, fw = self.cfg, self.fw
        with ExitStack() as es:
            sc = self.sb(es, "sc", [128, KC, 2])
            bada = self.sb(es, "bada", [128, cfg.DEPTH, 24])
            ng = self.sb(es, "ng", [128, cfg.DEPTH, KC])
            wt = [self.sb(es, f"wada{i}", [128, KC, 512]) for i in range(2)]
            fw.dma(sc[:], self.inp["cvec"], writes=[sc.r])
            fw.dma(bada[:], self.inp["b_ada_c"], writes=[bada.r])
            fw.dma(ng[:], self.inp["norm_g_c"], writes=[ng.r])
            self.act(sc[:], sc[:], AF.Silu, [sc.r], [sc.r])
            it = 0
            for l in range(cfg.DEPTH):
                pv, prs = self.pb(1)
                pm = pv[:, 0:48].rearrange("p (c v) -> p c v", v=2)
                wv = self.inp["w_ada"][l].rearrange("(k p) n -> p k n", p=128)
                for cg in range(6):
                    b = it % 2
                    it += 1
                    fw.dma(wt[b][:], wv[:, :, cg * 512:(cg + 1) * 512], writes=[wt[b].r])
                    for j in range(4):
                        for k in range(KC):
                            self.mm(pm[:, cg * 4 + j, :], wt[b][:, k, j * 128:(j + 1) * 128], sc[:, k, :],
                                    [wt[b].r, sc.r], prs, start=(k == 0), stop=(k == KC - 1))
                self.tt("dve", self.mod[:, l], pm, bc(bada[:, l].unsqueeze(2), [128, 24, 2]), ALU.add,
                        prs + [bada.r], [self.mod.r])
                self.ts("dve", self.gs[:, l], self.mod[:, l, 8:16, :], 1.0, ALU.add, [self.mod.r], [self.gs.r])
                self.tt("dve", self.gs[:, l], self.gs[:, l], bc(ng[:, l].unsqueeze(2), [128, KC, 2]), ALU.mult,
                        [self.gs.r, ng.r], [self.gs.r])

    def stage_norm(self, l, hT):
        cfg, fw = self.cfg, self.fw
        XTv = self.scr["XT"].rearrange("k p t -> p k t")
        NG = 256
        with ExitStack() as es:
            xg = [self.sb(es, f"xg{i}", [128, KC, NG]) for i in range(2)]
            sq = self.sb(es, "sq", [128, KC, NG])
            rs = self.sb(es, "rs", [128, NG])
            for gi, s0 in enumerate(range(0, cfg.T, NG)):
                v = 1 if s0 < CTX else 0
                b = gi % 2
                fw.dma(xg[b][:], XTv[:, :, s0:s0 + NG], writes=[xg[b].r])
                self.act(sq[:], xg[b][:], AF.Square, [xg[b].r], [sq.r])
                pv, prs = self.pb(1)
                for k in range(KC):
                    self.mm(pv[:, 0:NG], self.cst(C_ONES), sq[:, k, :], [sq.r, self.consts.r], prs,
                            start=(k == 0), stop=(k == KC - 1))
                self.act(rs[:], pv[:, 0:NG], AF.Sqrt, prs, [rs.r], bias=EPS, scale=1.0 / D)
                self.fw.op("dve", lambda h: h.reciprocal(out=rs[:], in_=rs[:]), reads=[rs.r], writes=[rs.r])
                self.tt("dve", xg[b][:], xg[b][:], bc(rs[:].unsqueeze(1), [128, KC, NG]), ALU.mult,
                        [xg[b].r, rs.r], [xg[b].r])
                for k in range(KC):
                    self.act(hT[:, k, s0:s0 + NG], xg[b][:, k, :], AF.Identity, [xg[b].r, self.gs.r, self.mod.r],
                             [hT.r], bias=self.mod[:, l, k, v:v + 1], scale=self.gs[:, l, k, v:v + 1])

    def hview(self, hT, l, k, gi):
        cfg = self.cfg
        s0, n = cfg.groups[gi]
        if gi == 0 or l % 2 == 0:
            return hT[:, k, s0:s0 + n]
        c0 = (s0 - CTX) // cfg.ROWS
        c1 = c0 + n // cfg.ROWS
        return hT[:, k, CTX:].rearrange("p (r c) -> p c r", c=cfg.GW)[:, c0:c1, :]

    def pview(self, ap2d, l, gi):
        cfg = self.cfg
        if gi == 0 or l % 2 == 0:
            return ap2d
        return ap2d.rearrange("p (c r) -> p c r", r=cfg.ROWS)

    def stage_inproj(self, l, hT):
        cfg, fw = self.cfg, self.fw
        wv = self.inp["w_in"][l].rearrange("(k p) n -> p k n", p=128)
        funcs = {"copy": AF.Copy, "silu": AF.Silu, "sigmoid": AF.Sigmoid}
        with ExitStack() as es:
            wt = [self.sb(es, f"win{i}", [128, KC, 128]) for i in range(2)]
            stg = [self.sb(es, f"stg{i}", [128, cfg.T]) for i in range(2)]
            it = 0
            for name, col0, ncols, fname in SEGS:
                for c0 in range(0, ncols, 128):
                    nn = min(128, ncols - c0)
                    b = it % 2
                    it += 1
                    fw.dma(wt[b][:, :, 0:nn], wv[:, :, col0 + c0:col0 + c0 + nn], writes=[wt[b].r])
                    for gi, (s0, n) in enumerate(cfg.groups):
                        pv, prs = self.pb(1)
                        for k in range(KC):
                            self.mm(self.pview(pv[0:nn, 0:n], l, gi), wt[b][:, k, 0:nn], self.hview(hT, l, k, gi),
                                    [wt[b].r, hT.r], prs, start=(k == 0), stop=(k == KC - 1), fast=(nn == 128))
                        self.act(stg[b][0:nn, s0:s0 + n], pv[0:nn, 0:n], funcs[fname], prs, [stg[b].r])
                    fw.dma(self.scr[name][c0:c0 + nn, :], stg[b][0:nn, :], reads=[stg[b].r])


    def tile_order(self, d):
        NT = self.cfg.NT
        return [0, 1] + list(range(2, NT)) if d == 0 else [1, 0] + list(range(NT - 1, 1, -1))

    def stage_mlstm(self, l):
        cfg, fw = self.cfg, self.fw
        H = 4
        fm = lambda name: self.scr[name].rearrange("(c p) t -> p c t", p=128)
        MQ, MK, MV, MO, MZ, YC = fm("MQ"), fm("MK"), fm("MV"), fm("MO"), fm("MZ"), fm("YC")
        IFs, HF = self.scr["IF"], self.scr["HF"]
        cr = self.consts.r
        with ExitStack() as es:
            sm = self.sb(es, "mlsm", [128, 272])
            fw.dma(sm[:], bc(self.inp["ml_small"][l:l + 1, :], [128, 272]), writes=[sm.r])
            qT = self.sb(es, "qT", [128, 8, 128])
            kT = self.sb(es, "kT", [128, 8, 128])
            vT = self.sb(es, "vT", [128, 8, 128])
            ktok = self.sb(es, "ktok", [128, 8, 128])
            vtok = self.sb(es, "vtok", [128, H, 257])
            ifT = self.sb(es, "ifT", [16, 128])
            ift = self.sb(es, "ift", [128, 16])
            gt = self.sb(es, "gt", [128, 24])
            CT = self.sb(es, "CT", [128, 8, 257])
            GL = self.sb(es, "GL", [128, H, 128])
            M2 = self.sb(es, "M2", [128, H, 128])
            DT = self.sb(es, "DT", [128, H, 128])
            EB = self.sb(es, "EB", [128, H, 128])
            WT = self.sb(es, "WT", [128, H, 128])
            qd = self.sb(es, "qd", [128, 8, 128])
            kw = self.sb(es, "kw", [128, 8, 128])
            cols = self.sb(es, "cols", [128, 4, H])
            hd = self.sb(es, "hd", [128, H, 256])
            hr = self.sb(es, "hr", [128, H, 257])
            hf = self.sb(es, "hf", [128, H, 256])
            sq = self.sb(es, "msq", [128, H, 256])
            zs = self.sb(es, "zs", [128, 8, 128])
            oT = self.sb(es, "oT", [128, 8, 128])
            yo = self.sb(es, "yo", [128, 8, 128])
            fw.op("pool", lambda h: h.memset(vtok[:], 1.0), writes=[vtok.r])
            for d in range(2):
                Cm, NM, last = (C_UP, C_NUI, 127) if d == 0 else (C_LO, C_NLI, 0)
                fw.op("pool", lambda h: h.memset(CT[:], 0.0), writes=[CT.r])
                for tl in self.tile_order(d):
                    t0 = tl * 128
                    tsl = slice(t0, t0 + 128)
                    fw.dma(qT[:], MQ[:, :, tsl], writes=[qT.r])
                    fw.dma(kT[:], MK[:, :, tsl], writes=[kT.r])
                    fw.dma(vT[:], MV[:, :, tsl], writes=[vT.r])
                    fw.dma(ifT[:], IFs[:, tsl], writes=[ifT.r])
                    self.ts("pool", kT[:], kT[:], 0.0625, ALU.mult, [kT.r], [kT.r])
                    if self.ck(1):
                        return
                    pv, prs = self.pb(2)
                    pv3 = pv.rearrange("p (c t) -> p c t", t=128)
                    for c in range(8):
                        self.tr(pv3[:, c, :], kT[:, c, :], [kT.r], prs)
                    self.act(ktok[:], pv3, AF.Copy, prs, [ktok.r])
                    pv, prs = self.pb(2)
                    pv3 = pv.rearrange("p (c t) -> p c t", t=128)
                    for c in range(8):
                        self.tr(pv3[:, c, :], vT[:, c, :], [vT.r], prs)
                    self.act(vtok[:, :, 0:256].rearrange("p h (c e) -> p h c e", e=128),
                             pv.rearrange("p (h c e) -> p h c e", c=2, e=128), AF.Copy, prs, [vtok.r])
                    if self.ck(2):
                        return
                    pv, prs = self.pb(1)
                    self.tr(pv[:, 0:16], ifT[:], [ifT.r], prs)
                    self.act(ift[:], pv[:, 0:16], AF.Copy, prs, [ift.r])
                    if self.ck(2.2):
                        return
                    self.tt("dve", gt[:, 0:8], ift[:, 0:8], sm[:, 0:8], ALU.add, [ift.r, sm.r], [gt.r])
                    self.tt("dve", gt[:, 16:24], ift[:, 8:16], sm[:, 8:16], ALU.add, [ift.r, sm.r], [gt.r])
                    if self.ck(2.4):
                        return
                    self.act(gt[:, 16:24], gt[:, 16:24], AF.Exp, [gt.r], [gt.r], scale=-1.0)
                    if self.ck(2.6):
                        return
                    self.act(gt[:, 16:24], gt[:, 16:24], AF.Ln, [gt.r], [gt.r], bias=1.0)
                    self.ts("dve", gt[:, 8:16], gt[:, 16:24], -1.0, ALU.mult, [gt.r], [gt.r])
                    if self.ck(3):
                        return
                    ig = gt[:, d * 4:d * 4 + 4]
                    lf = gt[:, 8 + d * 4:8 + d * 4 + 4]
                    colA, colE, den, rc = cols[:, 0, :], cols[:, 1, :], cols[:, 2, :], cols[:, 3, :]
                    self.tt("dve", GL[:], bc(self.cst(Cm).unsqueeze(1), [128, H, 128]),
                            bc(lf.unsqueeze(2), [128, H, 128]), ALU.mult, [gt.r, cr], [GL.r])
                    if self.ck(3.1):
                        return
                    pv, prs = self.pb(1)
                    self.mm(pv[:, 0:H], self.cst(Cm), lf, [gt.r, cr], prs)
                    self.tt("dve", colA, ig, pv[:, 0:H], ALU.subtract, prs + [gt.r], [cols.r])
                    if self.ck(3.2):
                        return
                    self.tt("dve", M2[:], bc(self.cst(NM).unsqueeze(1), [128, H, 128]),
                            bc(colA.unsqueeze(2), [128, H, 128]), ALU.add, [cols.r, cr], [M2.r])
                    if self.ck(3.3):
                        return
                    pv, prs = self.pb(1)
                    self.mm(pv, self.cst(C_ONES), GL[:].rearrange("p h y -> p (h y)"), [GL.r, cr], prs, True, False)
                    self.mm(pv, self.cst(C_IDENT), M2[:].rearrange("p h y -> p (h y)"), [M2.r, cr], prs, False, True)
                    self.act(DT[:].rearrange("p h y -> p (h y)"), pv, AF.Exp, prs, [DT.r])
                    if self.ck(3.4):
                        return
                    pv, prs = self.pb(1)
                    pe3 = pv.rearrange("p (h y) -> p h y", y=128)
                    self.mm(pv, self.cst(C_ONES), GL[:].rearrange("p h y -> p (h y)"), [GL.r, cr], prs)
                    self.act(EB[:].rearrange("p h y -> p (h y)"), pv, AF.Exp, prs, [EB.r])
                    if self.ck(3.5):
                        return
                    self.act(colE, colA, AF.Exp, [cols.r], [cols.r])
                    self.tt("dve", colE, colE, EB[:, :, last], ALU.mult, [cols.r, EB.r], [cols.r])
                    if self.ck(4):
                        return
                    pv, prs = self.pb(1)
                    ps3 = pv.rearrange("p (h y) -> p h y", y=128)
                    for h_ in range(H):
                        for kc in range(2):
                            c = 2 * h_ + kc
                            self.mm(ps3[:, h_, :], kT[:, c, :], qT[:, c, :], [kT.r, qT.r], prs, kc == 0, kc == 1)
                    self.tt("dve", WT[:], ps3, DT[:], ALU.mult, prs + [DT.r], [WT.r])
                    self.tt("dve", qd[:].rearrange("p (h c) t -> p h c t", c=2),
                            qT[:].rearrange("p (h c) t -> p h c t", c=2),
                            bc(EB[:].unsqueeze(2), [128, H, 2, 128]), ALU.mult, [qT.r, EB.r], [qd.r])
                    self.tt("pool", kw[:].rearrange("p (h c) t -> p h c t", c=2),
                            ktok[:].rearrange("p (h c) t -> p h c t", c=2),
                            bc(colE.unsqueeze(2).unsqueeze(3), [128, H, 2, 128]), ALU.mult, [ktok.r, cols.r], [kw.r])
                    if self.ck(5):
                        return
                    pv, prs = self.pb(4)
                    pn = pv.rearrange("p (h y) -> p h y", y=512)
                    for h_ in range(H):
                        self.mm(pn[:, h_, 0:257], WT[:, h_, :], vtok[:, h_, :], [WT.r, vtok.r], prs, True, False)
                        for kc in range(2):
                            c = 2 * h_ + kc
                            self.mm(pn[:, h_, 0:257], qd[:, c, :], CT[:, c, :], [qd.r, CT.r], prs, False, kc == 1)
                    self.act(hr[:], pn[:, :, 0:257], AF.Copy, prs, [hr.r])
                    self.act(den, hr[:, :, 256], AF.Abs, [hr.r], [cols.r])
                    self.ts("dve", den, den, 1.0, ALU.max, [cols.r], [cols.r])
                    fw.op("dve", lambda h: h.reciprocal(out=rc, in_=den), reads=[cols.r], writes=[cols.r])
                    self.tt("dve", hd[:], hr[:, :, 0:256], bc(rc.unsqueeze(2), [128, H, 256]), ALU.mult,
                            [hr.r, cols.r], [hd.r])
                    if self.ck(6):
                        return
                    for nm, bf in (("d_gt", gt), ("d_cols", cols), ("d_DT", DT), ("d_EB", EB), ("d_WT", WT),
                                   ("d_hr", hr), ("d_GL", GL), ("d_M2", M2), ("d_vtok", vtok), ("d_qd", qd)):
                        self.dump(nm, bf)
                    bend = EB[:, :, last]
                    for kc in range(2):
                        pv, prs = self.pb(4)
                        pc = pv.rearrange("p (h y) -> p h y", y=512)
                        for h_ in range(H):
                            self.mm(pc[:, h_, 0:257], kw[:, 2 * h_ + kc, :], vtok[:, h_, :], [kw.r, vtok.r], prs)
                        cv = CT[:].rearrange("p (h c) e -> p h c e", c=2)[:, :, kc, :]
                        self.tt("dve", cv, cv, bc(bend.unsqueeze(2), [128, H, 257]), ALU.mult, [CT.r, EB.r], [CT.r])
                        self.tt("dve", cv, cv, pc[:, :, 0:257], ALU.add, prs + [CT.r], [CT.r])
                    if self.ck(7):
                        return
                    if d == 0:
                        fw.dma(HF[tsl, :], hd[:].rearrange("p h e -> p (h e)"), reads=[hd.r])
                        continue
                    fw.dma(hf[:].rearrange("p h e -> p (h e)"), HF[tsl, :], writes=[hf.r])
                    fw.dma(oT[:], MO[:, :, tsl], writes=[oT.r])
                    fw.dma(zs[:], MZ[:, :, tsl], writes=[zs.r])
                    self.tt("dve", hd[:], hd[:], hf[:], ALU.add, [hd.r, hf.r], [hd.r])
                    pv, prs = self.pb(2)
                    for c in range(8):
                        self.tr(pv[:, c * 128:(c + 1) * 128], oT[:, c, :], [oT.r], prs)
                    self.tt("dve", hd[:].rearrange("p h e -> p (h e)"), hd[:].rearrange("p h e -> p (h e)"), pv,
                            ALU.mult, prs + [hd.r], [hd.r])
                    self.act(sq[:], hd[:], AF.Square, [hd.r], [sq.r])
                    fw.op("dve", lambda h: h.tensor_reduce(out=den, in_=sq[:], axis=AX.X, op=ALU.add),
                          reads=[sq.r], writes=[cols.r])
                    self.act(den, den, AF.Sqrt, [cols.r], [cols.r], bias=EPS, scale=1.0 / 256)
                    fw.op("dve", lambda h: h.reciprocal(out=rc, in_=den), reads=[cols.r], writes=[cols.r])
                    self.tt("dve", hd[:], hd[:], bc(rc.unsqueeze(2), [128, H, 256]), ALU.mult, [hd.r, cols.r], [hd.r])
                    self.tt("pool", hd[:], hd[:], bc(sm[:, 16:272].unsqueeze(1), [128, H, 256]), ALU.mult,
                            [hd.r, sm.r], [hd.r])
                    pv, prs = self.pb(2)
                    pv3 = pv.rearrange("p (c t) -> p c t", t=128)
                    hd2 = hd[:].rearrange("p h e -> p (h e)")
                    for c in range(8):
                        self.tr(pv3[:, c, :], hd2[:, c * 128:(c + 1) * 128], [hd.r], prs)
                    self.tt("dve", yo[:], pv3, zs[:], ALU.mult, prs + [zs.r], [yo.r])
                    fw.dma(YC[:, :, tsl], yo[:], reads=[yo.r])


    def stage_gdn(self, l):
        cfg, fw = self.cfg, self.fw
        H = 8
        fm = lambda name: self.scr[name].rearrange("(c p) t -> p c t", p=128)
        QKV, ZG, YA = fm("QKV"), fm("ZG"), fm("YA")
        ABs, HF = self.scr["AB"], self.scr["HF"]
        cr = self.consts.r
        B3 = [128, H, 128]
        flat = lambda ap: ap.rearrange("p h y -> p (h y)")
        with ExitStack() as es:
            sm = self.sb(es, "gsm", [128, 160])
            cw = self.sb(es, "cw", [128, 5, 24])
            fw.dma(sm[:], bc(self.inp["gdn_small"][l:l + 1, :], [128, 160]), writes=[sm.r])
            fw.dma(cw[:], self.inp["gdn_conv_c"][:, l], writes=[cw.r])
            self.act(sm[:, 0:16], sm[:, 0:16], AF.Exp, [sm.r], [sm.r])
            self.ts("dve", sm[:, 0:16], sm[:, 0:16], -1.0, ALU.mult, [sm.r], [sm.r])
            buf = self.sb(es, "buf", [128, 24, 132])
            acc = self.sb(es, "acc", [128, 24, 128])
            tmp = self.sb(es, "tmp", [128, 24, 128])
            sq = self.sb(es, "gsq", [128, 16, 128])
            rn = self.sb(es, "grn", [128, 16, 128])
            ktok = self.sb(es, "gktok", B3)
            vtok = self.sb(es, "gvtok", B3)
            abT = self.sb(es, "abT", [32, 128])
            gt = self.sb(es, "ggt", [128, 4, 16])
            cl = self.sb(es, "gcl", [128, 6, H])
            GL = self.sb(es, "gGL", B3)
            GLb = self.sb(es, "gGLb", B3)
            PQ = [self.sb(es, f"gPQ{i}", [128, 2, H, 128]) for i in range(2)]
            TT = self.sb(es, "gTT", B3)
            QKD = self.sb(es, "gQKD", B3)
            kend = self.sb(es, "gkend", B3)
            kbg = self.sb(es, "gkbg", B3)
            vb = self.sb(es, "gvb", B3)
            qdec = self.sb(es, "gqdec", B3)
            nwT = self.sb(es, "gnwT", B3)
            vnew = self.sb(es, "gvnew", B3)
            ob = self.sb(es, "gob", B3)
            hf = self.sb(es, "ghf", B3)
            S = self.sb(es, "gS", B3)
            zs = self.sb(es, "gzs", B3)
            yo = self.sb(es, "gyo", B3)
            M2 = [tmp[:, 0:8, :], tmp[:, 8:16, :], tmp[:, 16:24, :]]
            DTi, Q0D, P0D, EG = sq[:, 0:8, :], sq[:, 8:16, :], rn[:, 0:8, :], rn[:, 8:16, :]
            qT, kT, vT = acc[:, 0:8, :], acc[:, 8:16, :], acc[:, 16:24, :]
            gall, lnball, betall, gtmp = gt[:, 0, :], gt[:, 1, :], gt[:, 2, :], gt[:, 3, :]
            gcc, col2, glast, colE, col3, gend = (cl[:, i, :] for i in range(6))

            def decay(dst, dst_r, lhs_c, GLx, M2x):
                pv, prs = self.pb(2)
                for half in range(2):
                    o = pv[:, half * 512:(half + 1) * 512]
                    self.mm(o, self.cst(lhs_c), flat(GLx[:, 4 * half:4 * half + 4, :]), [GLx_r(GLx), cr], prs,
                            True, M2x is None)
                    if M2x is not None:
                        self.mm(o, self.cst(C_IDENT), flat(M2x[:, 4 * half:4 * half + 4, :]), [tmp.r, cr], prs,
                                False, True)
                self.act(flat(dst), pv, AF.Exp, prs, [dst_r])

            def GLx_r(g):
                return GL.r if g is GLt else GLb.r
            GLt, GLbt = GL[:], GLb[:]

            for d in range(2):
                if d == 0:
                    Cm, NDTi, NDTs, NDs = C_UP, C_NUI, C_NUS, C_NLS
                else:
                    Cm, NDTi, NDTs, NDs = C_LO, C_NLI, C_NLS, C_NUS
                fw.op("pool", lambda h: h.memset(S[:], 0.0), writes=[S.r])
                for tl in self.tile_order(d):
                    t0 = tl * 128
                    tsl = slice(t0, t0 + 128)
                    seg_lo, seg_hi = (0, CTX) if tl < 2 else (CTX, cfg.T)
                    lo, hi = max(t0 - 2, seg_lo), min(t0 + 130, seg_hi)
                    if lo > t0 - 2:
                        fw.op("pool", lambda h: h.memset(buf[:, :, 0:2], 0.0), writes=[buf.r])
                    if hi < t0 + 130:
                        fw.op("pool", lambda h: h.memset(buf[:, :, 130:132], 0.0), writes=[buf.r])
                    for i in range(3):
                        fw.dma(buf[:, 8 * i:8 * i + 8, lo - (t0 - 2):hi - (t0 - 2)], QKV[:, 8 * i:8 * i + 8, lo:hi],
                               writes=[buf.r])
                    fw.dma(abT[:], ABs[:, tsl], writes=[abT.r])
                    for j in range(5):
                        wj = bc(cw[:, j, :].unsqueeze(2), [128, 24, 128])
                        if j == 0:
                            self.tt("dve", acc[:], buf[:, :, 0:128], wj, ALU.mult, [buf.r, cw.r], [acc.r])
                        else:
                            self.tt("pool", tmp[:], buf[:, :, j:j + 128], wj, ALU.mult, [buf.r, cw.r], [tmp.r])
                            self.tt("dve", acc[:], acc[:], tmp[:], ALU.add, [acc.r, tmp.r], [acc.r])
                    self.act(acc[:], acc[:], AF.Silu, [acc.r], [acc.r])
                    self.act(sq[:], acc[:, 0:16, :], AF.Square, [acc.r], [sq.r])
                    pv, prs = self.pb(4)
                    for i in range(4):
                        self.mm(pv[:, i * 512:(i + 1) * 512], self.cst(C_ONES), flat(sq[:, 4 * i:4 * i + 4, :]),
                                [sq.r, cr], prs)
                    self.act(flat(rn[:, 0:8, :]), pv[:, 0:1024], AF.Sqrt, prs, [rn.r], bias=128.0 * EPS, scale=128.0)
                    self.act(flat(rn[:, 8:16, :]), pv[:, 1024:2048], AF.Sqrt, prs, [rn.r], bias=EPS, scale=1.0)
                    fw.op("dve", lambda h: h.reciprocal(out=rn[:], in_=rn[:]), reads=[rn.r], writes=[rn.r])
                    self.tt("dve", acc[:, 0:16, :], acc[:, 0:16, :], rn[:], ALU.mult, [acc.r, rn.r], [acc.r])
                    for src, dstb in ((kT, ktok), (vT, vtok)):
                        pv, prs = self.pb(2)
                        pv3 = pv.rearrange("p (h y) -> p h y", y=128)
                        for h_ in range(H):
                            self.tr(pv3[:, h_, :], src[:, h_, :], [acc.r], prs)
                        self.act(dstb[:], pv3, AF.Copy, prs, [dstb.r])
                    pv, prs = self.pb(1)
                    self.tr(pv[:, 0:32], abT[:], [abT.r], prs)
                    self.act(gall, pv[:, 0:16], AF.Identity, prs, [gt.r])
                    self.act(gtmp, pv[:, 16:32], AF.Exp, prs, [gt.r], scale=-1.0)
                    self.tt("dve", gall, gall, sm[:, 16:32], ALU.add, [gt.r, sm.r], [gt.r])
                    self.act(gall, gall, AF.Exp, [gt.r], [gt.r])
                    self.act(gall, gall, AF.Ln, [gt.r], [gt.r], bias=1.0)
                    self.tt("dve", gall, gall, sm[:, 0:16], ALU.mult, [gt.r, sm.r], [gt.r])
                    self.act(gtmp, gtmp, AF.Ln, [gt.r], [gt.r], bias=1.0)
                    self.ts("dve", lnball, gtmp, -1.0, ALU.mult, [gt.r], [gt.r])
                    self.act(betall, lnball, AF.Exp, [gt.r], [gt.r])
                    g_d, lnb_d, beta_d = gall[:, 8 * d:8 * d + 8], lnball[:, 8 * d:8 * d + 8], betall[:, 8 * d:8 * d + 8]
                    self.tt("dve", GL[:], bc(self.cst(Cm).unsqueeze(1), B3), bc(g_d.unsqueeze(2), B3), ALU.mult,
                            [gt.r, cr], [GL.r])
                    self.tt("pool", GLb[:], bc(self.cst(C_IDENT).unsqueeze(1), B3), bc(lnb_d.unsqueeze(2), B3),
                            ALU.mult, [gt.r, cr], [GLb.r])
                    self.tt("dve", GLb[:], GLb[:], GL[:], ALU.add, [GLb.r, GL.r], [GLb.r])
                    pv, prs = self.pb(1)
                    self.mm(pv[:, 0:H], self.cst(Cm), g_d, [gt.r, cr], prs)
                    self.mm(pv[:, 8:8 + H], self.cst(C_ONES), g_d, [gt.r, cr], prs)
                    self.act(cl[:, 0, :], pv[:, 0:H], AF.Copy, prs, [cl.r])
                    self.act(cl[:, 2, :], pv[:, 8:8 + H], AF.Copy, prs, [cl.r])
                    self.tt("dve", col2, gcc, lnb_d, ALU.add, [cl.r, gt.r], [cl.r])
                    self.tt("dve", colE, glast, gcc, ALU.subtract, [cl.r], [cl.r])
                    self.act(colE, colE, AF.Exp, [cl.r], [cl.r])
                    self.act(col3, col2, AF.Exp, [cl.r], [cl.r])
                    self.act(gend, glast, AF.Exp, [cl.r], [cl.r])
                    self.tt("dve", M2[0], bc(self.cst(NDTi).unsqueeze(1), B3), bc(gcc.unsqueeze(2), B3), ALU.subtract,
                            [cl.r, cr], [tmp.r])
                    self.tt("dve", M2[1], bc(self.cst(NDTs).unsqueeze(1), B3), bc(gcc.unsqueeze(2), B3), ALU.subtract,
                            [cl.r, cr], [tmp.r])
                    self.tt("dve", M2[2], bc(self.cst(NDs).unsqueeze(1), B3), bc(col2.unsqueeze(2), B3), ALU.add,
                            [cl.r, cr], [tmp.r])
                    decay(DTi, sq.r, C_ONES, GLt, M2[0])
                    decay(Q0D, sq.r, C_ONES, GLbt, M2[1])
                    decay(P0D, rn.r, C_NEGONES, GLt, M2[2])
                    decay(EG, rn.r, C_ONES, GLt, None)
                    pv, prs = self.pb(2)
                    pk3 = pv.rearrange("p (h y) -> p h y", y=128)
                    for h_ in range(H):
                        self.mm(pk3[:, h_, :], kT[:, h_, :], kT[:, h_, :], [acc.r], prs)
                    Pc, Pn = PQ[0], PQ[1]
                    self.stt(Pc[:, 0], pk3, -1.0, P0D, ALU.mult, ALU.mult, prs + [rn.r], [Pc.r])
                    self.stt(Pc[:, 1], pk3, -1.0, Q0D, ALU.mult, ALU.mult, prs + [sq.r], [Pc.r])
                    pv, prs = self.pb(2)
                    pq3 = pv.rearrange("p (h y) -> p h y", y=128)
                    for h_ in range(H):
                        self.mm(pq3[:, h_, :], kT[:, h_, :], qT[:, h_, :], [acc.r], prs)
                    self.tt("dve", QKD[:], pq3, DTi, ALU.mult, prs + [sq.r], [QKD.r])
                    self.tt("pool", kend[:], ktok[:], bc(colE.unsqueeze(2), B3), ALU.mult, [ktok.r, cl.r], [kend.r])
                    self.tt("pool", kbg[:], ktok[:], bc(col3.unsqueeze(2), B3), ALU.mult, [ktok.r, cl.r], [kbg.r])
                    self.tt("pool", vb[:], vtok[:], bc(beta_d.unsqueeze(2), B3), ALU.mult, [vtok.r, gt.r], [vb.r])
                    self.tt("dve", qdec[:], qT, EG, ALU.mult, [acc.r, rn.r], [qdec.r])
                    self.tt("dve", TT[:], Pc[:, 1], bc(self.cst(C_IDENT).unsqueeze(1), B3), ALU.add, [Pc.r, cr], [TT.r])
                    for k in range(1, 8):
                        need_q = k <= 5
                        last_it = k == 7
                        if need_q:
                            pv1, prs1 = self.pb(2)
                            p13 = pv1.rearrange("p (h y) -> p h y", y=128)
                            for h_ in range(H):
                                self.mm(p13[:, h_, :], Pc[:, 0, h_, :], Pc[:, 1, h_, :], [Pc.r], prs1)
                        if k >= 2:
                            pv2, prs2 = self.pb(2)
                            p23 = pv2.rearrange("p (h y) -> p h y", y=128)
                            for h_ in range(H):
                                self.mm(p23[:, h_, :], Pc[:, 0, h_, :], TT[:, h_, :], [Pc.r, TT.r], prs2)
                        if not last_it:
                            pv3_, prs3 = self.pb(2)
                            p33 = pv3_.rearrange("p (h y) -> p h y", y=128)
                            for h_ in range(H):
                                self.mm(p33[:, h_, :], Pc[:, 1, h_, :], Pc[:, 0, h_, :], [Pc.r], prs3)
                        if need_q:
                            self.act(Pn[:, 1], p13, AF.Copy, prs1, [Pn.r])
                        if k >= 2:
                            self.tt("dve", TT[:], TT[:], p23, ALU.add, prs2 + [TT.r], [TT.r])
                        if not last_it:
                            self.act(Pn[:, 0], p33, AF.Copy, prs3, [Pn.r])
                        Pc, Pn = Pn, Pc
                    pv, prs = self.pb(2)
                    pw3 = pv.rearrange("p (h y) -> p h y", y=128)
                    for h_ in range(H):
                        self.mm(pw3[:, h_, :], kbg[:, h_, :], TT[:, h_, :], [kbg.r, TT.r], prs)
                    self.act(nwT[:], pw3, AF.Identity, prs, [nwT.r], scale=-1.0)
                    pv, prs = self.pb(2)
                    pn3 = pv.rearrange("p (h y) -> p h y", y=128)
                    for h_ in range(H):
                        self.mm(pn3[:, h_, :], TT[:, h_, :], vb[:, h_, :], [TT.r, vb.r], prs, True, False)
                        self.mm(pn3[:, h_, :], nwT[:, h_, :], S[:, h_, :], [nwT.r, S.r], prs, False, True)
                    self.act(vnew[:], pn3, AF.Copy, prs, [vnew.r])
                    pv, prs = self.pb(2)
                    po3 = pv.rearrange("p (h y) -> p h y", y=128)
                    for h_ in range(H):
                        self.mm(po3[:, h_, :], qdec[:, h_, :], S[:, h_, :], [qdec.r, S.r], prs, True, False)
                        self.mm(po3[:, h_, :], QKD[:, h_, :], vnew[:, h_, :], [QKD.r, vnew.r], prs, False, True)
                    self.act(ob[:], po3, AF.Copy, prs, [ob.r])
                    pv, prs = self.pb(2)
                    ps3 = pv.rearrange("p (h y) -> p h y", y=128)
                    for h_ in range(H):
                        self.mm(ps3[:, h_, :], kend[:, h_, :], vnew[:, h_, :], [kend.r, vnew.r], prs)
                    self.tt("dve", S[:], S[:], bc(gend.unsqueeze(2), B3), ALU.mult, [S.r, cl.r], [S.r])
                    self.tt("dve", S[:], S[:], ps3, ALU.add, prs + [S.r], [S.r])
                    for nm, bf in (("g_acc", acc), ("g_gt", gt), ("g_cl", cl), ("g_sq", sq), ("g_rn", rn), ("g_TT", TT),
                                   ("g_ob", ob), ("g_vnew", vnew), ("g_QKD", QKD)):
                        self.dump(nm, bf)
                    if d == 0:
                        fw.dma(HF[tsl, :], flat(ob[:]), reads=[ob.r])
                        continue
                    fw.dma(flat(hf[:]), HF[tsl, :], writes=[hf.r])
                    fw.dma(zs[:], ZG[:, :, tsl], writes=[zs.r])
                    self.tt("dve", ob[:], ob[:], hf[:], ALU.add, [ob.r, hf.r], [ob.r])
                    self.act(vnew[:], ob[:], AF.Square, [ob.r], [vnew.r])
                    fw.op("dve", lambda h: h.tensor_reduce(out=gcc, in_=vnew[:], axis=AX.X, op=ALU.add),
                          reads=[vnew.r], writes=[cl.r])
                    self.act(gcc, gcc, AF.Sqrt, [cl.r], [cl.r], bias=EPS, scale=1.0 / 128)
                    fw.op("dve", lambda h: h.reciprocal(out=gcc, in_=gcc), reads=[cl.r], writes=[cl.r])
                    self.tt("dve", ob[:], ob[:], bc(gcc.unsqueeze(2), B3), ALU.mult, [ob.r, cl.r], [ob.r])
                    self.tt("pool", ob[:], ob[:], bc(sm[:, 32:160].unsqueeze(1), B3), ALU.mult, [ob.r, sm.r], [ob.r])
                    pv, prs = self.pb(2)
                    pv3 = pv.rearrange("p (h y) -> p h y", y=128)
                    for h_ in range(H):
                        self.tr(pv3[:, h_, :], ob[:, h_, :], [ob.r], prs)
                    self.tt("dve", yo[:], pv3, zs[:], ALU.mult, prs + [zs.r], [yo.r])
                    fw.dma(YA[:, :, tsl], yo[:], reads=[yo.r])


    def stage_s5(self, l):
        cfg, fw = self.cfg, self.fw
        T = cfg.T
        NL = (T - 1).bit_length()
        cr = self.consts.r
        with ExitStack() as es:
            lam = self.sb(es, "lam", [128, 32, 2, 2])
            stp = self.sb(es, "stp", [128, 32, 2])
            Bb = self.sb(es, "Bb", [32, 2, 128])
            Cb = self.sb(es, "Cb", [128, 32, 2, 32])
            dq = self.sb(es, "dq", [32, 32])
            fw.dma(lam[:], self.inp["s5_lam"][:, l], writes=[lam.r])
            fw.dma(stp[:], self.inp["s5_lstep"][:, l], writes=[stp.r])
            fw.dma(Cb[:], self.inp["s5_Cblk"][:, l], writes=[Cb.r])
            fw.dma(dq[:], self.inp["s5_d_q"][:, l], writes=[dq.r])
            self.ts("dve", Cb[:, :, 1, :], Cb[:, :, 1, :], -1.0, ALU.mult, [Cb.r], [Cb.r])
            UNI = self.sb(es, "UNI", [128, NL, 32, 2, 2])
            U2 = self.sb(es, "U2", [128, NL, 32, 2, 2])
            RR = self.sb(es, "RR", [128, 32, 2])
            FF = self.sb(es, "FF", [128, 32, 2, 2])
            FS = self.sb(es, "FS", [128, 32, 2, 2])
            w = [self.sb(es, f"s5w{i}", [128, 32, 2]) for i in range(8)]
            lre, lim = lam[:, :, :, 0], lam[:, :, :, 1]
            ar, th, cc, ss, t1, t2, lbr, lbi = (x[:] for x in w)
            wr = [x.r for x in w]
            self.act(stp[:], stp[:], AF.Exp, [stp.r], [stp.r])
            self.tt("dve", ar, lre, stp[:], ALU.mult, [lam.r, stp.r], [wr[0]])
            self.tt("dve", th, lim, stp[:], ALU.mult, [lam.r, stp.r], [wr[1]])
            self.act(RR[:], ar, AF.Exp, [wr[0]], [RR.r])
            hp = self.sb(es, "halfpi", [128, 1])
            fw.op("dve", lambda h: h.memset(hp[:], float(np.pi / 2)), writes=[hp.r])
            self.act(cc, th, AF.Sin, [wr[1], hp.r], [wr[2]], bias=hp[:, 0:1], scale=1.0 / 16)
            self.act(ss, th, AF.Sin, [wr[1]], [wr[3]], scale=1.0 / 16)

            def csq(a, b, ra, rb):
                self.tt("dve", t1, a, a, ALU.mult, [ra], [wr[4]])
                self.tt("dve", t2, b, b, ALU.mult, [rb], [wr[5]])
                self.stt(b, a, 2.0, b, ALU.mult, ALU.mult, [ra, rb], [rb])
                self.tt("dve", a, t1, t2, ALU.subtract, [wr[4], wr[5]], [ra])
            for _ in range(4):
                csq(cc, ss, wr[2], wr[3])
            self.tt("dve", lbr, cc, RR[:], ALU.mult, [wr[2], RR.r], [wr[6]])
            self.tt("dve", lbi, ss, RR[:], ALU.mult, [wr[3], RR.r], [wr[7]])
            self.tt("dve", t1, lre, lre, ALU.mult, [lam.r], [wr[4]])
            self.tt("dve", t2, lim, lim, ALU.mult, [lam.r], [wr[5]])
            self.tt("dve", t1, t1, t2, ALU.add, [wr[4], wr[5]], [wr[4]])
            fw.op("dve", lambda h: h.reciprocal(out=t1, in_=t1), reads=[wr[4]], writes=[wr[4]])
            self.ts("dve", ar, lbr, -1.0, ALU.add, [wr[6]], [wr[0]])
            self.tt("dve", t2, ar, lre, ALU.mult, [wr[0], lam.r], [wr[5]])
            self.tt("dve", th, lbi, lim, ALU.mult, [wr[7], lam.r], [wr[1]])
            self.tt("dve", t2, t2, th, ALU.add, [wr[5], wr[1]], [wr[5]])
            self.tt("dve", FF[:, :, :, 0], t2, t1, ALU.mult, [wr[5], wr[4]], [FF.r])
            self.tt("dve", t2, lbi, lre, ALU.mult, [wr[7], lam.r], [wr[5]])
            self.tt("dve", th, ar, lim, ALU.mult, [wr[0], lam.r], [wr[1]])
            self.tt("dve", t2, t2, th, ALU.subtract, [wr[5], wr[1]], [wr[5]])
            self.tt("dve", FF[:, :, :, 1], t2, t1, ALU.mult, [wr[5], wr[4]], [FF.r])
            self.ts("dve", FS[:, :, :, 0], FF[:, :, :, 1], -1.0, ALU.mult, [FF.r], [FS.r])
            self.act(FS[:, :, :, 1], FF[:, :, :, 1], AF.Copy, [FF.r], [FS.r])
            for k in range(NL):
                self.act(UNI[:, k, :, :, 0], cc, AF.Copy, [wr[2]], [UNI.r])
                self.act(UNI[:, k, :, :, 1], ss, AF.Copy, [wr[3]], [UNI.r])
                self.ts("dve", U2[:, k, :, :, 0], ss, -1.0, ALU.mult, [wr[3]], [U2.r])
                self.act(U2[:, k, :, :, 1], ss, AF.Copy, [wr[3]], [U2.r])
                if k < NL - 1:
                    csq(cc, ss, wr[2], wr[3])
            u32 = self.sb(es, "u32", [32, T])
            XA = self.sb(es, "XA", [128, 2, T])
            XB = self.sb(es, "XB", [128, 2, T])
            TM = self.sb(es, "TM", [128, 2, T])
            EE = self.sb(es, "EE", [128, 2, T])
            Y = self.sb(es, "Y5y", [32, T])
            yt = self.sb(es, "Y5t", [32, 512])
            segs = [(0, CTX), (CTX, T)]

            def rev(ap3, lo, hi, d):
                if d == 0:
                    return ap3[:, :, lo:hi]
                return ap3[:, :, hi - 1:lo - 1:-1] if lo > 0 else ap3[:, :, hi - 1::-1]

            for q in range(32):
                fw.dma(u32[:], self.scr["U5"][32 * q:32 * q + 32, :], writes=[u32.r])
                fw.dma(Bb[:], self.inp["s5_Bblk"][:, l, q], writes=[Bb.r])
                for d in range(2):
                    RAW = XB
                    for c0 in range(0, T, 2048):
                        n = min(2048, T - c0)
                        for ri in range(2):
                            pv, prs = self.pb(4)
                            for s0 in range(0, n, 512):
                                m = min(512, n - s0)
                                self.mm(pv[:, s0:s0 + m], Bb[:, ri, :], u32[:, c0 + s0:c0 + s0 + m], [Bb.r, u32.r], prs)
                            self.act(RAW[:, ri, c0:c0 + n], pv[:, 0:n], AF.Copy, prs, [RAW.r])
                    fre = FF[:, q, d, 0:1]
                    fs2 = FS[:, q, d, :]
                    for lo, hi in segs:
                        n = hi - lo
                        self.ts("dve", XA[:, :, lo:hi], rev(RAW[:], lo, hi, d), fre, ALU.mult, [RAW.r, FF.r], [XA.r])
                        self.tt("pool", TM[:, :, lo:hi], rev(RAW[:, ::-1, :], lo, hi, d),
                                bc(fs2.unsqueeze(2), [128, 2, n]), ALU.mult, [RAW.r, FS.r], [TM.r])
                    self.tt("dve", XA[:], XA[:], TM[:], ALU.add, [XA.r, TM.r], [XA.r])
                    self.dump("s_b", XA)
                    fw.op("pool", lambda h: h.memset(EE[:, 0, 0:1], 1.0), writes=[EE.r])
                    fw.op("pool", lambda h: h.memset(EE[:, 1, 0:1], 0.0), writes=[EE.r])
                    for k in range(NL):
                        n = 1 << k
                        m = min(n, T - n)
                        if m <= 0:
                            break
                        self.ts("dve", EE[:, :, n:n + m], EE[:, :, 0:m], UNI[:, k, q, d, 0:1], ALU.mult,
                                [EE.r, UNI.r], [EE.r])
                        self.tt("pool", TM[:, :, 0:m], EE[:, ::-1, 0:m], bc(U2[:, k, q, d, :].unsqueeze(2), [128, 2, m]),
                                ALU.mult, [EE.r, U2.r], [TM.r])
                        self.tt("dve", EE[:, :, n:n + m], EE[:, :, n:n + m], TM[:, :, 0:m], ALU.add, [EE.r, TM.r], [EE.r])
                    Ec = bc(EE[:, 0:1, :], [128, 2, T])
                    Es = bc(EE[:, 1:2, :], [128, 2, T])
                    self.dump("s_EE", EE)
                    self.tt("dve", TM[:], XA[:], Ec, ALU.mult, [XA.r, EE.r], [TM.r])
                    self.tt("pool", XB[:], XA[:, ::-1, :], Es, ALU.mult, [XA.r, EE.r], [XB.r])
                    self.tt("dve", XA[:, 0, :], TM[:, 0, :], XB[:, 0, :], ALU.add, [TM.r, XB.r], [XA.r])
                    self.tt("dve", XA[:, 1, :], TM[:, 1, :], XB[:, 1, :], ALU.subtract, [TM.r, XB.r], [XA.r])
                    self.dump("s_bt", XA)
                    self.dump("s_RR", RR)
                    self.dump("s_UNI", UNI)
                    rcoef = bc(RR[:, q, d:d + 1], [128, T])
                    for ri in range(2):
                        fw.op("dve", lambda h, ri=ri, rcoef=rcoef: h.tensor_tensor_scan(out=XB[:, ri, :], data0=rcoef, data1=XA[:, ri, :],
                                                                           initial=0.0, op0=ALU.mult, op1=ALU.add),
                              reads=[XA.r, RR.r], writes=[XB.r])
                    self.dump("s_z", XB)
                    self.tt("dve", TM[:], XB[:], Ec, ALU.mult, [XB.r, EE.r], [TM.r])
                    self.tt("pool", XA[:], XB[:, ::-1, :], Es, ALU.mult, [XB.r, EE.r], [XA.r])
                    self.tt("dve", XB[:, 0, :], TM[:, 0, :], XA[:, 0, :], ALU.subtract, [TM.r, XA.r], [XB.r])
                    self.tt("dve", XB[:, 1, :], TM[:, 1, :], XA[:, 1, :], ALU.add, [TM.r, XA.r], [XB.r])
                    cur = XB
                    self.dump("s_x", XB)
                    for lo, hi in segs:
                        for g0 in range(lo, hi, 512):
                            m = min(512, hi - g0)
                            pv, prs = self.pb(1)
                            self.mm(pv[0:32, 0:m], Cb[:, q, 0, :], cur[:, 0, g0:g0 + m], [Cb.r, cur.r], prs, True, False)
                            self.mm(pv[0:32, 0:m], Cb[:, q, 1, :], cur[:, 1, g0:g0 + m], [Cb.r, cur.r], prs, False, True)
                            if d == 0:
                                self.act(Y[:, g0:g0 + m], pv[0:32, 0:m], AF.Copy, prs, [Y.r])
                            else:
                                p_hi = hi - (g0 - lo)
                                p_lo = p_hi - m
                                self.act(yt[:, 0:m], pv[0:32, 0:m], AF.Copy, prs, [yt.r])
                                self.tt("dve", Y[:, p_lo:p_hi], Y[:, p_lo:p_hi], yt[:, m - 1::-1] if m > 0 else yt[:, 0:m],
                                        ALU.add, [Y.r, yt.r], [Y.r])
                self.stt(Y[:], u32[:], dq[:, q:q + 1], Y[:], ALU.mult, ALU.add, [u32.r, dq.r, Y.r], [Y.r])
                self.act(Y[:], Y[:], AF.Gelu, [Y.r], [Y.r])
                fw.dma(self.scr["Y5"][32 * q:32 * q + 32, :], Y[:], reads=[Y.r])

    def stage_s5post(self, l):
        cfg, fw = self.cfg, self.fw
        fm = lambda name: self.scr[name].rearrange("(c p) t -> p c t", p=128)
        Y5, Z5, YB = fm("Y5"), fm("Z5"), fm("YB")
        wv = self.inp["s5_w_glu"][l].rearrange("(k p) n -> p k n", p=128)
        with ExitStack() as es:
            bg = self.sb(es, "bglu", [128, 16])
            fw.dma(bg[:], self.inp["b_glu_c"][:, l], writes=[bg.r])
            yg = self.sb(es, "y5g", [128, KC, 512])
            wa = [self.sb(es, f"wa{i}", [128, KC, 128]) for i in range(2)]
            wb = [self.sb(es, f"wb{i}", [128, KC, 128]) for i in range(2)]
            zt = [self.sb(es, f"z5t{i}", [128, 512]) for i in range(2)]
            sg = self.sb(es, "sg5", [128, 512])
            ot = [self.sb(es, f"o5t{i}", [128, 512]) for i in range(2)]
            it = 0
            for (s0, n) in cfg.groups:
                fw.dma(yg[:, :, 0:n], Y5[:, :, s0:s0 + n], writes=[yg.r])
                for ct in range(8):
                    b = it % 2
                    it += 1
                    fw.dma(wa[b][:], wv[:, :, ct * 128:(ct + 1) * 128], writes=[wa[b].r])
                    fw.dma(wb[b][:], wv[:, :, D + ct * 128:D + (ct + 1) * 128], writes=[wb[b].r])
                    fw.dma(zt[b][:, 0:n], Z5[:, ct, s0:s0 + n], writes=[zt[b].r])
                    pa, pra = self.pb(1)
                    for k in range(KC):
                        self.mm(pa[:, 0:n], wa[b][:, k, :], yg[:, k, 0:n], [wa[b].r, yg.r], pra, k == 0, k == KC - 1, fast=True)
                    pb_, prb = self.pb(1)
                    for k in range(KC):
                        self.mm(pb_[:, 0:n], wb[b][:, k, :], yg[:, k, 0:n], [wb[b].r, yg.r], prb, k == 0, k == KC - 1, fast=True)
                    self.act(sg[:, 0:n], pb_[:, 0:n], AF.Sigmoid, prb + [bg.r], [sg.r], bias=bg[:, 8 + ct:9 + ct])
                    self.stt(ot[b][:, 0:n], pa[:, 0:n], bg[:, ct:ct + 1], sg[:, 0:n], ALU.add, ALU.mult,
                             pra + [bg.r, sg.r], [ot[b].r])
                    self.tt("dve", ot[b][:, 0:n], ot[b][:, 0:n], zt[b][:, 0:n], ALU.mult, [ot[b].r, zt[b].r], [ot[b].r])
                    fw.dma(YB[:, ct, s0:s0 + n], ot[b][:, 0:n], reads=[ot[b].r])


    def stage_merge(self, l):
        cfg, fw = self.cfg, self.fw
        T = cfg.T
        last = (l == cfg.DEPTH - 1)
        fm = lambda name: self.scr[name].rearrange("(c p) t -> p c t", p=128)
        YS = [fm("YA"), fm("YB"), fm("YC")]
        GT = self.scr["GATE"].rearrange("(i c p) t -> p i c t", i=3, p=128)
        OUTT = fm("OUTT")
        with ExitStack() as es:
            wo = self.sb(es, "wo", [128, KC, D])
            fw.dma(wo[:], self.inp["w_out"][l].rearrange("(k p) n -> p k n", p=128), writes=[wo.r])
            ys = self.sb(es, "ys", [128, 3, KC, 512])
            mg_ = self.sb(es, "mg", [128, KC, 512])
            og = self.sb(es, "og", [128, KC, 512])
            wt = [[self.sb(es, f"wbr{b}{i}", [128, KC, 128]) for i in range(3)] for b in range(2)]
            sgt = [self.sb(es, f"sgt{b}", [128, 3, 512]) for b in range(2)]
            tm = self.sb(es, "mtm", [128, 512])
            it = 0
            for gi, (s0, n) in enumerate(cfg.groups):
                if last and gi == 0:
                    continue
                for i in range(3):
                    fw.dma(ys[:, i, :, 0:n], YS[i][:, :, s0:s0 + n], writes=[ys.r])
                for dc in range(KC):
                    b = it % 2
                    it += 1
                    for i in range(3):
                        fw.dma(wt[b][i][:], self.inp["w_branch"][l, i].rearrange("(k p) n -> p k n", p=128)
                               [:, :, dc * 128:(dc + 1) * 128], writes=[wt[b][i].r])
                    fw.dma(sgt[b][:, :, 0:n], GT[:, :, dc, s0:s0 + n], writes=[sgt[b].r])
                    pp = []
                    for i in range(3):
                        pv, prs = self.pb(1)
                        for k in range(KC):
                            self.mm(pv[:, 0:n], wt[b][i][:, k, :], ys[:, i, k, 0:n], [wt[b][i].r, ys.r], prs,
                                    k == 0, k == KC - 1, fast=True)
                        pp.append((pv, prs))
                    self.tt("dve", mg_[:, dc, 0:n], pp[0][0][:, 0:n], sgt[b][:, 0, 0:n], ALU.mult,
                            pp[0][1] + [sgt[b].r], [mg_.r])
                    for i in (1, 2):
                        self.tt("dve", tm[:, 0:n], pp[i][0][:, 0:n], sgt[b][:, i, 0:n], ALU.mult,
                                pp[i][1] + [sgt[b].r], [tm.r])
                        self.tt("dve", mg_[:, dc, 0:n], mg_[:, dc, 0:n], tm[:, 0:n], ALU.add, [mg_.r, tm.r], [mg_.r])
                for dc in range(KC):
                    pv, prs = self.pb(1)
                    for k in range(KC):
                        self.mm(pv[:, 0:n], wo[:, k, dc * 128:(dc + 1) * 128], mg_[:, k, 0:n], [wo.r, mg_.r], prs,
                                k == 0, k == KC - 1, fast=True)
                    self.act(og[:, dc, 0:n], pv[:, 0:n], AF.Copy, prs, [og.r])
                fw.dma(OUTT[:, :, s0:s0 + n], og[:, :, 0:n], reads=[og.r])
        fw.barrier()
        with ExitStack() as es:
            xt = [self.sb(es, f"rxt{i}", [128, T]) for i in range(2)]
            ot = [self.sb(es, f"rot{i}", [128, T]) for i in range(2)]
            for dc in range(KC):
                b = dc % 2
                fw.dma(xt[b][:], self.scr["XT"][dc], writes=[xt[b].r])
                fw.dma(ot[b][:], OUTT[:, dc, :], writes=[ot[b].r])
                rd = [xt[b].r, ot[b].r, self.mod.r]
                if not last:
                    self.stt(xt[b][:, 0:CTX], ot[b][:, 0:CTX], self.mod[:, l, 16 + dc, 1:2], xt[b][:, 0:CTX],
                             ALU.mult, ALU.add, rd, [xt[b].r])
                if l % 2 == 0:
                    xv, ov = xt[b][:, CTX:], ot[b][:, CTX:]
                else:
                    xv = xt[b][:, CTX:].rearrange("p (r c) -> p c r", c=cfg.GW)
                    ov = ot[b][:, CTX:].rearrange("p (c r) -> p c r", r=cfg.ROWS)
                self.stt(xv, ov, self.mod[:, l, 16 + dc, 0:1], xv, ALU.mult, ALU.add, rd, [xt[b].r])
                fw.dma(self.scr["XT"][dc], xt[b][:], reads=[xt[b].r])

    def stage_final(self):
        cfg, fw = self.cfg, self.fw
        XTv = self.scr["XT"].rearrange("k p t -> p k t")
        with ExitStack() as es:
            gb = self.sb(es, "gb", [128, D])
            fw.dma(gb[:], bc(self.inp["final_norm_g"][0:1, :], [128, D]), writes=[gb.r])
            xf = [self.sb(es, f"xf{i}", [128, KC, 128]) for i in range(2)]
            sq = [self.sb(es, f"fsq{i}", [128, D]) for i in range(2)]
            st = [self.sb(es, f"fst{i}", [128, 2]) for i in range(2)]
            for tl in range(2, cfg.NT):
                b = tl % 2
                fw.dma(xf[b][:], XTv[:, :, tl * 128:(tl + 1) * 128], writes=[xf[b].r])
                pv, prs = self.pb(2)
                for k in range(KC):
                    self.tr(pv[:, k * 128:(k + 1) * 128], xf[b][:, k, :], [xf[b].r], prs)
                self.act(sq[b][:], pv, AF.Square, prs, [sq[b].r])
                self.fw.op("dve", lambda h, b=b: h.tensor_reduce(out=st[b][:, 0:1], in_=sq[b][:], axis=AX.X, op=ALU.add),
                           reads=[sq[b].r], writes=[st[b].r])
                self.act(st[b][:, 1:2], st[b][:, 0:1], AF.Sqrt, [st[b].r], [st[b].r], bias=EPS, scale=1.0 / D)
                self.fw.op("dve", lambda h, b=b: h.reciprocal(out=st[b][:, 0:1], in_=st[b][:, 1:2]),
                           reads=[st[b].r], writes=[st[b].r])
                self.stt(sq[b][:], pv, st[b][:, 0:1], gb[:], ALU.mult, ALU.mult, prs + [st[b].r, gb.r], [sq[b].r])
                fw.dma(self.out[(tl - 2) * 128:(tl - 1) * 128, :], sq[b][:], reads=[sq[b].r])


def colfmt(v, nk):
    v = np.asarray(v, np.float32)
    lead = v.shape[:-1]
    return np.ascontiguousarray(np.moveaxis(v.reshape(lead + (nk, 128)), -1, 0))


def host_inputs(inputs, cfg, b):
    Dp = cfg.DEPTH
    m = {}
    f32 = lambda a: np.asarray(a, np.float32)
    m["x"] = np.ascontiguousarray(inputs["x"][b], dtype=np.float32)
    m["ctx"] = np.ascontiguousarray(inputs["ctx"][b], dtype=np.float32)
    cv = np.stack([inputs["c"][b], inputs["c_ctx"]], axis=0)
    m["cvec"] = np.ascontiguousarray(np.transpose(colfmt(cv, KC), (0, 2, 1)))
    m["w_ada"] = np.ascontiguousarray(inputs["w_ada"][:Dp], dtype=np.float32)
    m["b_ada_c"] = colfmt(inputs["b_ada"][:Dp], 24)
    m["norm_g_c"] = colfmt(inputs["norm_g"][:Dp], KC)
    m["w_in"] = np.ascontiguousarray(inputs["w_in"][:Dp], dtype=np.float32)
    m["consts"] = make_consts()
    m["final_norm_g"] = np.asarray(inputs["final_norm_g"], np.float32).reshape(1, D)
    m["w_branch"] = np.ascontiguousarray(f32(inputs["w_branch"][:Dp]))
    m["w_out"] = np.ascontiguousarray(f32(inputs["w_out"][:Dp]))
    G, Pn, Hh = 64, 64, 16
    lam = np.stack([f32(inputs["s5_lam_re"][:Dp]), f32(inputs["s5_lam_im"][:Dp])], axis=-1)
    lam = lam.reshape(Dp, 2, 32, 2, Pn, 2)
    m["s5_lam"] = np.ascontiguousarray(np.transpose(lam, (3, 4, 0, 2, 1, 5)).reshape(128, Dp, 32, 2, 2))
    ls = f32(inputs["s5_log_step"][:Dp]).reshape(Dp, 2, 32, 2)
    ls = np.broadcast_to(ls[..., None], (Dp, 2, 32, 2, Pn))
    m["s5_lstep"] = np.ascontiguousarray(np.transpose(ls, (3, 4, 0, 2, 1)).reshape(128, Dp, 32, 2))
    Bblk = np.zeros((2, Hh, Dp, 32, 2, 2, Pn), np.float32)
    Cblk = np.zeros((2, Pn, Dp, 32, 2, 2, Hh), np.float32)
    for ri, (bn, cn) in enumerate((("s5_b_re", "s5_c_re"), ("s5_b_im", "s5_c_im"))):
        Bm = f32(inputs[bn][:Dp]).reshape(Dp, 32, 2, Pn, Hh)
        Cm = f32(inputs[cn][:Dp]).reshape(Dp, 32, 2, Hh, Pn)
        for gl in range(2):
            Bblk[gl, :, :, :, ri, gl, :] = np.transpose(Bm[:, :, gl], (3, 0, 1, 2))
            Cblk[gl, :, :, :, ri, gl, :] = np.transpose(Cm[:, :, gl], (3, 0, 1, 2))
    m["s5_Bblk"] = np.ascontiguousarray(Bblk.reshape(32, Dp, 32, 2, 128))
    m["s5_Cblk"] = np.ascontiguousarray(Cblk.reshape(128, Dp, 32, 2, 32))
    m["s5_d_q"] = np.ascontiguousarray(np.transpose(f32(inputs["s5_d"][:Dp]).reshape(Dp, 32, 32), (2, 0, 1)))
    m["b_glu_c"] = colfmt(inputs["s5_b_glu"][:Dp], 16)
    m["s5_w_glu"] = np.ascontiguousarray(f32(inputs["s5_w_glu"][:Dp]))
    m["gdn_small"] = np.ascontiguousarray(np.concatenate(
        [f32(inputs["gdn_a_log"][:Dp]).reshape(Dp, 16), f32(inputs["gdn_dt_bias"][:Dp]).reshape(Dp, 16),
         f32(inputs["gdn_norm_g"][:Dp]).reshape(Dp, 128)], axis=1))
    m["gdn_conv_c"] = colfmt(inputs["gdn_conv"][:Dp], 24)
    m["ml_small"] = np.ascontiguousarray(np.concatenate(
        [f32(inputs["ml_i_bias"][:Dp]).reshape(Dp, 8), f32(inputs["ml_f_bias"][:Dp]).reshape(Dp, 8),
         f32(inputs["ml_norm_g"][:Dp]).reshape(Dp, 256)], axis=1))
    return m


def run(inputs, cfg, cores, dbg=(), stages=None):
    kb = KB(cfg, dbg=dbg, stages=stages)
    nc = kb.build()
    in_maps = [host_inputs(inputs, cfg, b) for b in cores]
    res = run_bass_kernel_spmd(nc, in_maps, core_ids=list(range(len(cores))))
    return kb, res


def kernel(**inputs):
    cfg = Cfg()
    kb, res = run(inputs, cfg, list(range(N_CORES)))
    return np.stack([np.asarray(r["y"], dtype=np.float32) for r in res.results], axis=0)
```

```python
from contextlib import ExitStack
import threading
import os
import numpy as np
import concourse.bass as bass
import concourse.mybir as mybir
from concourse.bass_utils import run_bass_kernel_spmd

F32 = mybir.dt.float32
F32R = mybir.dt.float32r
INTERLEAVE = True
FAST_DENSE = False
AF = mybir.ActivationFunctionType
ALU = mybir.AluOpType
AX = mybir.AxisListType

D = 1024
KC = 8
CTX = 256
EPS = 1e-6
N_CORES = 8
NEG = -30000.0

SEGS = [("QKV", 0, 3072, "copy"), ("ZG", 3072, 1024, "silu"), ("AB", 4096, 32, "copy"),
        ("U5", 4128, 1024, "copy"), ("Z5", 5152, 1024, "silu"), ("MQ", 6176, 1024, "copy"),
        ("MK", 7200, 1024, "copy"), ("MV", 8224, 1024, "copy"), ("MO", 9248, 1024, "sigmoid"),
        ("MZ", 10272, 1024, "silu"), ("IF", 11296, 16, "copy"), ("GATE", 11312, 3072, "sigmoid")]

C_IDENT, C_ONES, C_NEGONES, C_UP, C_LO, C_NUI, C_NUS, C_NLI, C_NLS = range(9)


def make_consts():
    x = np.arange(128)[:, None]
    y = np.arange(128)[None, :]
    c = np.zeros((128, 9, 128), np.float32)
    c[:, C_IDENT] = (x == y)
    c[:, C_ONES] = 1.0
    c[:, C_NEGONES] = -1.0
    c[:, C_UP] = (x <= y)
    c[:, C_LO] = (x >= y)
    c[:, C_NUI] = np.where(x <= y, 0.0, NEG)
    c[:, C_NUS] = np.where(x < y, 0.0, NEG)
    c[:, C_NLI] = np.where(x >= y, 0.0, NEG)
    c[:, C_NLS] = np.where(x > y, 0.0, NEG)
    return c


class Cfg:
    def __init__(self, L=4096, GW=64, DEPTH=4):
        self.L, self.GW, self.DEPTH = L, GW, DEPTH
        self.ROWS = L // GW
        self.T = CTX + L
        self.NT = self.T // 128
        self.groups = [(0, 256)] + [(CTX + 512 * i, 512) for i in range(L // 512)]


class Res:
    __slots__ = ("name", "lw", "rd")

    def __init__(self, name=""):
        self.name = name
        self.lw = None
        self.rd = []


class Buf:
    def __init__(self, t, name):
        self.t = t
        self.r = Res(name)

    def __getitem__(self, k):
        return self.t[k]


class FW:
    ENG = ("pe", "act", "dve", "pool", "sp")

    def __init__(self, nc, n_dma_slots=16):
        self.nc = nc
        self.sem, self.cnt = {}, {}
        self.prog = {e: [] for e in self.ENG}
        self.waited = {e: {} for e in self.ENG}
        for e in self.ENG:
            self.sem[e] = nc.alloc_semaphore(name=f"s_{e}")
            self.cnt[e] = 0
        self.slots = []
        for i in range(n_dma_slots):
            k = f"dma{i}"
            self.sem[k] = nc.alloc_semaphore(name=f"s_{k}")
            self.cnt[k] = 0
            self.slots.append(k)
        self.slot_rr = 0
        self.n_ops = 0
        self.yield_hook = None

    def _wait(self, eng, k, v):
        wl = self.waited[eng]
        if (k != eng or eng != "pe") and wl.get(k, 0) < v:
            wl[k] = v
            self.prog[eng].append(lambda h, sem=self.sem[k], v=v: h.wait_ge(sem, v))

    def _deps(self, eng, reads, writes):
        deps = {}

        def add(ts):
            if ts is not None and deps.get(ts[0], 0) < ts[1]:
                deps[ts[0]] = ts[1]
        for r in reads:
            add(r.lw)
        for w in writes:
            add(w.lw)
            for t in w.rd:
                add(t)
        for k, v in deps.items():
            self._wait(eng, k, v)

    def _mark(self, ts, reads, writes):
        for r in reads:
            r.rd.append(ts)
            if len(r.rd) > 64:
                best = {}
                for k, v in r.rd:
                    if best.get(k, 0) < v:
                        best[k] = v
                r.rd = list(best.items())
        for w in writes:
            w.lw = ts
            w.rd = []

    def op(self, eng, fn, reads=(), writes=()):
        self._deps(eng, reads, writes)
        self.cnt[eng] += 1
        self.prog[eng].append(lambda h, fn=fn, sem=self.sem[eng]: fn(h).then_inc(sem, 1))
        self._mark((eng, self.cnt[eng]), reads, writes)
        self.n_ops += 1
        if self.yield_hook:
            self.yield_hook()

    def dma(self, out, in_, reads=(), writes=(), q="sp"):
        self._deps(q, reads, writes)
        k = self.slots[self.slot_rr]
        self.slot_rr = (self.slot_rr + 1) % len(self.slots)
        self._wait(q, k, self.cnt[k])
        self.cnt[k] += 16
        self.prog[q].append(
            lambda h, sem=self.sem[k], out=out, in_=in_: h.dma_start(out=out, in_=in_).then_inc(sem, 16))
        self._mark((k, self.cnt[k]), reads, writes)
        self.n_ops += 1
        if self.yield_hook:
            self.yield_hook()

    def barrier(self, engines=None):
        for e in (engines or self.ENG):
            for k in self.sem:
                self._wait(e, k, self.cnt[k])

    def emit(self):
        self.barrier(["sp"])
        with self.nc.Block() as block:
            @block.tensor
            def _(h):
                for f in self.prog["pe"]:
                    f(h)

            @block.scalar
            def _(h):
                for f in self.prog["act"]:
                    f(h)

            @block.vector
            def _(h):
                for f in self.prog["dve"]:
                    f(h)

            @block.gpsimd
            def _(h):
                for f in self.prog["pool"]:
                    f(h)

            @block.sync
            def _(h):
                for f in self.prog["sp"]:
                    f(h)


class Alias:
    def __init__(self, buf, ap):
        self.r = buf.r
        self.ap = ap

    def __getitem__(self, k):
        return self.ap[k]


class _Keep:
    def __init__(self, es):
        self.es = es

    def __enter__(self):
        return self.es

    def __exit__(self, *a):
        return False


def bc(ap, shape):
    return ap.to_broadcast(list(shape))


class KB:
    def __init__(self, cfg, dbg=(), stages=None):
        self.cfg = cfg
        self.dbg = set(dbg)
        self.stages = stages
        self.nc = nc = bass.Bass("TRN2", target_bir_lowering=False)
        self.fw = FW(nc)
        self.inp = {}
        self.scr = {}
        self.P = nc.alloc_psum_tensor("P", [128, 4096], F32)
        self.pr = [Res(f"bank{i}") for i in range(8)]
        self.pptrs = {}
        self.uid = 0
        self.stop_at = float(os.environ.get('KSTOP', '999'))

    def din(self, name, shape):
        self.inp[name] = self.nc.dram_tensor(name, list(shape), F32, kind="ExternalInput").ap()
        return self.inp[name]

    def dscr(self, name, shape):
        kind = "ExternalOutput" if name in self.dbg else "Internal"
        self.scr[name] = self.nc.dram_tensor(name, list(shape), F32, kind=kind).ap()
        return self.scr[name]

    def sb(self, es, name, shape):
        self.uid += 1
        t = es.enter_context(self.nc.sbuf_tensor(f"{name}_{self.uid}", list(shape), F32))
        return Buf(t, name)

    def pb(self, n=1):
        sid = getattr(threading.current_thread(), "baton_id", None)
        lo, hi = (0, 8) if sid is None else ((0, 4), (4, 8))[sid]
        key = sid if sid is not None else -1
        ptr = self.pptrs.get(key, lo)
        if ptr < lo or ptr >= hi:
            ptr = lo
        if (ptr - lo) % n:
            ptr += n - (ptr - lo) % n
        if ptr + n > hi:
            ptr = lo
        b0 = ptr
        self.pptrs[key] = ptr + n
        return self.P[:, b0 * 512:(b0 + n) * 512], self.pr[b0:b0 + n]

    def cst(self, i):
        return self.consts[:, i, :]

    def mm(self, out, lhsT, rhs, reads, writes, start=True, stop=True, fast=False):
        if fast and FAST_DENSE:
            lhsT, rhs = lhsT.bitcast(F32R), rhs.bitcast(F32R)
        self.fw.op("pe", lambda h: h.matmul(out, lhsT=lhsT, rhs=rhs, start=start, stop=stop),
                   reads=reads, writes=writes)

    def tr(self, out, in_, reads, writes):
        n = in_.shape[0]
        ident = self.consts[0:n, C_IDENT, 0:n]
        self.fw.op("pe", lambda h: h.transpose(out=out, in_=in_, identity=ident),
                   reads=list(reads) + [self.consts.r], writes=writes)

    def act(self, out, in_, func, reads, writes, bias=None, scale=None):
        kw = {}
        if bias is not None:
            kw["bias"] = bias
        if scale is not None:
            kw["scale"] = scale
        self.fw.op("act", lambda h: h.activation(out=out, in_=in_, func=func, **kw), reads=reads, writes=writes)

    def tt(self, eng, out, in0, in1, op, reads, writes):
        self.fw.op(eng, lambda h: h.tensor_tensor(out=out, in0=in0, in1=in1, op=op), reads=reads, writes=writes)

    def ts(self, eng, out, in0, s1, op0, reads, writes, s2=None, op1=None):
        if op1 is None:
            self.fw.op(eng, lambda h: h.tensor_scalar(out=out, in0=in0, scalar1=s1, scalar2=None, op0=op0),
                       reads=reads, writes=writes)
        else:
            self.fw.op(eng, lambda h: h.tensor_scalar(out=out, in0=in0, scalar1=s1, scalar2=s2, op0=op0, op1=op1),
                       reads=reads, writes=writes)

    def stt(self, out, in0, scalar, in1, op0, op1, reads, writes):
        self.fw.op("dve", lambda h: h.scalar_tensor_tensor(out=out, in0=in0, scalar=scalar, in1=in1,
                                                           op0=op0, op1=op1), reads=reads, writes=writes)

    def dump(self, name, buf, ap=None):
        if name not in self.dbg or name in self.scr:
            return
        ap = buf[:] if ap is None else ap
        t = self.nc.dram_tensor(name, list(ap.shape), F32, kind="ExternalOutput").ap()
        self.scr[name] = t
        self.fw.dma(t, ap, reads=[buf.r])

    def ck(self, n):
        return n >= self.stop_at

    def run_interleaved(self, fns):
        n = len(fns)
        cv = threading.Condition()
        state = {"turn": 0, "alive": [True] * n, "err": None}

        def pass_baton(i):
            for j in range(1, n + 1):
                k = (i + j) % n
                if state["alive"][k]:
                    state["turn"] = k
                    break
            cv.notify_all()

        def hook():
            i = threading.current_thread().baton_id
            with cv:
                pass_baton(i)
                while state["turn"] != i:
                    cv.wait()

        def worker(i):
            with cv:
                while state["turn"] != i:
                    cv.wait()
            try:
                fns[i]()
            except BaseException as e:
                state["err"] = e
            with cv:
                state["alive"][i] = False
                if any(state["alive"]):
                    pass_baton(i)

        self.fw.yield_hook = hook
        ths = []
        for i in range(n):
            t = threading.Thread(target=worker, args=(i,))
            t.baton_id = i
            ths.append(t)
        for t in ths:
            t.start()
        for t in ths:
            t.join()
        self.fw.yield_hook = None
        if state["err"] is not None:
            raise state["err"]

    def on(self, name):
        return self.stages is None or name in self.stages

    def build(self):
        cfg, nc, fw = self.cfg, self.nc, self.fw
        T, L, DEPTH = cfg.T, cfg.L, cfg.DEPTH
        self.din("x", [L, D])
        self.din("ctx", [CTX, D])
        self.din("cvec", [128, KC, 2])
        self.din("w_ada", [DEPTH, D, 3 * D])
        self.din("b_ada_c", [128, DEPTH, 24])
        self.din("norm_g_c", [128, DEPTH, KC])
        self.din("w_in", [DEPTH, D, 14384])
        self.din("consts", [128, 9, 128])
        self.din("final_norm_g", [1, D])
        self.din("ml_small", [DEPTH, 272])
        self.din("gdn_small", [DEPTH, 160])
        self.din("gdn_conv_c", [128, DEPTH, 5, 24])
        self.din("s5_lam", [128, DEPTH, 32, 2, 2])
        self.din("s5_lstep", [128, DEPTH, 32, 2])
        self.din("s5_Bblk", [32, DEPTH, 32, 2, 128])
        self.din("s5_Cblk", [128, DEPTH, 32, 2, 32])
        self.din("s5_d_q", [32, DEPTH, 32])
        self.din("b_glu_c", [128, DEPTH, 16])
        self.din("s5_w_glu", [DEPTH, D, 2 * D])
        self.din("w_branch", [DEPTH, 3, D, D])
        self.din("w_out", [DEPTH, D, D])
        self.out = nc.dram_tensor("y", [L, D], F32, kind="ExternalOutput").ap()
        self.dscr("XT", [KC, 128, T])
        for name, c0, n, f in SEGS:
            self.dscr(name, [n, T])
        for name in ("YA", "YB", "YC", "OUTT"):
            self.dscr(name, [D, T])
        self.dscr("HF", [T, D])
        self.dscr("HF2", [T, D])
        self.dscr("Y5", [D, T])

        with ExitStack() as es:
            self.consts = self.sb(es, "consts", [128, 9, 128])
            fw.dma(self.consts[:], self.inp["consts"], writes=[self.consts.r])
            self.mod = self.sb(es, "mod", [128, DEPTH, 24, 2])
            self.gs = self.sb(es, "gs", [128, DEPTH, KC, 2])
            if self.on("xin"):
                self.stage_xin()
                fw.barrier()
            if self.on("mods"):
                self.stage_mods()
                fw.barrier()
            for l in range(DEPTH):
                with ExitStack() as esl:
                    if self.on("norm"):
                        hT = self.sb(esl, "hT", [128, KC, T])
                        self.stage_norm(l, hT)
                        fw.barrier()
                        if self.on("inproj"):
                            self.stage_inproj(l, hT)
                            fw.barrier()
                if self.on("mlstm") and self.on("gdn") and INTERLEAVE:
                    with ExitStack() as esm:
                        self.run_interleaved([lambda: self.stage_gdn(l, esm), lambda: self.stage_mlstm(l, esm)])
                    fw.barrier()
                else:
                    if self.on("mlstm"):
                        self.stage_mlstm(l)
                        fw.barrier()
                    if self.on("gdn"):
                        self.stage_gdn(l)
                        fw.barrier()
                if self.on("s5"):
                    self.stage_s5(l)
                    fw.barrier()
                    self.stage_s5post(l)
                    fw.barrier()
                if self.on("merge"):
                    self.stage_merge(l)
                    fw.barrier()
            if self.on("final"):
                self.stage_final()
        fw.emit()
        return nc

    def stage_xin(self):
        cfg, fw = self.cfg, self.fw
        XTv = self.scr["XT"].rearrange("k p t -> p k t")
        with ExitStack() as es:
            xt = [self.sb(es, f"xin{i}", [128, D]) for i in range(2)]
            xo = [self.sb(es, f"xo{i}", [128, KC, 128]) for i in range(2)]
            for tl in range(cfg.NT):
                b = tl % 2
                src = (self.inp["ctx"][tl * 128:(tl + 1) * 128, :] if tl < 2
                       else self.inp["x"][(tl - 2) * 128:(tl - 1) * 128, :])
                fw.dma(xt[b][:], src, writes=[xt[b].r])
                pv, prs = self.pb(2)
                pv3 = pv.rearrange("p (k t) -> p k t", t=128)
                for k in range(KC):
                    self.tr(pv3[:, k, :], xt[b][:, k * 128:(k + 1) * 128], [xt[b].r], prs)
                self.act(xo[b][:], pv3, AF.Copy, prs, [xo[b].r])
                fw.dma(XTv[:, :, tl * 128:(tl + 1) * 128], xo[b][:], reads=[xo[b].r])

    def stage_mods(self):
        cfg, fw = self.cfg, self.fw
        with ExitStack() as es:
            sc = self.sb(es, "sc", [128, KC, 2])
            bada = self.sb(es, "bada", [128, cfg.DEPTH, 24])
            ng = self.sb(es, "ng", [128, cfg.DEPTH, KC])
            wt = [self.sb(es, f"wada{i}", [128, KC, 512]) for i in range(2)]
            fw.dma(sc[:], self.inp["cvec"], writes=[sc.r])
            fw.dma(bada[:], self.inp["b_ada_c"], writes=[bada.r])
            fw.dma(ng[:], self.inp["norm_g_c"], writes=[ng.r])
            self.act(sc[:], sc[:], AF.Silu, [sc.r], [sc.r])
            it = 0
            for l in range(cfg.DEPTH):
                pv, prs = self.pb(1)
                pm = pv[:, 0:48].rearrange("p (c v) -> p c v", v=2)
                wv = self.inp["w_ada"][l].rearrange("(k p) n -> p k n", p=128)
                for cg in range(6):
                    b = it % 2
                    it += 1
                    fw.dma(wt[b][:], wv[:, :, cg * 512:(cg + 1) * 512], writes=[wt[b].r])
                    for j in range(4):
                        for k in range(KC):
                            self.mm(pm[:, cg * 4 + j, :], wt[b][:, k, j * 128:(j + 1) * 128], sc[:, k, :],
                                    [wt[b].r, sc.r], prs, start=(k == 0), stop=(k == KC - 1))
                self.tt("dve", self.mod[:, l], pm, bc(bada[:, l].unsqueeze(2), [128, 24, 2]), ALU.add,
                        prs + [bada.r], [self.mod.r])
                self.ts("dve", self.gs[:, l], self.mod[:, l, 8:16, :], 1.0, ALU.add, [self.mod.r], [self.gs.r])
                self.tt("dve", self.gs[:, l], self.gs[:, l], bc(ng[:, l].unsqueeze(2), [128, KC, 2]), ALU.mult,
                        [self.gs.r, ng.r], [self.gs.r])

    def stage_norm(self, l, hT):
        cfg, fw = self.cfg, self.fw
        XTv = self.scr["XT"].rearrange("k p t -> p k t")
        NG = 256
        with ExitStack() as es:
            xg = [self.sb(es, f"xg{i}", [128, KC, NG]) for i in range(2)]
            sq = self.sb(es, "sq", [128, KC, NG])
            rs = self.sb(es, "rs", [128, NG])
            for gi, s0 in enumerate(range(0, cfg.T, NG)):
                v = 1 if s0 < CTX else 0
                b = gi % 2
                fw.dma(xg[b][:], XTv[:, :, s0:s0 + NG], writes=[xg[b].r])
                self.act(sq[:], xg[b][:], AF.Square, [xg[b].r], [sq.r])
                pv, prs = self.pb(1)
                for k in range(KC):
                    self.mm(pv[:, 0:NG], self.cst(C_ONES), sq[:, k, :], [sq.r, self.consts.r], prs,
                            start=(k == 0), stop=(k == KC - 1))
                self.act(rs[:], pv[:, 0:NG], AF.Sqrt, prs, [rs.r], bias=EPS, scale=1.0 / D)
                self.fw.op("dve", lambda h: h.reciprocal(out=rs[:], in_=rs[:]), reads=[rs.r], writes=[rs.r])
                self.tt("dve", xg[b][:], xg[b][:], bc(rs[:].unsqueeze(1), [128, KC, NG]), ALU.mult,
                        [xg[b].r, rs.r], [xg[b].r])
                for k in range(KC):
                    self.act(hT[:, k, s0:s0 + NG], xg[b][:, k, :], AF.Identity, [xg[b].r, self.gs.r, self.mod.r],
                             [hT.r], bias=self.mod[:, l, k, v:v + 1], scale=self.gs[:, l, k, v:v + 1])

    def hview(self, hT, l, k, gi):
        cfg = self.cfg
        s0, n = cfg.groups[gi]
        if gi == 0 or l % 2 == 0:
            return hT[:, k, s0:s0 + n]
        c0 = (s0 - CTX) // cfg.ROWS
        c1 = c0 + n // cfg.ROWS
        return hT[:, k, CTX:].rearrange("p (r c) -> p c r", c=cfg.GW)[:, c0:c1, :]

    def pview(self, ap2d, l, gi):
        cfg = self.cfg
        if gi == 0 or l % 2 == 0:
            return ap2d
        return ap2d.rearrange("p (c r) -> p c r", r=cfg.ROWS)

    def stage_inproj(self, l, hT):
        cfg, fw = self.cfg, self.fw
        wv = self.inp["w_in"][l].rearrange("(k p) n -> p k n", p=128)
        funcs = {"copy": AF.Copy, "silu": AF.Silu, "sigmoid": AF.Sigmoid}
        with ExitStack() as es:
            wt = [self.sb(es, f"win{i}", [128, KC, 128]) for i in range(2)]
            stg = [self.sb(es, f"stg{i}", [128, cfg.T]) for i in range(2)]
            it = 0
            for name, col0, ncols, fname in SEGS:
                for c0 in range(0, ncols, 128):
                    nn = min(128, ncols - c0)
                    b = it % 2
                    it += 1
                    fw.dma(wt[b][:, :, 0:nn], wv[:, :, col0 + c0:col0 + c0 + nn], writes=[wt[b].r])
                    for gi, (s0, n) in enumerate(cfg.groups):
                        pv, prs = self.pb(1)
                        for k in range(KC):
                            self.mm(self.pview(pv[0:nn, 0:n], l, gi), wt[b][:, k, 0:nn], self.hview(hT, l, k, gi),
                                    [wt[b].r, hT.r], prs, start=(k == 0), stop=(k == KC - 1), fast=(nn == 128))
                        self.act(stg[b][0:nn, s0:s0 + n], pv[0:nn, 0:n], funcs[fname], prs, [stg[b].r])
                    fw.dma(self.scr[name][c0:c0 + nn, :], stg[b][0:nn, :], reads=[stg[b].r])


    def tile_order(self, d):
        NT = self.cfg.NT
        return [0, 1] + list(range(2, NT)) if d == 0 else [1, 0] + list(range(NT - 1, 1, -1))

    def stage_mlstm(self, l, es_in=None):
        cfg, fw = self.cfg, self.fw
        H = 4
        fm = lambda name: self.scr[name].rearrange("(c p) t -> p c t", p=128)
        MQ, MK, MV, MO, MZ, YC = fm("MQ"), fm("MK"), fm("MV"), fm("MO"), fm("MZ"), fm("YC")
        IFs, HF = self.scr["IF"], self.scr["HF2"]
        hfres = [Res(f"hf2_{i}") for i in range(cfg.NT)]
        cr = self.consts.r
        with (ExitStack() if es_in is None else _Keep(es_in)) as es:
            sm = self.sb(es, "mlsm", [128, 272])
            fw.dma(sm[:], bc(self.inp["ml_small"][l:l + 1, :], [128, 272]), writes=[sm.r])
            qT = self.sb(es, "qT", [128, 8, 128])
            kT = self.sb(es, "kT", [128, 8, 128])
            vT = self.sb(es, "vT", [128, 8, 128])
            ktok = self.sb(es, "ktok", [128, 8, 128])
            vtok = self.sb(es, "vtok", [128, H, 257])
            ifT = self.sb(es, "ifT", [16, 128])
            ift = self.sb(es, "ift", [128, 16])
            gt = self.sb(es, "gt", [128, 24])
            CT = self.sb(es, "CT", [128, 8, 257])
            GL = self.sb(es, "GL", [128, H, 128])
            M2 = self.sb(es, "M2", [128, H, 128])
            DT = self.sb(es, "DT", [128, H, 128])
            EB = self.sb(es, "EB", [128, H, 128])
            WT = self.sb(es, "WT", [128, H, 128])
            qd = self.sb(es, "qd", [128, 8, 128])
            kw = self.sb(es, "kw", [128, 8, 128])
            cols = self.sb(es, "cols", [128, 4, H])
            hd = self.sb(es, "hd", [128, H, 256])
            hr = self.sb(es, "hr", [128, H, 257])
            hf = Alias(qd, qd[:].rearrange("p (h c) t -> p h (c t)", c=2))
            sq = self.sb(es, "msq", [128, H, 256])
            zs, oT, yo = kT, qT, vT
            fw.op("pool", lambda h: h.memset(vtok[:], 1.0), writes=[vtok.r])
            for d in range(2):
                Cm, NM, last = (C_UP, C_NUI, 127) if d == 0 else (C_LO, C_NLI, 0)
                fw.op("pool", lambda h: h.memset(CT[:], 0.0), writes=[CT.r])
                for tl in self.tile_order(d):
                    t0 = tl * 128
                    tsl = slice(t0, t0 + 128)
                    fw.dma(qT[:], MQ[:, :, tsl], writes=[qT.r])
                    fw.dma(kT[:], MK[:, :, tsl], writes=[kT.r])
                    fw.dma(vT[:], MV[:, :, tsl], writes=[vT.r])
                    fw.dma(ifT[:], IFs[:, tsl], writes=[ifT.r])
                    self.ts("pool", kT[:], kT[:], 0.0625, ALU.mult, [kT.r], [kT.r])
                    if self.ck(1):
                        return
                    pv, prs = self.pb(2)
                    pv3 = pv.rearrange("p (c t) -> p c t", t=128)
                    for c in range(8):
                        self.tr(pv3[:, c, :], kT[:, c, :], [kT.r], prs)
                    self.act(ktok[:], pv3, AF.Copy, prs, [ktok.r])
                    pv, prs = self.pb(2)
                    pv3 = pv.rearrange("p (c t) -> p c t", t=128)
                    for c in range(8):
                        self.tr(pv3[:, c, :], vT[:, c, :], [vT.r], prs)
                    self.act(vtok[:, :, 0:256].rearrange("p h (c e) -> p h c e", e=128),
                             pv.rearrange("p (h c e) -> p h c e", c=2, e=128), AF.Copy, prs, [vtok.r])
                    if self.ck(2):
                        return
                    pv, prs = self.pb(1)
                    self.tr(pv[:, 0:16], ifT[:], [ifT.r], prs)
                    self.act(ift[:], pv[:, 0:16], AF.Copy, prs, [ift.r])
                    if self.ck(2.2):
                        return
                    self.tt("dve", gt[:, 0:8], ift[:, 0:8], sm[:, 0:8], ALU.add, [ift.r, sm.r], [gt.r])
                    self.tt("dve", gt[:, 16:24], ift[:, 8:16], sm[:, 8:16], ALU.add, [ift.r, sm.r], [gt.r])
                    if self.ck(2.4):
                        return
                    self.act(gt[:, 16:24], gt[:, 16:24], AF.Exp, [gt.r], [gt.r], scale=-1.0)
                    if self.ck(2.6):
                        return
                    self.act(gt[:, 16:24], gt[:, 16:24], AF.Ln, [gt.r], [gt.r], bias=1.0)
                    self.ts("dve", gt[:, 8:16], gt[:, 16:24], -1.0, ALU.mult, [gt.r], [gt.r])
                    if self.ck(3):
                        return
                    ig = gt[:, d * 4:d * 4 + 4]
                    lf = gt[:, 8 + d * 4:8 + d * 4 + 4]
                    colA, colE, den, rc = cols[:, 0, :], cols[:, 1, :], cols[:, 2, :], cols[:, 3, :]
                    self.tt("dve", GL[:], bc(self.cst(Cm).unsqueeze(1), [128, H, 128]),
                            bc(lf.unsqueeze(2), [128, H, 128]), ALU.mult, [gt.r, cr], [GL.r])
                    if self.ck(3.1):
                        return
                    pv, prs = self.pb(1)
                    self.mm(pv[:, 0:H], self.cst(Cm), lf, [gt.r, cr], prs)
                    self.tt("dve", colA, ig, pv[:, 0:H], ALU.subtract, prs + [gt.r], [cols.r])
                    if self.ck(3.2):
                        return
                    self.tt("dve", M2[:], bc(self.cst(NM).unsqueeze(1), [128, H, 128]),
                            bc(colA.unsqueeze(2), [128, H, 128]), ALU.add, [cols.r, cr], [M2.r])
                    if self.ck(3.3):
                        return
                    pv, prs = self.pb(1)
                    self.mm(pv, self.cst(C_ONES), GL[:].rearrange("p h y -> p (h y)"), [GL.r, cr], prs, True, False)
                    self.mm(pv, self.cst(C_IDENT), M2[:].rearrange("p h y -> p (h y)"), [M2.r, cr], prs, False, True)
                    self.act(DT[:].rearrange("p h y -> p (h y)"), pv, AF.Exp, prs, [DT.r])
                    if self.ck(3.4):
                        return
                    pv, prs = self.pb(1)
                    pe3 = pv.rearrange("p (h y) -> p h y", y=128)
                    self.mm(pv, self.cst(C_ONES), GL[:].rearrange("p h y -> p (h y)"), [GL.r, cr], prs)
                    self.act(EB[:].rearrange("p h y -> p (h y)"), pv, AF.Exp, prs, [EB.r])
                    if self.ck(3.5):
                        return
                    self.act(colE, colA, AF.Exp, [cols.r], [cols.r])
                    self.tt("dve", colE, colE, EB[:, :, last], ALU.mult, [cols.r, EB.r], [cols.r])
                    if self.ck(4):
                        return
                    pv, prs = self.pb(1)
                    ps3 = pv.rearrange("p (h y) -> p h y", y=128)
                    for h_ in range(H):
                        for kc in range(2):
                            c = 2 * h_ + kc
                            self.mm(ps3[:, h_, :], kT[:, c, :], qT[:, c, :], [kT.r, qT.r], prs, kc == 0, kc == 1)
                    self.tt("dve", WT[:], ps3, DT[:], ALU.mult, prs + [DT.r], [WT.r])
                    self.tt("dve", qd[:].rearrange("p (h c) t -> p h c t", c=2),
                            qT[:].rearrange("p (h c) t -> p h c t", c=2),
                            bc(EB[:].unsqueeze(2), [128, H, 2, 128]), ALU.mult, [qT.r, EB.r], [qd.r])
                    self.tt("pool", kw[:].rearrange("p (h c) t -> p h c t", c=2),
                            ktok[:].rearrange("p (h c) t -> p h c t", c=2),
                            bc(colE.unsqueeze(2).unsqueeze(3), [128, H, 2, 128]), ALU.mult, [ktok.r, cols.r], [kw.r])
                    if self.ck(5):
                        return
                    pv, prs = self.pb(4)
                    pn = pv.rearrange("p (h y) -> p h y", y=512)
                    for h_ in range(H):
                        self.mm(pn[:, h_, 0:257], WT[:, h_, :], vtok[:, h_, :], [WT.r, vtok.r], prs, True, False)
                        for kc in range(2):
                            c = 2 * h_ + kc
                            self.mm(pn[:, h_, 0:257], qd[:, c, :], CT[:, c, :], [qd.r, CT.r], prs, False, kc == 1)
                    self.act(hr[:], pn[:, :, 0:257], AF.Copy, prs, [hr.r])
                    self.act(den, hr[:, :, 256], AF.Abs, [hr.r], [cols.r])
                    self.ts("dve", den, den, 1.0, ALU.max, [cols.r], [cols.r])
                    fw.op("dve", lambda h: h.reciprocal(out=rc, in_=den), reads=[cols.r], writes=[cols.r])
                    self.tt("dve", hd[:], hr[:, :, 0:256], bc(rc.unsqueeze(2), [128, H, 256]), ALU.mult,
                            [hr.r, cols.r], [hd.r])
                    if self.ck(6):
                        return
                    for nm, bf in (("d_gt", gt), ("d_cols", cols), ("d_DT", DT), ("d_EB", EB), ("d_WT", WT),
                                   ("d_hr", hr), ("d_GL", GL), ("d_M2", M2), ("d_vtok", vtok), ("d_qd", qd)):
                        self.dump(nm, bf)
                    bend = EB[:, :, last]
                    for kc in range(2):
                        pv, prs = self.pb(4)
                        pc = pv.rearrange("p (h y) -> p h y", y=512)
                        for h_ in range(H):
                            self.mm(pc[:, h_, 0:257], kw[:, 2 * h_ + kc, :], vtok[:, h_, :], [kw.r, vtok.r], prs)
                        cv = CT[:].rearrange("p (h c) e -> p h c e", c=2)[:, :, kc, :]
                        self.tt("dve", cv, cv, bc(bend.unsqueeze(2), [128, H, 257]), ALU.mult, [CT.r, EB.r], [CT.r])
                        self.tt("dve", cv, cv, pc[:, :, 0:257], ALU.add, prs + [CT.r], [CT.r])
                    if self.ck(7):
                        return
                    if d == 0:
                        fw.dma(HF[tsl, :], hd[:].rearrange("p h e -> p (h e)"), reads=[hd.r], writes=[hfres[tl]])
                        continue
                    fw.dma(hf[:].rearrange("p h e -> p (h e)"), HF[tsl, :], reads=[hfres[tl]], writes=[hf.r])
                    fw.dma(oT[:], MO[:, :, tsl], writes=[oT.r])
                    fw.dma(zs[:], MZ[:, :, tsl], writes=[zs.r])
                    self.tt("dve", hd[:], hd[:], hf[:], ALU.add, [hd.r, hf.r], [hd.r])
                    pv, prs = self.pb(2)
                    for c in range(8):
                        self.tr(pv[:, c * 128:(c + 1) * 128], oT[:, c, :], [oT.r], prs)
                    self.tt("dve", hd[:].rearrange("p h e -> p (h e)"), hd[:].rearrange("p h e -> p (h e)"), pv,
                            ALU.mult, prs + [hd.r], [hd.r])
                    self.act(sq[:], hd[:], AF.Square, [hd.r], [sq.r])
                    fw.op("dve", lambda h: h.tensor_reduce(out=den, in_=sq[:], axis=AX.X, op=ALU.add),
                          reads=[sq.r], writes=[cols.r])
                    self.act(den, den, AF.Sqrt, [cols.r], [cols.r], bias=EPS, scale=1.0 / 256)
                    fw.op("dve", lambda h: h.reciprocal(out=rc, in_=den), reads=[cols.r], writes=[cols.r])
                    self.tt("dve", hd[:], hd[:], bc(rc.unsqueeze(2), [128, H, 256]), ALU.mult, [hd.r, cols.r], [hd.r])
                    self.tt("pool", hd[:], hd[:], bc(sm[:, 16:272].unsqueeze(1), [128, H, 256]), ALU.mult,
                            [hd.r, sm.r], [hd.r])
                    pv, prs = self.pb(2)
                    pv3 = pv.rearrange("p (c t) -> p c t", t=128)
                    hd2 = hd[:].rearrange("p h e -> p (h e)")
                    for c in range(8):
                        self.tr(pv3[:, c, :], hd2[:, c * 128:(c + 1) * 128], [hd.r], prs)
                    self.tt("dve", yo[:], pv3, zs[:], ALU.mult, prs + [zs.r], [yo.r])
                    fw.dma(YC[:, :, tsl], yo[:], reads=[yo.r])


    def stage_gdn(self, l, es_in=None):
        cfg, fw = self.cfg, self.fw
        H = 8
        fm = lambda name: self.scr[name].rearrange("(c p) t -> p c t", p=128)
        QKV, ZG, YA = fm("QKV"), fm("ZG"), fm("YA")
        ABs, HF = self.scr["AB"], self.scr["HF"]
        hfres = [Res(f"hf_{i}") for i in range(cfg.NT)]
        cr = self.consts.r
        B3 = [128, H, 128]
        flat = lambda ap: ap.rearrange("p h y -> p (h y)")
        with (ExitStack() if es_in is None else _Keep(es_in)) as es:
            sm = self.sb(es, "gsm", [128, 160])
            cw = self.sb(es, "cw", [128, 5, 24])
            fw.dma(sm[:], bc(self.inp["gdn_small"][l:l + 1, :], [128, 160]), writes=[sm.r])
            fw.dma(cw[:], self.inp["gdn_conv_c"][:, l], writes=[cw.r])
            self.act(sm[:, 0:16], sm[:, 0:16], AF.Exp, [sm.r], [sm.r])
            self.ts("dve", sm[:, 0:16], sm[:, 0:16], -1.0, ALU.mult, [sm.r], [sm.r])
            buf = self.sb(es, "buf", [128, 24, 132])
            acc = self.sb(es, "acc", [128, 24, 128])
            tmp = self.sb(es, "tmp", [128, 24, 128])
            sq = self.sb(es, "gsq", [128, 16, 128])
            rn = self.sb(es, "grn", [128, 16, 128])
            ktok = self.sb(es, "gktok", B3)
            vtok = self.sb(es, "gvtok", B3)
            abT = self.sb(es, "abT", [32, 128])
            gt = self.sb(es, "ggt", [128, 4, 16])
            cl = self.sb(es, "gcl", [128, 6, H])
            GL = self.sb(es, "gGL", B3)
            GLb = self.sb(es, "gGLb", B3)
            PQ = [self.sb(es, f"gPQ{i}", [128, 2, H, 128]) for i in range(2)]
            TT = self.sb(es, "gTT", B3)
            QKD = self.sb(es, "gQKD", B3)
            kend = self.sb(es, "gkend", B3)
            kbg = self.sb(es, "gkbg", B3)
            vb = self.sb(es, "gvb", B3)
            qdec = self.sb(es, "gqdec", B3)
            nwT = self.sb(es, "gnwT", B3)
            vnew = self.sb(es, "gvnew", B3)
            ob = self.sb(es, "gob", B3)
            hf = kbg
            S = self.sb(es, "gS", B3)
            zs, yo = vb, kend
            M2 = [tmp[:, 0:8, :], tmp[:, 8:16, :], tmp[:, 16:24, :]]
            DTi, Q0D, P0D, EG = sq[:, 0:8, :], sq[:, 8:16, :], rn[:, 0:8, :], rn[:, 8:16, :]
            qT, kT, vT = acc[:, 0:8, :], acc[:, 8:16, :], acc[:, 16:24, :]
            gall, lnball, betall, gtmp = gt[:, 0, :], gt[:, 1, :], gt[:, 2, :], gt[:, 3, :]
            gcc, col2, glast, colE, col3, gend = (cl[:, i, :] for i in range(6))

            def decay(dst, dst_r, lhs_c, GLx, M2x):
                pv, prs = self.pb(2)
                for half in range(2):
                    o = pv[:, half * 512:(half + 1) * 512]
                    self.mm(o, self.cst(lhs_c), flat(GLx[:, 4 * half:4 * half + 4, :]), [GLx_r(GLx), cr], prs,
                            True, M2x is None)
                    if M2x is not None:
                        self.mm(o, self.cst(C_IDENT), flat(M2x[:, 4 * half:4 * half + 4, :]), [tmp.r, cr], prs,
                                False, True)
                self.act(flat(dst), pv, AF.Exp, prs, [dst_r])

            def GLx_r(g):
                return GL.r if g is GLt else GLb.r
            GLt, GLbt = GL[:], GLb[:]

            for d in range(2):
                if d == 0:
                    Cm, NDTi, NDTs, NDs = C_UP, C_NUI, C_NUS, C_NLS
                else:
                    Cm, NDTi, NDTs, NDs = C_LO, C_NLI, C_NLS, C_NUS
                fw.op("pool", lambda h: h.memset(S[:], 0.0), writes=[S.r])
                for tl in self.tile_order(d):
                    t0 = tl * 128
                    tsl = slice(t0, t0 + 128)
                    seg_lo, seg_hi = (0, CTX) if tl < 2 else (CTX, cfg.T)
                    lo, hi = max(t0 - 2, seg_lo), min(t0 + 130, seg_hi)
                    if lo > t0 - 2:
                        fw.op("pool", lambda h: h.memset(buf[:, :, 0:2], 0.0), writes=[buf.r])
                    if hi < t0 + 130:
                        fw.op("pool", lambda h: h.memset(buf[:, :, 130:132], 0.0), writes=[buf.r])
                    for i in range(3):
                        fw.dma(buf[:, 8 * i:8 * i + 8, lo - (t0 - 2):hi - (t0 - 2)], QKV[:, 8 * i:8 * i + 8, lo:hi],
                               writes=[buf.r])
                    fw.dma(abT[:], ABs[:, tsl], writes=[abT.r])
                    for j in range(5):
                        wj = bc(cw[:, j, :].unsqueeze(2), [128, 24, 128])
                        if j == 0:
                            self.tt("dve", acc[:], buf[:, :, 0:128], wj, ALU.mult, [buf.r, cw.r], [acc.r])
                        else:
                            self.tt("pool", tmp[:], buf[:, :, j:j + 128], wj, ALU.mult, [buf.r, cw.r], [tmp.r])
                            self.tt("dve", acc[:], acc[:], tmp[:], ALU.add, [acc.r, tmp.r], [acc.r])
                    self.act(acc[:], acc[:], AF.Silu, [acc.r], [acc.r])
                    self.act(sq[:], acc[:, 0:16, :], AF.Square, [acc.r], [sq.r])
                    pv, prs = self.pb(4)
                    for i in range(4):
                        self.mm(pv[:, i * 512:(i + 1) * 512], self.cst(C_ONES), flat(sq[:, 4 * i:4 * i + 4, :]),
                                [sq.r, cr], prs)
                    self.act(flat(rn[:, 0:8, :]), pv[:, 0:1024], AF.Sqrt, prs, [rn.r], bias=128.0 * EPS, scale=128.0)
                    self.act(flat(rn[:, 8:16, :]), pv[:, 1024:2048], AF.Sqrt, prs, [rn.r], bias=EPS, scale=1.0)
                    fw.op("dve", lambda h: h.reciprocal(out=rn[:], in_=rn[:]), reads=[rn.r], writes=[rn.r])
                    self.tt("dve", acc[:, 0:16, :], acc[:, 0:16, :], rn[:], ALU.mult, [acc.r, rn.r], [acc.r])
                    for src, dstb in ((kT, ktok), (vT, vtok)):
                        pv, prs = self.pb(2)
                        pv3 = pv.rearrange("p (h y) -> p h y", y=128)
                        for h_ in range(H):
                            self.tr(pv3[:, h_, :], src[:, h_, :], [acc.r], prs)
                        self.act(dstb[:], pv3, AF.Copy, prs, [dstb.r])
                    pv, prs = self.pb(1)
                    self.tr(pv[:, 0:32], abT[:], [abT.r], prs)
                    self.act(gall, pv[:, 0:16], AF.Identity, prs, [gt.r])
                    self.act(gtmp, pv[:, 16:32], AF.Exp, prs, [gt.r], scale=-1.0)
                    self.tt("dve", gall, gall, sm[:, 16:32], ALU.add, [gt.r, sm.r], [gt.r])
                    self.act(gall, gall, AF.Exp, [gt.r], [gt.r])
                    self.act(gall, gall, AF.Ln, [gt.r], [gt.r], bias=1.0)
                    self.tt("dve", gall, gall, sm[:, 0:16], ALU.mult, [gt.r, sm.r], [gt.r])
                    self.act(gtmp, gtmp, AF.Ln, [gt.r], [gt.r], bias=1.0)
                    self.ts("dve", lnball, gtmp, -1.0, ALU.mult, [gt.r], [gt.r])
                    self.act(betall, lnball, AF.Exp, [gt.r], [gt.r])
                    g_d, lnb_d, beta_d = gall[:, 8 * d:8 * d + 8], lnball[:, 8 * d:8 * d + 8], betall[:, 8 * d:8 * d + 8]
                    self.tt("dve", GL[:], bc(self.cst(Cm).unsqueeze(1), B3), bc(g_d.unsqueeze(2), B3), ALU.mult,
                            [gt.r, cr], [GL.r])
                    self.tt("pool", GLb[:], bc(self.cst(C_IDENT).unsqueeze(1), B3), bc(lnb_d.unsqueeze(2), B3),
                            ALU.mult, [gt.r, cr], [GLb.r])
                    self.tt("dve", GLb[:], GLb[:], GL[:], ALU.add, [GLb.r, GL.r], [GLb.r])
                    pv, prs = self.pb(1)
                    self.mm(pv[:, 0:H], self.cst(Cm), g_d, [gt.r, cr], prs)
                    self.mm(pv[:, 8:8 + H], self.cst(C_ONES), g_d, [gt.r, cr], prs)
                    self.act(cl[:, 0, :], pv[:, 0:H], AF.Copy, prs, [cl.r])
                    self.act(cl[:, 2, :], pv[:, 8:8 + H], AF.Copy, prs, [cl.r])
                    self.tt("dve", col2, gcc, lnb_d, ALU.add, [cl.r, gt.r], [cl.r])
                    self.tt("dve", colE, glast, gcc, ALU.subtract, [cl.r], [cl.r])
                    self.act(colE, colE, AF.Exp, [cl.r], [cl.r])
                    self.act(col3, col2, AF.Exp, [cl.r], [cl.r])
                    self.act(gend, glast, AF.Exp, [cl.r], [cl.r])
                    self.tt("dve", M2[0], bc(self.cst(NDTi).unsqueeze(1), B3), bc(gcc.unsqueeze(2), B3), ALU.subtract,
                            [cl.r, cr], [tmp.r])
                    self.tt("dve", M2[1], bc(self.cst(NDTs).unsqueeze(1), B3), bc(gcc.unsqueeze(2), B3), ALU.subtract,
                            [cl.r, cr], [tmp.r])
                    self.tt("dve", M2[2], bc(self.cst(NDs).unsqueeze(1), B3), bc(col2.unsqueeze(2), B3), ALU.add,
                            [cl.r, cr], [tmp.r])
                    decay(DTi, sq.r, C_ONES, GLt, M2[0])
                    decay(Q0D, sq.r, C_ONES, GLbt, M2[1])
                    decay(P0D, rn.r, C_NEGONES, GLt, M2[2])
                    decay(EG, rn.r, C_ONES, GLt, None)
                    pv, prs = self.pb(2)
                    pk3 = pv.rearrange("p (h y) -> p h y", y=128)
                    for h_ in range(H):
                        self.mm(pk3[:, h_, :], kT[:, h_, :], kT[:, h_, :], [acc.r], prs)
                    Pc, Pn = PQ[0], PQ[1]
                    self.stt(Pc[:, 0], pk3, -1.0, P0D, ALU.mult, ALU.mult, prs + [rn.r], [Pc.r])
                    self.stt(Pc[:, 1], pk3, -1.0, Q0D, ALU.mult, ALU.mult, prs + [sq.r], [Pc.r])
                    pv, prs = self.pb(2)
                    pq3 = pv.rearrange("p (h y) -> p h y", y=128)
                    for h_ in range(H):
                        self.mm(pq3[:, h_, :], kT[:, h_, :], qT[:, h_, :], [acc.r], prs)
                    self.tt("dve", QKD[:], pq3, DTi, ALU.mult, prs + [sq.r], [QKD.r])
                    self.tt("pool", kend[:], ktok[:], bc(colE.unsqueeze(2), B3), ALU.mult, [ktok.r, cl.r], [kend.r])
                    self.tt("pool", kbg[:], ktok[:], bc(col3.unsqueeze(2), B3), ALU.mult, [ktok.r, cl.r], [kbg.r])
                    self.tt("pool", vb[:], vtok[:], bc(beta_d.unsqueeze(2), B3), ALU.mult, [vtok.r, gt.r], [vb.r])
                    self.tt("dve", qdec[:], qT, EG, ALU.mult, [acc.r, rn.r], [qdec.r])
                    self.tt("dve", TT[:], Pc[:, 1], bc(self.cst(C_IDENT).unsqueeze(1), B3), ALU.add, [Pc.r, cr], [TT.r])
                    for k in range(1, 8):
                        need_q = k <= 5
                        last_it = k == 7
                        if need_q:
                            pv1, prs1 = self.pb(2)
                            p13 = pv1.rearrange("p (h y) -> p h y", y=128)
                            for h_ in range(H):
                                self.mm(p13[:, h_, :], Pc[:, 0, h_, :], Pc[:, 1, h_, :], [Pc.r], prs1)
                            self.act(Pn[:, 1], p13, AF.Copy, prs1, [Pn.r])
                        if k >= 2:
                            pv2, prs2 = self.pb(2)
                            p23 = pv2.rearrange("p (h y) -> p h y", y=128)
                            for h_ in range(H):
                                self.mm(p23[:, h_, :], Pc[:, 0, h_, :], TT[:, h_, :], [Pc.r, TT.r], prs2)
                            self.tt("dve", TT[:], TT[:], p23, ALU.add, prs2 + [TT.r], [TT.r])
                        if not last_it:
                            pv3_, prs3 = self.pb(2)
                            p33 = pv3_.rearrange("p (h y) -> p h y", y=128)
                            for h_ in range(H):
                                self.mm(p33[:, h_, :], Pc[:, 1, h_, :], Pc[:, 0, h_, :], [Pc.r], prs3)
                            self.act(Pn[:, 0], p33, AF.Copy, prs3, [Pn.r])
                        Pc, Pn = Pn, Pc
                    pv, prs = self.pb(2)
                    pw3 = pv.rearrange("p (h y) -> p h y", y=128)
                    for h_ in range(H):
                        self.mm(pw3[:, h_, :], kbg[:, h_, :], TT[:, h_, :], [kbg.r, TT.r], prs)
                    self.act(nwT[:], pw3, AF.Identity, prs, [nwT.r], scale=-1.0)
                    pv, prs = self.pb(2)
                    pn3 = pv.rearrange("p (h y) -> p h y", y=128)
                    for h_ in range(H):
                        self.mm(pn3[:, h_, :], TT[:, h_, :], vb[:, h_, :], [TT.r, vb.r], prs, True, False)
                        self.mm(pn3[:, h_, :], nwT[:, h_, :], S[:, h_, :], [nwT.r, S.r], prs, False, True)
                    self.act(vnew[:], pn3, AF.Copy, prs, [vnew.r])
                    pv, prs = self.pb(2)
                    po3 = pv.rearrange("p (h y) -> p h y", y=128)
                    for h_ in range(H):
                        self.mm(po3[:, h_, :], qdec[:, h_, :], S[:, h_, :], [qdec.r, S.r], prs, True, False)
                        self.mm(po3[:, h_, :], QKD[:, h_, :], vnew[:, h_, :], [QKD.r, vnew.r], prs, False, True)
                    self.act(ob[:], po3, AF.Copy, prs, [ob.r])
                    pv, prs = self.pb(2)
                    ps3 = pv.rearrange("p (h y) -> p h y", y=128)
                    for h_ in range(H):
                        self.mm(ps3[:, h_, :], kend[:, h_, :], vnew[:, h_, :], [kend.r, vnew.r], prs)
                    self.tt("dve", S[:], S[:], bc(gend.unsqueeze(2), B3), ALU.mult, [S.r, cl.r], [S.r])
                    self.tt("dve", S[:], S[:], ps3, ALU.add, prs + [S.r], [S.r])
                    for nm, bf in (("g_acc", acc), ("g_gt", gt), ("g_cl", cl), ("g_sq", sq), ("g_rn", rn), ("g_TT", TT),
                                   ("g_ob", ob), ("g_vnew", vnew), ("g_QKD", QKD)):
                        self.dump(nm, bf)
                    if d == 0:
                        fw.dma(HF[tsl, :], flat(ob[:]), reads=[ob.r], writes=[hfres[tl]])
                        continue
                    fw.dma(flat(hf[:]), HF[tsl, :], reads=[hfres[tl]], writes=[hf.r])
                    fw.dma(zs[:], ZG[:, :, tsl], writes=[zs.r])
                    self.tt("dve", ob[:], ob[:], hf[:], ALU.add, [ob.r, hf.r], [ob.r])
                    self.act(vnew[:], ob[:], AF.Square, [ob.r], [vnew.r])
                    fw.op("dve", lambda h: h.tensor_reduce(out=gcc, in_=vnew[:], axis=AX.X, op=ALU.add),
                          reads=[vnew.r], writes=[cl.r])
                    self.act(gcc, gcc, AF.Sqrt, [cl.r], [cl.r], bias=EPS, scale=1.0 / 128)
                    fw.op("dve", lambda h: h.reciprocal(out=gcc, in_=gcc), reads=[cl.r], writes=[cl.r])
                    self.tt("dve", ob[:], ob[:], bc(gcc.unsqueeze(2), B3), ALU.mult, [ob.r, cl.r], [ob.r])
                    self.tt("pool", ob[:], ob[:], bc(sm[:, 32:160].unsqueeze(1), B3), ALU.mult, [ob.r, sm.r], [ob.r])
                    pv, prs = self.pb(2)
                    pv3 = pv.rearrange("p (h y) -> p h y", y=128)
                    for h_ in range(H):
                        self.tr(pv3[:, h_, :], ob[:, h_, :], [ob.r], prs)
                    self.tt("dve", yo[:], pv3, zs[:], ALU.mult, prs + [zs.r], [yo.r])
                    fw.dma(YA[:, :, tsl], yo[:], reads=[yo.r])


    def stage_s5(self, l):
        cfg, fw = self.cfg, self.fw
        T = cfg.T
        NL = (T - 1).bit_length()
        cr = self.consts.r
        with ExitStack() as es:
            lam = self.sb(es, "lam", [128, 32, 2, 2])
            stp = self.sb(es, "stp", [128, 32, 2])
            Bb = self.sb(es, "Bb", [32, 2, 128])
            Cb = self.sb(es, "Cb", [128, 32, 2, 32])
            dq = self.sb(es, "dq", [32, 32])
            fw.dma(lam[:], self.inp["s5_lam"][:, l], writes=[lam.r])
            fw.dma(stp[:], self.inp["s5_lstep"][:, l], writes=[stp.r])
            fw.dma(Cb[:], self.inp["s5_Cblk"][:, l], writes=[Cb.r])
            fw.dma(dq[:], self.inp["s5_d_q"][:, l], writes=[dq.r])
            self.ts("dve", Cb[:, :, 1, :], Cb[:, :, 1, :], -1.0, ALU.mult, [Cb.r], [Cb.r])
            UNI = self.sb(es, "UNI", [128, NL, 32, 2, 2])
            U2 = self.sb(es, "U2", [128, NL, 32, 2, 2])
            RR = self.sb(es, "RR", [128, 32, 2])
            FF = self.sb(es, "FF", [128, 32, 2, 2])
            FS = self.sb(es, "FS", [128, 32, 2, 2])
            w = [self.sb(es, f"s5w{i}", [128, 32, 2]) for i in range(8)]
            lre, lim = lam[:, :, :, 0], lam[:, :, :, 1]
            ar, th, cc, ss, t1, t2, lbr, lbi = (x[:] for x in w)
            wr = [x.r for x in w]
            self.act(stp[:], stp[:], AF.Exp, [stp.r], [stp.r])
            self.tt("dve", ar, lre, stp[:], ALU.mult, [lam.r, stp.r], [wr[0]])
            self.tt("dve", th, lim, stp[:], ALU.mult, [lam.r, stp.r], [wr[1]])
            self.act(RR[:], ar, AF.Exp, [wr[0]], [RR.r])
            hp = self.sb(es, "halfpi", [128, 1])
            fw.op("dve", lambda h: h.memset(hp[:], float(np.pi / 2)), writes=[hp.r])
            self.act(cc, th, AF.Sin, [wr[1], hp.r], [wr[2]], bias=hp[:, 0:1], scale=1.0 / 16)
            self.act(ss, th, AF.Sin, [wr[1]], [wr[3]], scale=1.0 / 16)

            def csq(a, b, ra, rb):
                self.tt("dve", t1, a, a, ALU.mult, [ra], [wr[4]])
                self.tt("dve", t2, b, b, ALU.mult, [rb], [wr[5]])
                self.stt(b, a, 2.0, b, ALU.mult, ALU.mult, [ra, rb], [rb])
                self.tt("dve", a, t1, t2, ALU.subtract, [wr[4], wr[5]], [ra])
            for _ in range(4):
                csq(cc, ss, wr[2], wr[3])
            self.tt("dve", lbr, cc, RR[:], ALU.mult, [wr[2], RR.r], [wr[6]])
            self.tt("dve", lbi, ss, RR[:], ALU.mult, [wr[3], RR.r], [wr[7]])
            self.tt("dve", t1, lre, lre, ALU.mult, [lam.r], [wr[4]])
            self.tt("dve", t2, lim, lim, ALU.mult, [lam.r], [wr[5]])
            self.tt("dve", t1, t1, t2, ALU.add, [wr[4], wr[5]], [wr[4]])
            fw.op("dve", lambda h: h.reciprocal(out=t1, in_=t1), reads=[wr[4]], writes=[wr[4]])
            self.ts("dve", ar, lbr, -1.0, ALU.add, [wr[6]], [wr[0]])
            self.tt("dve", t2, ar, lre, ALU.mult, [wr[0], lam.r], [wr[5]])
            self.tt("dve", th, lbi, lim, ALU.mult, [wr[7], lam.r], [wr[1]])
            self.tt("dve", t2, t2, th, ALU.add, [wr[5], wr[1]], [wr[5]])
            self.tt("dve", FF[:, :, :, 0], t2, t1, ALU.mult, [wr[5], wr[4]], [FF.r])
            self.tt("dve", t2, lbi, lre, ALU.mult, [wr[7], lam.r], [wr[5]])
            self.tt("dve", th, ar, lim, ALU.mult, [wr[0], lam.r], [wr[1]])
            self.tt("dve", t2, t2, th, ALU.subtract, [wr[5], wr[1]], [wr[5]])
            self.tt("dve", FF[:, :, :, 1], t2, t1, ALU.mult, [wr[5], wr[4]], [FF.r])
            self.ts("dve", FS[:, :, :, 0], FF[:, :, :, 1], -1.0, ALU.mult, [FF.r], [FS.r])
            self.act(FS[:, :, :, 1], FF[:, :, :, 1], AF.Copy, [FF.r], [FS.r])
            for k in range(NL):
                self.act(UNI[:, k, :, :, 0], cc, AF.Copy, [wr[2]], [UNI.r])
                self.act(UNI[:, k, :, :, 1], ss, AF.Copy, [wr[3]], [UNI.r])
                self.ts("dve", U2[:, k, :, :, 0], ss, -1.0, ALU.mult, [wr[3]], [U2.r])
                self.act(U2[:, k, :, :, 1], ss, AF.Copy, [wr[3]], [U2.r])
                if k < NL - 1:
                    csq(cc, ss, wr[2], wr[3])
            u32 = self.sb(es, "u32", [32, T])
            XA = self.sb(es, "XA", [128, 2, T])
            XB = self.sb(es, "XB", [128, 2, T])
            TM = self.sb(es, "TM", [128, 2, T])
            EE = self.sb(es, "EE", [128, 2, T])
            Y = self.sb(es, "Y5y", [32, T])
            yt = self.sb(es, "Y5t", [32, 512])
            segs = [(0, CTX), (CTX, T)]

            def rev(ap3, lo, hi, d):
                if d == 0:
                    return ap3[:, :, lo:hi]
                return ap3[:, :, hi - 1:lo - 1:-1] if lo > 0 else ap3[:, :, hi - 1::-1]

            for q in range(32):
                fw.dma(u32[:], self.scr["U5"][32 * q:32 * q + 32, :], writes=[u32.r])
                fw.dma(Bb[:], self.inp["s5_Bblk"][:, l, q], writes=[Bb.r])
                for d in range(2):
                    RAW = XB
                    for c0 in range(0, T, 2048):
                        n = min(2048, T - c0)
                        for ri in range(2):
                            pv, prs = self.pb(4)
                            for s0 in range(0, n, 512):
                                m = min(512, n - s0)
                                self.mm(pv[:, s0:s0 + m], Bb[:, ri, :], u32[:, c0 + s0:c0 + s0 + m], [Bb.r, u32.r], prs)
                            self.act(RAW[:, ri, c0:c0 + n], pv[:, 0:n], AF.Copy, prs, [RAW.r])
                    fre = FF[:, q, d, 0:1]
                    fs2 = FS[:, q, d, :]
                    for lo, hi in segs:
                        n = hi - lo
                        self.ts("dve", XA[:, :, lo:hi], rev(RAW[:], lo, hi, d), fre, ALU.mult, [RAW.r, FF.r], [XA.r])
                        self.tt("pool", TM[:, :, lo:hi], rev(RAW[:, ::-1, :], lo, hi, d),
                                bc(fs2.unsqueeze(2), [128, 2, n]), ALU.mult, [RAW.r, FS.r], [TM.r])
                    self.tt("dve", XA[:], XA[:], TM[:], ALU.add, [XA.r, TM.r], [XA.r])
                    self.dump("s_b", XA)
                    fw.op("pool", lambda h: h.memset(EE[:, 0, 0:1], 1.0), writes=[EE.r])
                    fw.op("pool", lambda h: h.memset(EE[:, 1, 0:1], 0.0), writes=[EE.r])
                    for k in range(NL):
                        n = 1 << k
                        m = min(n, T - n)
                        if m <= 0:
                            break
                        self.ts("dve", EE[:, :, n:n + m], EE[:, :, 0:m], UNI[:, k, q, d, 0:1], ALU.mult,
                                [EE.r, UNI.r], [EE.r])
                        self.tt("pool", TM[:, :, 0:m], EE[:, ::-1, 0:m], bc(U2[:, k, q, d, :].unsqueeze(2), [128, 2, m]),
                                ALU.mult, [EE.r, U2.r], [TM.r])
                        self.tt("dve", EE[:, :, n:n + m], EE[:, :, n:n + m], TM[:, :, 0:m], ALU.add, [EE.r, TM.r], [EE.r])
                    Ec = bc(EE[:, 0:1, :], [128, 2, T])
                    Es = bc(EE[:, 1:2, :], [128, 2, T])
                    self.dump("s_EE", EE)
                    self.tt("dve", TM[:], XA[:], Ec, ALU.mult, [XA.r, EE.r], [TM.r])
                    self.tt("pool", XB[:], XA[:, ::-1, :], Es, ALU.mult, [XA.r, EE.r], [XB.r])
                    self.tt("dve", XA[:, 0, :], TM[:, 0, :], XB[:, 0, :], ALU.add, [TM.r, XB.r], [XA.r])
                    self.tt("dve", XA[:, 1, :], TM[:, 1, :], XB[:, 1, :], ALU.subtract, [TM.r, XB.r], [XA.r])
                    self.dump("s_bt", XA)
                    self.dump("s_RR", RR)
                    self.dump("s_UNI", UNI)
                    rcoef = bc(RR[:, q, d:d + 1], [128, T])
                    for ri in range(2):
                        fw.op("dve", lambda h, ri=ri, rcoef=rcoef: h.tensor_tensor_scan(out=XB[:, ri, :], data0=rcoef, data1=XA[:, ri, :],
                                                                           initial=0.0, op0=ALU.mult, op1=ALU.add),
                              reads=[XA.r, RR.r], writes=[XB.r])
                    self.dump("s_z", XB)
                    self.tt("dve", TM[:], XB[:], Ec, ALU.mult, [XB.r, EE.r], [TM.r])
                    self.tt("pool", XA[:], XB[:, ::-1, :], Es, ALU.mult, [XB.r, EE.r], [XA.r])
                    self.tt("dve", XB[:, 0, :], TM[:, 0, :], XA[:, 0, :], ALU.subtract, [TM.r, XA.r], [XB.r])
                    self.tt("dve", XB[:, 1, :], TM[:, 1, :], XA[:, 1, :], ALU.add, [TM.r, XA.r], [XB.r])
                    cur = XB
                    self.dump("s_x", XB)
                    for lo, hi in segs:
                        for g0 in range(lo, hi, 512):
                            m = min(512, hi - g0)
                            pv, prs = self.pb(1)
                            self.mm(pv[0:32, 0:m], Cb[:, q, 0, :], cur[:, 0, g0:g0 + m], [Cb.r, cur.r], prs, True, False)
                            self.mm(pv[0:32, 0:m], Cb[:, q, 1, :], cur[:, 1, g0:g0 + m], [Cb.r, cur.r], prs, False, True)
                            if d == 0:
                                self.act(Y[:, g0:g0 + m], pv[0:32, 0:m], AF.Copy, prs, [Y.r])
                            else:
                                p_hi = hi - (g0 - lo)
                                p_lo = p_hi - m
                                self.act(yt[:, 0:m], pv[0:32, 0:m], AF.Copy, prs, [yt.r])
                                self.tt("dve", Y[:, p_lo:p_hi], Y[:, p_lo:p_hi], yt[:, m - 1::-1] if m > 0 else yt[:, 0:m],
                                        ALU.add, [Y.r, yt.r], [Y.r])
                self.stt(Y[:], u32[:], dq[:, q:q + 1], Y[:], ALU.mult, ALU.add, [u32.r, dq.r, Y.r], [Y.r])
                self.act(Y[:], Y[:], AF.Gelu, [Y.r], [Y.r])
                fw.dma(self.scr["Y5"][32 * q:32 * q + 32, :], Y[:], reads=[Y.r])

    def stage_s5post(self, l):
        cfg, fw = self.cfg, self.fw
        fm = lambda name: self.scr[name].rearrange("(c p) t -> p c t", p=128)
        Y5, Z5, YB = fm("Y5"), fm("Z5"), fm("YB")
        wv = self.inp["s5_w_glu"][l].rearrange("(k p) n -> p k n", p=128)
        with ExitStack() as es:
            bg = self.sb(es, "bglu", [128, 16])
            fw.dma(bg[:], self.inp["b_glu_c"][:, l], writes=[bg.r])
            yg = self.sb(es, "y5g", [128, KC, 512])
            wa = [self.sb(es, f"wa{i}", [128, KC, 128]) for i in range(2)]
            wb = [self.sb(es, f"wb{i}", [128, KC, 128]) for i in range(2)]
            zt = [self.sb(es, f"z5t{i}", [128, 512]) for i in range(2)]
            sg = self.sb(es, "sg5", [128, 512])
            ot = [self.sb(es, f"o5t{i}", [128, 512]) for i in range(2)]
            it = 0
            for (s0, n) in cfg.groups:
                fw.dma(yg[:, :, 0:n], Y5[:, :, s0:s0 + n], writes=[yg.r])
                for ct in range(8):
                    b = it % 2
                    it += 1
                    fw.dma(wa[b][:], wv[:, :, ct * 128:(ct + 1) * 128], writes=[wa[b].r])
                    fw.dma(wb[b][:], wv[:, :, D + ct * 128:D + (ct + 1) * 128], writes=[wb[b].r])
                    fw.dma(zt[b][:, 0:n], Z5[:, ct, s0:s0 + n], writes=[zt[b].r])
                    pa, pra = self.pb(1)
                    for k in range(KC):
                        self.mm(pa[:, 0:n], wa[b][:, k, :], yg[:, k, 0:n], [wa[b].r, yg.r], pra, k == 0, k == KC - 1, fast=True)
                    pb_, prb = self.pb(1)
                    for k in range(KC):
                        self.mm(pb_[:, 0:n], wb[b][:, k, :], yg[:, k, 0:n], [wb[b].r, yg.r], prb, k == 0, k == KC - 1, fast=True)
                    self.act(sg[:, 0:n], pb_[:, 0:n], AF.Sigmoid, prb + [bg.r], [sg.r], bias=bg[:, 8 + ct:9 + ct])
                    self.stt(ot[b][:, 0:n], pa[:, 0:n], bg[:, ct:ct + 1], sg[:, 0:n], ALU.add, ALU.mult,
                             pra + [bg.r, sg.r], [ot[b].r])
                    self.tt("dve", ot[b][:, 0:n], ot[b][:, 0:n], zt[b][:, 0:n], ALU.mult, [ot[b].r, zt[b].r], [ot[b].r])
                    fw.dma(YB[:, ct, s0:s0 + n], ot[b][:, 0:n], reads=[ot[b].r])


    def stage_merge(self, l):
        cfg, fw = self.cfg, self.fw
        T = cfg.T
        last = (l == cfg.DEPTH - 1)
        fm = lambda name: self.scr[name].rearrange("(c p) t -> p c t", p=128)
        YS = [fm("YA"), fm("YB"), fm("YC")]
        GT = self.scr["GATE"].rearrange("(i c p) t -> p i c t", i=3, p=128)
        OUTT = fm("OUTT")
        with ExitStack() as es:
            wo = self.sb(es, "wo", [128, KC, D])
            fw.dma(wo[:], self.inp["w_out"][l].rearrange("(k p) n -> p k n", p=128), writes=[wo.r])
            ys = self.sb(es, "ys", [128, 3, KC, 512])
            mg_ = self.sb(es, "mg", [128, KC, 512])
            og = self.sb(es, "og", [128, KC, 512])
            wt = [[self.sb(es, f"wbr{b}{i}", [128, KC, 128]) for i in range(3)] for b in range(2)]
            sgt = [self.sb(es, f"sgt{b}", [128, 3, 512]) for b in range(2)]
            tm = self.sb(es, "mtm", [128, 512])
            it = 0
            for gi, (s0, n) in enumerate(cfg.groups):
                if last and gi == 0:
                    continue
                for i in range(3):
                    fw.dma(ys[:, i, :, 0:n], YS[i][:, :, s0:s0 + n], writes=[ys.r])
                for dc in range(KC):
                    b = it % 2
                    it += 1
                    for i in range(3):
                        fw.dma(wt[b][i][:], self.inp["w_branch"][l, i].rearrange("(k p) n -> p k n", p=128)
                               [:, :, dc * 128:(dc + 1) * 128], writes=[wt[b][i].r])
                    fw.dma(sgt[b][:, :, 0:n], GT[:, :, dc, s0:s0 + n], writes=[sgt[b].r])
                    pp = []
                    for i in range(3):
                        pv, prs = self.pb(1)
                        for k in range(KC):
                            self.mm(pv[:, 0:n], wt[b][i][:, k, :], ys[:, i, k, 0:n], [wt[b][i].r, ys.r], prs,
                                    k == 0, k == KC - 1, fast=True)
                        pp.append((pv, prs))
                    self.tt("dve", mg_[:, dc, 0:n], pp[0][0][:, 0:n], sgt[b][:, 0, 0:n], ALU.mult,
                            pp[0][1] + [sgt[b].r], [mg_.r])
                    for i in (1, 2):
                        self.tt("dve", tm[:, 0:n], pp[i][0][:, 0:n], sgt[b][:, i, 0:n], ALU.mult,
                                pp[i][1] + [sgt[b].r], [tm.r])
                        self.tt("dve", mg_[:, dc, 0:n], mg_[:, dc, 0:n], tm[:, 0:n], ALU.add, [mg_.r, tm.r], [mg_.r])
                for dc in range(KC):
                    pv, prs = self.pb(1)
                    for k in range(KC):
                        self.mm(pv[:, 0:n], wo[:, k, dc * 128:(dc + 1) * 128], mg_[:, k, 0:n], [wo.r, mg_.r], prs,
                                k == 0, k == KC - 1, fast=True)
                    self.act(og[:, dc, 0:n], pv[:, 0:n], AF.Copy, prs, [og.r])
                fw.dma(OUTT[:, :, s0:s0 + n], og[:, :, 0:n], reads=[og.r])
        fw.barrier()
        with ExitStack() as es:
            xt = [self.sb(es, f"rxt{i}", [128, T]) for i in range(2)]
            ot = [self.sb(es, f"rot{i}", [128, T]) for i in range(2)]
            for dc in range(KC):
                b = dc % 2
                fw.dma(xt[b][:], self.scr["XT"][dc], writes=[xt[b].r])
                fw.dma(ot[b][:], OUTT[:, dc, :], writes=[ot[b].r])
                rd = [xt[b].r, ot[b].r, self.mod.r]
                if not last:
                    self.stt(xt[b][:, 0:CTX], ot[b][:, 0:CTX], self.mod[:, l, 16 + dc, 1:2], xt[b][:, 0:CTX],
                             ALU.mult, ALU.add, rd, [xt[b].r])
                if l % 2 == 0:
                    xv, ov = xt[b][:, CTX:], ot[b][:, CTX:]
                else:
                    xv = xt[b][:, CTX:].rearrange("p (r c) -> p c r", c=cfg.GW)
                    ov = ot[b][:, CTX:].rearrange("p (c r) -> p c r", r=cfg.ROWS)
                self.stt(xv, ov, self.mod[:, l, 16 + dc, 0:1], xv, ALU.mult, ALU.add, rd, [xt[b].r])
                fw.dma(self.scr["XT"][dc], xt[b][:], reads=[xt[b].r])

    def stage_final(self):
        cfg, fw = self.cfg, self.fw
        XTv = self.scr["XT"].rearrange("k p t -> p k t")
        with ExitStack() as es:
            gb = self.sb(es, "gb", [128, D])
            fw.dma(gb[:], bc(self.inp["final_norm_g"][0:1, :], [128, D]), writes=[gb.r])
            xf = [self.sb(es, f"xf{i}", [128, KC, 128]) for i in range(2)]
            sq = [self.sb(es, f"fsq{i}", [128, D]) for i in range(2)]
            st = [self.sb(es, f"fst{i}", [128, 2]) for i in range(2)]
            for tl in range(2, cfg.NT):
                b = tl % 2
                fw.dma(xf[b][:], XTv[:, :, tl * 128:(tl + 1) * 128], writes=[xf[b].r])
                pv, prs = self.pb(2)
                for k in range(KC):
                    self.tr(pv[:, k * 128:(k + 1) * 128], xf[b][:, k, :], [xf[b].r], prs)
                self.act(sq[b][:], pv, AF.Square, prs, [sq[b].r])
                self.fw.op("dve", lambda h, b=b: h.tensor_reduce(out=st[b][:, 0:1], in_=sq[b][:], axis=AX.X, op=ALU.add),
                           reads=[sq[b].r], writes=[st[b].r])
                self.act(st[b][:, 1:2], st[b][:, 0:1], AF.Sqrt, [st[b].r], [st[b].r], bias=EPS, scale=1.0 / D)
                self.fw.op("dve", lambda h, b=b: h.reciprocal(out=st[b][:, 0:1], in_=st[b][:, 1:2]),
                           reads=[st[b].r], writes=[st[b].r])
                self.stt(sq[b][:], pv, st[b][:, 0:1], gb[:], ALU.mult, ALU.mult, prs + [st[b].r, gb.r], [sq[b].r])
                fw.dma(self.out[(tl - 2) * 128:(tl - 1) * 128, :], sq[b][:], reads=[sq[b].r])


def colfmt(v, nk):
    v = np.asarray(v, np.float32)
    lead = v.shape[:-1]
    return np.ascontiguousarray(np.moveaxis(v.reshape(lead + (nk, 128)), -1, 0))


def host_inputs(inputs, cfg, b):
    Dp = cfg.DEPTH
    m = {}
    f32 = lambda a: np.asarray(a, np.float32)
    m["x"] = np.ascontiguousarray(inputs["x"][b], dtype=np.float32)
    m["ctx"] = np.ascontiguousarray(inputs["ctx"][b], dtype=np.float32)
    cv = np.stack([inputs["c"][b], inputs["c_ctx"]], axis=0)
    m["cvec"] = np.ascontiguousarray(np.transpose(colfmt(cv, KC), (0, 2, 1)))
    m["w_ada"] = np.ascontiguousarray(inputs["w_ada"][:Dp], dtype=np.float32)
    m["b_ada_c"] = colfmt(inputs["b_ada"][:Dp], 24)
    m["norm_g_c"] = colfmt(inputs["norm_g"][:Dp], KC)
    m["w_in"] = np.ascontiguousarray(inputs["w_in"][:Dp], dtype=np.float32)
    m["consts"] = make_consts()
    m["final_norm_g"] = np.asarray(inputs["final_norm_g"], np.float32).reshape(1, D)
    m["w_branch"] = np.ascontiguousarray(f32(inputs["w_branch"][:Dp]))
    m["w_out"] = np.ascontiguousarray(f32(inputs["w_out"][:Dp]))
    G, Pn, Hh = 64, 64, 16
    lam = np.stack([f32(inputs["s5_lam_re"][:Dp]), f32(inputs["s5_lam_im"][:Dp])], axis=-1)
    lam = lam.reshape(Dp, 2, 32, 2, Pn, 2)
    m["s5_lam"] = np.ascontiguousarray(np.transpose(lam, (3, 4, 0, 2, 1, 5)).reshape(128, Dp, 32, 2, 2))
    ls = f32(inputs["s5_log_step"][:Dp]).reshape(Dp, 2, 32, 2)
    ls = np.broadcast_to(ls[..., None], (Dp, 2, 32, 2, Pn))
    m["s5_lstep"] = np.ascontiguousarray(np.transpose(ls, (3, 4, 0, 2, 1)).reshape(128, Dp, 32, 2))
    Bblk = np.zeros((2, Hh, Dp, 32, 2, 2, Pn), np.float32)
    Cblk = np.zeros((2, Pn, Dp, 32, 2, 2, Hh), np.float32)
    for ri, (bn, cn) in enumerate((("s5_b_re", "s5_c_re"), ("s5_b_im", "s5_c_im"))):
        Bm = f32(inputs[bn][:Dp]).reshape(Dp, 32, 2, Pn, Hh)
        Cm = f32(inputs[cn][:Dp]).reshape(Dp, 32, 2, Hh, Pn)
        for gl in range(2):
            Bblk[gl, :, :, :, ri, gl, :] = np.transpose(Bm[:, :, gl], (3, 0, 1, 2))
            Cblk[gl, :, :, :, ri, gl, :] = np.transpose(Cm[:, :, gl], (3, 0, 1, 2))
    m["s5_Bblk"] = np.ascontiguousarray(Bblk.reshape(32, Dp, 32, 2, 128))
    m["s5_Cblk"] = np.ascontiguousarray(Cblk.reshape(128, Dp, 32, 2, 32))
    m["s5_d_q"] = np.ascontiguousarray(np.transpose(f32(inputs["s5_d"][:Dp]).reshape(Dp, 32, 32), (2, 0, 1)))
    m["b_glu_c"] = colfmt(inputs["s5_b_glu"][:Dp], 16)
    m["s5_w_glu"] = np.ascontiguousarray(f32(inputs["s5_w_glu"][:Dp]))
    m["gdn_small"] = np.ascontiguousarray(np.concatenate(
        [f32(inputs["gdn_a_log"][:Dp]).reshape(Dp, 16), f32(inputs["gdn_dt_bias"][:Dp]).reshape(Dp, 16),
         f32(inputs["gdn_norm_g"][:Dp]).reshape(Dp, 128)], axis=1))
    m["gdn_conv_c"] = colfmt(inputs["gdn_conv"][:Dp], 24)
    m["ml_small"] = np.ascontiguousarray(np.concatenate(
        [f32(inputs["ml_i_bias"][:Dp]).reshape(Dp, 8), f32(inputs["ml_f_bias"][:Dp]).reshape(Dp, 8),
         f32(inputs["ml_norm_g"][:Dp]).reshape(Dp, 256)], axis=1))
    return m


def run(inputs, cfg, cores, dbg=(), stages=None):
    kb = KB(cfg, dbg=dbg, stages=stages)
    nc = kb.build()
    in_maps = [host_inputs(inputs, cfg, b) for b in cores]
    res = run_bass_kernel_spmd(nc, in_maps, core_ids=list(range(len(cores))))
    return kb, res


def kernel(**inputs):
    cfg = Cfg()
    kb, res = run(inputs, cfg, list(range(N_CORES)))
    return np.stack([np.asarray(r["y"], dtype=np.float32) for r in res.results], axis=0)
```
